# Optimizing a Trainium2 kernel written in Bass

```python
import jax, jax.numpy as jnp
from jax import lax
import numpy as np

D_MODEL = 2048
BATCH = 1
SEQ = 8192
DEPTH = 1

HEAD_DIM = 128
DILATED_GROUPS = ((128, 1), (512, 4), (2048, 16))
HEADS_PER_GROUP = 8
N_HEADS_A = HEADS_PER_GROUP * len(DILATED_GROUPS)
A_OUT_WIDTH = HEADS_PER_GROUP * HEAD_DIM
ATTN_BLOCK = 128
NUM_BUCKETS = 32
MAX_DISTANCE = 2048
NEG_INF = -1e30
RET_HEADS = 8
RET_QK_DIM = 128
RET_V_DIM = 256
RET_V_WIDTH = RET_HEADS * RET_V_DIM
RET_CHUNK = 128
ROPE_BASE = 10000.0
GN_EPS = 1e-5
N_EXPERTS = 64
N_GROUPS = 8
TOPK_GROUPS = 4
TOP_K = 8
EXPERT_DIM = 512
SHARED_DIM = 512
ROUTED_SCALE = 2.5
MOE_BLOCK = 128
RMS_EPS = 1e-6
A_QKV = N_HEADS_A * HEAD_DIM
R_QK = RET_HEADS * RET_QK_DIM
IN_WIDTH = 3 * A_QKV + 2 * R_QK + 2 * RET_V_WIDTH + 2 * D_MODEL

kernel_name = 'hybrid_dilated_retention_moe_block'


def _rmsnorm(x, g):
    xf = x.astype(jnp.float32)
    y = xf * lax.rsqrt(jnp.mean(xf * xf, axis=-1, keepdims=True) + RMS_EPS)
    return (y * g.astype(jnp.float32)).astype(x.dtype)


def _t5_bucket(dist):
    max_exact = NUM_BUCKETS // 2
    safe = np.maximum(dist, 1).astype(np.float32)
    large = max_exact + (np.log(safe / max_exact) / np.log(MAX_DISTANCE / max_exact)
                         * (NUM_BUCKETS - max_exact)).astype(np.int32)
    return np.where(dist < max_exact, dist, np.minimum(large, NUM_BUCKETS - 1)).astype(np.int32)


def _dilated_group(q, k, v, bias_tab, w_steps, dilation):
    B, S, H, E = q.shape
    L = S // dilation
    nb = -(-L // ATTN_BLOCK)
    Lp = nb * ATTN_BLOCK

    def to_res(t):
        t = t.reshape(B, L, dilation, H, E).transpose(0, 2, 3, 1, 4)
        return jnp.pad(t, ((0, 0), (0, 0), (0, 0), (0, Lp - L), (0, 0)))

    def key_blocks(t):
        t = jnp.pad(to_res(t), ((0, 0), (0, 0), (0, 0), (ATTN_BLOCK, 0), (0, 0)))
        t = t.reshape(B, dilation, H, nb + 1, ATTN_BLOCK, E)
        return jnp.concatenate([t[:, :, :, :-1], t[:, :, :, 1:]], axis=4)

    qb = to_res(q).reshape(B, dilation, H, nb, ATTN_BLOCK, E)
    kb, vb = key_blocks(k), key_blocks(v)
    a = np.arange(ATTN_BLOCK)[:, None]
    cc = np.arange(2 * ATTN_BLOCK)[None, :]
    delta = ATTN_BLOCK + a - cc
    band = (delta >= 0) & (delta <= w_steps)
    key_ok = ((np.arange(nb)[:, None] - 1) * ATTN_BLOCK + np.arange(2 * ATTN_BLOCK)[None, :]) >= 0
    mask = band[None] & key_ok[:, None, :]
    bucket = _t5_bucket(np.maximum(delta, 0) * dilation)
    bias = jnp.transpose(bias_tab.astype(jnp.float32)[bucket], (2, 0, 1))
    s = jnp.einsum('bdhnqe,bdhnke->bdhnqk', qb, kb, preferred_element_type=jnp.float32)
    s = jnp.where(mask, s + bias[None, None, :, None], NEG_INF)
    m = jnp.max(s, axis=-1, keepdims=True)
    p = jnp.exp(s - m)
    l = jnp.sum(p, axis=-1, keepdims=True)
    o = jnp.einsum('bdhnqk,bdhnke->bdhnqe', p.astype(v.dtype), vb,
                   preferred_element_type=jnp.float32) / l
    lse = (m + jnp.log(l))[..., 0]

    def from_res(t):
        rest = t.shape[5:]
        t = t.reshape((B, dilation, H, Lp) + rest)[:, :, :, :L]
        perm = (0, 3, 1, 2) + tuple(range(4, t.ndim))
        return t.transpose(perm).reshape((B, S, H) + rest)

    return from_res(o), from_res(lse)


def _rotary(t):
    S, E = t.shape[1], t.shape[3]
    inv = ROPE_BASE ** (-jnp.arange(0, E, 2, dtype=jnp.float32) / E)
    ang = jnp.arange(S, dtype=jnp.float32)[:, None] * inv[None, :]
    cos, sin = jnp.cos(ang)[None, :, None, :], jnp.sin(ang)[None, :, None, :]
    tf = t.astype(jnp.float32)
    t1, t2 = tf[..., :E // 2], tf[..., E // 2:]
    return jnp.concatenate([t1 * cos - t2 * sin, t1 * sin + t2 * cos], axis=-1).astype(t.dtype)


def _retention(q, k, v):
    B, S, H, DK = q.shape
    DV = v.shape[-1]
    C = RET_CHUNK
    N = S // C
    log_g = jnp.log1p(-jnp.exp2(-5.0 - jnp.arange(H, dtype=jnp.float32)))

    def chunk(t):
        return t.astype(jnp.float32).reshape(B, N, C, H, t.shape[-1]).transpose(0, 3, 1, 2, 4)

    qc, kc, vc = chunk(q), chunk(k), chunk(v)
    idx = jnp.arange(C, dtype=jnp.float32)
    diff = idx[:, None] - idx[None, :]
    dmat = jnp.where(diff >= 0, jnp.exp(log_g[:, None, None] * jnp.maximum(diff, 0.0)), 0.0)
    inner = jnp.einsum('bhnqd,bhnkd->bhnqk', qc, kc) * dmat[None, :, None]
    inner = jnp.einsum('bhnqk,bhnke->bhnqe', inner, vc)
    zeta = jnp.exp(log_g[:, None] * (C - 1 - idx))
    upd = jnp.einsum('bhnkd,bhnke->bhnde', kc, vc * zeta[None, :, None, :, None])
    g_chunk = jnp.exp(log_g * C)[None, :, None, None]

    def step(state, u):
        return g_chunk * state + u, state

    _, prev = lax.scan(step, jnp.zeros((B, H, DK, DV), jnp.float32), jnp.moveaxis(upd, 2, 0))
    prev = jnp.moveaxis(prev, 0, 2)
    xi = jnp.exp(log_g[:, None] * (idx + 1.0))
    cross = jnp.einsum('bhnqd,bhnde->bhnqe', qc, prev) * xi[None, :, None, :, None]
    return (inner + cross).transpose(0, 2, 3, 1, 4).reshape(B, S, H, DV)


def _moe(h, router_w, router_bias, w_gate_e, w_up_e, w_down_e):
    T, D = h.shape
    E = N_EXPERTS
    scores = jax.nn.sigmoid(jnp.dot(h.astype(jnp.float32), router_w.astype(jnp.float32)))
    sel = scores + router_bias.astype(jnp.float32)
    grp_score = lax.top_k(sel.reshape(T, N_GROUPS, E // N_GROUPS), 2)[0].sum(-1)
    _, grp_idx = lax.top_k(grp_score, TOPK_GROUPS)
    grp_mask = jax.nn.one_hot(grp_idx, N_GROUPS, dtype=jnp.float32).sum(1)
    exp_mask = jnp.repeat(grp_mask, E // N_GROUPS, axis=1) > 0
    _, top_idx = lax.top_k(jnp.where(exp_mask, sel, -jnp.inf), TOP_K)
    top_w = jnp.take_along_axis(scores, top_idx, axis=1)
    top_w = top_w / jnp.sum(top_w, axis=-1, keepdims=True) * ROUTED_SCALE
    TK = T * TOP_K
    flat_e = top_idx.reshape(TK)
    flat_w = top_w.reshape(TK)
    order = jnp.argsort(flat_e)
    sorted_e = flat_e[order]
    counts = jnp.bincount(flat_e, length=E)
    padded = (counts + MOE_BLOCK - 1) // MOE_BLOCK * MOE_BLOCK
    pad_end = jnp.cumsum(padded)
    pad_start = pad_end - padded
    start = jnp.cumsum(counts) - counts
    dest = pad_start[sorted_e] + jnp.arange(TK, dtype=jnp.int32) - start[sorted_e]
    n_blocks = (TK + E * (MOE_BLOCK - 1) + MOE_BLOCK - 1) // MOE_BLOCK
    P = n_blocks * MOE_BLOCK
    tok_buf = jnp.full((P,), T, jnp.int32).at[dest].set((order // TOP_K).astype(jnp.int32))
    w_buf = jnp.zeros((P,), jnp.float32).at[dest].set(flat_w[order])
    block_e = jnp.minimum(jnp.searchsorted(pad_end, jnp.arange(n_blocks) * MOE_BLOCK, side='right'), E - 1)
    h_pad = jnp.concatenate([h, jnp.zeros((1, D), h.dtype)], axis=0)

    def body(out, blk):
        tok, wt, e = blk
        xb = h_pad[tok]
        y = (jax.nn.silu(xb @ w_gate_e[e]) * (xb @ w_up_e[e])) @ w_down_e[e]
        return out.at[tok].add(y.astype(jnp.float32) * wt[:, None]), None

    out, _ = lax.scan(body, jnp.zeros((T + 1, D), jnp.float32),
                      (tok_buf.reshape(n_blocks, MOE_BLOCK), w_buf.reshape(n_blocks, MOE_BLOCK), block_e))
    return out[:T].astype(h.dtype)


def setup_inputs(seed: int = 0) -> dict:
    key = jax.random.key(seed)
    ks = jax.random.split(key, 22)
    f32 = jnp.float32

    def nrm(k, shape, scale):
        return jax.random.normal(k, shape, f32) * scale

    L = DEPTH
    return {
        'x': nrm(ks[0], (BATCH, SEQ, D_MODEL), 1.0),
        'c': nrm(ks[1], (BATCH, D_MODEL), 1.0),
        'rel_bias': nrm(ks[2], (NUM_BUCKETS, N_HEADS_A), 0.5),
        'w_ada': nrm(ks[3], (L, D_MODEL, 6 * D_MODEL), 0.5 * D_MODEL ** -0.5),
        'b_ada': nrm(ks[4], (L, 6 * D_MODEL), 0.02),
        'ln1_g': 1.0 + nrm(ks[5], (L, D_MODEL), 0.02),
        'w_in': nrm(ks[6], (L, D_MODEL, IN_WIDTH), D_MODEL ** -0.5),
        'q_norm_g': 1.0 + nrm(ks[7], (L, HEAD_DIM), 0.02),
        'k_norm_g': 1.0 + nrm(ks[8], (L, HEAD_DIM), 0.02),
        'ret_gn_g': 1.0 + nrm(ks[9], (L, RET_V_WIDTH), 0.02),
        'p_a': nrm(ks[10], (L, A_OUT_WIDTH, D_MODEL), A_OUT_WIDTH ** -0.5),
        'p_b': nrm(ks[11], (L, RET_V_WIDTH, D_MODEL), RET_V_WIDTH ** -0.5),
        'w_o': nrm(ks[12], (L, D_MODEL, D_MODEL), D_MODEL ** -0.5),
        'ln2_g': 1.0 + nrm(ks[13], (L, D_MODEL), 0.02),
        'router_w': nrm(ks[14], (L, D_MODEL, N_EXPERTS), D_MODEL ** -0.5),
        'router_bias': nrm(ks[15], (L, N_EXPERTS), 0.01),
        'w_gate_e': nrm(ks[16], (L, N_EXPERTS, D_MODEL, EXPERT_DIM), D_MODEL ** -0.5),
        'w_up_e': nrm(ks[17], (L, N_EXPERTS, D_MODEL, EXPERT_DIM), D_MODEL ** -0.5),
        'w_down_e': nrm(ks[18], (L, N_EXPERTS, EXPERT_DIM, D_MODEL), EXPERT_DIM ** -0.5),
        'w_gate_s': nrm(ks[19], (L, D_MODEL, SHARED_DIM), D_MODEL ** -0.5),
        'w_up_s': nrm(ks[20], (L, D_MODEL, SHARED_DIM), D_MODEL ** -0.5),
        'w_down_s': nrm(ks[21], (L, SHARED_DIM, D_MODEL), SHARED_DIM ** -0.5),
    }


def reference(x, c, rel_bias, w_ada, b_ada, ln1_g, w_in, q_norm_g, k_norm_g, ret_gn_g,
              p_a, p_b, w_o, ln2_g, router_w, router_bias, w_gate_e, w_up_e, w_down_e,
              w_gate_s, w_up_s, w_down_s):
    B, S, D = x.shape
    sizes = [A_QKV, A_QKV, A_QKV, R_QK, R_QK, RET_V_WIDTH, RET_V_WIDTH, D_MODEL, D_MODEL]
    cuts = [int(v) for v in np.cumsum(sizes)[:-1]]
    for l in range(DEPTH):
        mod = jax.nn.silu(c) @ w_ada[l] + b_ada[l]
        sh1, sc1, g1, sh2, sc2, g2 = jnp.split(mod[:, None, :], 6, axis=-1)
        h = _rmsnorm(x, ln1_g[l]) * (1.0 + sc1) + sh1
        proj = h @ w_in[l]
        qa, ka, va, qr, kr, vr, gr, gate_a, gate_b = jnp.split(proj, cuts, axis=-1)
        qa = _rmsnorm(qa.reshape(B, S, N_HEADS_A, HEAD_DIM), q_norm_g[l]) * (HEAD_DIM ** -0.5)
        ka = _rmsnorm(ka.reshape(B, S, N_HEADS_A, HEAD_DIM), k_norm_g[l])
        va = va.reshape(B, S, N_HEADS_A, HEAD_DIM)
        outs, lses = [], []
        for gi, (win, dil) in enumerate(DILATED_GROUPS):
            hs = slice(gi * HEADS_PER_GROUP, (gi + 1) * HEADS_PER_GROUP)
            o, lse = _dilated_group(qa[:, :, hs], ka[:, :, hs], va[:, :, hs], rel_bias[:, hs], win // dil, dil)
            outs.append(o)
            lses.append(lse)
        alpha = jax.nn.softmax(jnp.stack(lses, axis=0), axis=0)
        y_a = jnp.sum(alpha[..., None] * jnp.stack(outs, axis=0), axis=0)
        y_a = y_a.reshape(B, S, A_OUT_WIDTH).astype(x.dtype)
        qr = _rotary(qr.reshape(B, S, RET_HEADS, RET_QK_DIM))
        kr = _rotary(kr.reshape(B, S, RET_HEADS, RET_QK_DIM)) * (RET_QK_DIM ** -0.5)
        ret = _retention(qr, kr, vr.reshape(B, S, RET_HEADS, RET_V_DIM))
        mu = jnp.mean(ret, axis=-1, keepdims=True)
        var = jnp.mean(jnp.square(ret - mu), axis=-1, keepdims=True)
        ret = ((ret - mu) * lax.rsqrt(var + GN_EPS)).reshape(B, S, RET_V_WIDTH) * ret_gn_g[l].astype(jnp.float32)
        y_b = (ret * jax.nn.silu(gr.astype(jnp.float32))).astype(x.dtype)
        merged = jax.nn.sigmoid(gate_a) * (y_a @ p_a[l]) + jax.nn.sigmoid(gate_b) * (y_b @ p_b[l])
        x = x + g1 * (merged @ w_o[l])
        h2 = _rmsnorm(x, ln2_g[l]) * (1.0 + sc2) + sh2
        hf = h2.reshape(B * S, D)
        routed = _moe(hf, router_w[l], router_bias[l], w_gate_e[l], w_up_e[l], w_down_e[l])
        shared = (jax.nn.silu(hf @ w_gate_s[l]) * (hf @ w_up_s[l])) @ w_down_s[l]
        x = x + g2 * (routed + shared).reshape(B, S, D)
    return x
```

```python
import math
from contextlib import ExitStack

import numpy as np
import ml_dtypes
import concourse.bass as bass
import concourse.mybir as mybir
from concourse.bass_utils import run_bass_kernel_spmd

F32 = mybir.dt.float32
BF16 = mybir.dt.bfloat16
ALU = mybir.AluOpType
AF = mybir.ActivationFunctionType
AX = mybir.AxisListType

NCORES = 8
S = 8192
D = 2048
KC = 16
NEG = -30000.0
RMS_EPS = 1e-6
GN_EPS = 1e-5
DILS = (1, 4, 16)
NDSEM = 90


class DSem:
    def __init__(self, sem):
        self.sem = sem
        self.total = 0


class Res:
    def __init__(self, name):
        self.name = name
        self.lw = []
        self.rd = []
        self.dsem = None


class Tl:
    def __init__(self, k, t, name):
        self.k = k
        self.t = t
        self.name = name
        self.whole = Res(name)
        self.subs = {}

    def r(self, key):
        if key not in self.subs:
            self.subs[key] = Res(f"{self.name}.{key}")
        return self.subs[key]

    def __getitem__(self, idx):
        return self.t[idx]


def _res(x):
    return x.whole if isinstance(x, Tl) else x


class K:
    def __init__(self, nc):
        self.nc = nc
        self.eng = {"pe": nc.tensor, "dve": nc.vector, "act": nc.scalar, "pool": nc.gpsimd, "sp": nc.sync}
        self.esem = {}
        self.ecnt = {}
        for q in ("pe", "dve", "act", "pool"):
            self.esem[q] = nc.alloc_semaphore(name="e_" + q)
            self.ecnt[q] = 0
        self.waited = {}
        self.dsems = []
        self.nm = 0
        self.dsems = [DSem(nc.alloc_semaphore(name=f"d{i}")) for i in range(NDSEM)]
        self.dfree = list(self.dsems)
        self.bound = []
        for q in self.esem:
            nc.gpsimd.sem_clear(self.esem[q])
        for d in self.dsems:
            nc.gpsimd.sem_clear(d.sem)
        nc.all_engine_barrier()

    def sb(self, es, name, shape, dt):
        self.nm += 1
        t = es.enter_context(self.nc.sbuf_tensor(f"s{self.nm}_{name}", list(shape), dt))
        return Tl(self, t, name)

    def ps(self, es, name, shape, dt):
        self.nm += 1
        t = es.enter_context(self.nc.psum_tensor(f"p{self.nm}_{name}", list(shape), dt))
        return Tl(self, t, name)

    def dram(self, name, shape, dt, kind):
        t = self.nc.dram_tensor(name, list(shape), dt, kind=kind).ap()
        return Tl(self, t, name)

    def _wait(self, q, toks):
        best = {}
        for tok in toks:
            s, v = tok
            if isinstance(s, DSem):
                v = s.total
                key = id(s)
                semh = s.sem
            else:
                key = s
                semh = self.esem[s]
                if s == q and q == "pe":
                    continue
            if self.waited.get((q, key), 0) >= v:
                continue
            if key not in best or best[key][1] < v:
                best[key] = (semh, v)
        for key, (semh, v) in best.items():
            self.eng[q].wait_ge(semh, v)
            self.waited[(q, key)] = v

    def _deps(self, reads, writes):
        deps = []
        for r in reads:
            deps += _res(r).lw
        for w in writes:
            w = _res(w)
            deps += w.lw
            deps += w.rd
        return deps

    def _commit(self, tok, reads, writes):
        for r in reads:
            _res(r).rd.append(tok)
        for w in writes:
            w = _res(w)
            w.lw = [tok]
            w.rd = []

    def op(self, q, fn, reads=(), writes=()):
        self._wait(q, self._deps(reads, writes))
        ins = fn(self.eng[q])
        self.ecnt[q] += 1
        ins.then_inc(self.esem[q], 1)
        self._commit((q, self.ecnt[q]), reads, writes)

    def pe(self, fn, reads=(), writes=()):
        self.op("pe", fn, reads, writes)

    def dve(self, fn, reads=(), writes=()):
        self.op("dve", fn, reads, writes)

    def act(self, fn, reads=(), writes=()):
        self.op("act", fn, reads, writes)

    def pool(self, fn, reads=(), writes=()):
        self.op("pool", fn, reads, writes)

    def dma(self, q, out, in_, reads=(), writes=(), semres=None, **kw):
        r = _res(semres)
        if r.dsem is None:
            r.dsem = self.dfree.pop(0)
            self.bound.append(r)
        self._wait(q, self._deps(reads, writes))
        ins = self.eng[q].dma_start(out=out, in_=in_, **kw)
        ins.then_inc(r.dsem.sem, 16)
        r.dsem.total += 16
        self._commit((r.dsem, r.dsem.total), reads, writes)

    def barrier(self):
        toks = [(q, self.ecnt[q]) for q in self.esem if self.ecnt[q] > 0]
        toks += [(d, d.total) for d in self.dsems if d.total > 0]
        for q in self.eng:
            self._wait(q, toks)
        for r in self.bound:
            self.dfree.append(r.dsem)
            r.dsem = None
        self.bound = []

    def finish(self):
        toks = [(q, self.ecnt[q]) for q in self.esem if self.ecnt[q] > 0]
        toks += [(d, d.total) for d in self.dsems if d.total > 0]
        self._wait("sp", toks)
        self._wait("pool", toks)
        self.nc.all_engine_barrier()
        for q in self.esem:
            self.nc.gpsimd.sem_clear(self.esem[q])
        for d in self.dsems:
            self.nc.gpsimd.sem_clear(d.sem)


def mm(out, lhsT, rhs, start, stop):
    return lambda e: e.matmul(out, lhsT, rhs, start=start, stop=stop)


def tt(out, a, b, op):
    return lambda e: e.tensor_tensor(out=out, in0=a, in1=b, op=op)


def ts(out, a, s1, op0, s2=None, op1=None):
    if op1 is None:
        return lambda e: e.tensor_scalar(out=out, in0=a, scalar1=s1, scalar2=None, op0=op0)
    return lambda e: e.tensor_scalar(out=out, in0=a, scalar1=s1, scalar2=s2, op0=op0, op1=op1)


def stt(out, a, sc, b, op0, op1):
    return lambda e: e.scalar_tensor_tensor(out=out, in0=a, scalar=sc, in1=b, op0=op0, op1=op1)


def actf(out, in_, func, bias=None, scale=1.0, accum_out=None):
    kw = {}
    if bias is not None:
        kw["bias"] = bias
    if accum_out is not None:
        kw["accum_out"] = accum_out
    return lambda e: e.activation(out=out, in_=in_, func=func, scale=scale, **kw)


def cp(out, in_):
    return lambda e: e.tensor_copy(out=out, in_=in_)


def recip(out, in_):
    return lambda e: e.reciprocal(out=out, in_=in_)


def emit_silu_c(k, es, c_d):
    cf = k.sb(es, "c_f", [128, KC], F32)
    cb = k.sb(es, "c_b", [128, KC], BF16)
    k.dma("sp", cf[:, :], c_d[:, :], writes=[cf], semres=cf)
    k.act(actf(cb[:, :], cf[:, :], AF.Silu), reads=[cf], writes=[cb])
    return cb


def emit_mod_cols(k, es, cb, wada_d, bada_d, ncols_tiles, name, ps_bank):
    nt = ncols_tiles
    out = k.sb(es, name, [128, nt], F32)
    bt = k.sb(es, name + "_b", [128, nt], F32)
    k.dma("sp", bt[:, :], bada_d[:, :], writes=[bt], semres=bt)
    wv = wada_d.t.rearrange("(kc p) n -> p kc n", p=128)
    with ExitStack() as es2:
        wbufs = [k.sb(es2, f"{name}_w{i}", [128, KC, 512], BF16) for i in range(2)]
        ngrp = (nt * 128 + 511) // 512
        for g in range(ngrp):
            wb = wbufs[g % 2]
            c0 = g * 512
            cw = min(512, nt * 128 - c0)
            for h in range(2):
                k.dma("pool", wb[:, h * 8:(h + 1) * 8, 0:cw], wv[:, h * 8:(h + 1) * 8, c0:c0 + cw],
                      writes=[wb], semres=wb)
            for j in range(cw // 128):
                col = g * 4 + j
                for kc in range(KC):
                    k.pe(mm(ps_bank[:, col:col + 1], wb[:, kc, j * 128:(j + 1) * 128], cb[:, kc:kc + 1],
                            kc == 0, kc == KC - 1), reads=[wb, cb], writes=[ps_bank])
        k.dve(tt(out[:, :], ps_bank[:, 0:nt], bt[:, :], ALU.add), reads=[ps_bank, bt], writes=[out])
        k.barrier()
    return out


def build_l1(debug=False):
    nc = bass.Bass("TRN2", target_bir_lowering=False)
    k = K(nc)
    EI, EO, IN = "ExternalInput", "ExternalOutput", "Internal"
    xT_d = k.dram("xT", [D, S], F32, EI)
    c_d = k.dram("c_pk", [128, KC], F32, EI)
    wada_d = k.dram("wada1", [D, 4096], F32, EI)
    bada_d = k.dram("bada1", [128, 32], F32, EI)
    ln1_d = k.dram("ln1_pk", [128, KC], F32, EI)
    win_d = k.dram("win", [D, 1920], F32, EI)
    qg_d = k.dram("qg", [128, 1], F32, EI)
    kg_d = k.dram("kg", [128, 1], F32, EI)
    rb_d = k.dram("rb", [1, 96], F32, EI)
    idx_d = k.dram("idxT", [128, 3 * 256], F32, EI)
    neg_d = k.dram("negT", [128, 256], F32, EI)
    cos_d = k.dram("cosT", [128, S], F32, EI)
    sin_d = k.dram("sinT", [128, S], F32, EI)
    rm_d = k.dram("rmat", [128, 128], F32, EI)
    idf_d = k.dram("identf", [128, 128], F32, EI)
    dmat_d = k.dram("dmatT", [128, 128], F32, EI)
    xi_d = k.dram("xibc", [128, 128], F32, EI)
    zeta_d = k.dram("zetacol", [128, 1], F32, EI)
    gch_d = k.dram("gchunk", [128, 1], F32, EI)
    gng_d = k.dram("gng", [1, 256], F32, EI)
    yT_d = k.dram("yT", [384, S], BF16, EO)
    qk_s = k.dram("qk_s", [8, 128, S], BF16, EO if debug else IN)
    vt_s = k.dram("vt_s", [S, 896], BF16, EO if debug else IN)

    with ExitStack() as es0:
        ones_b = k.sb(es0, "ones_b", [128, 128], BF16)
        idf = k.sb(es0, "idf", [128, 128], F32)
        idb = k.sb(es0, "idb", [128, 128], BF16)
        k.dve(lambda e: e.memset(ones_b[:, :], 1.0), writes=[ones_b])
        k.dma("sp", idf[:, :], idf_d[:, :], writes=[idf], semres=idf)
        k.dve(cp(idb[:, :], idf[:, :]), reads=[idf], writes=[idb])

        with ExitStack() as es:
            psA = [k.ps(es, f"psA{i}", [128, 512], F32) for i in range(8)]
            cb = emit_silu_c(k, es, c_d)
            mod1 = emit_mod_cols(k, es, cb, wada_d, bada_d, 32, "mod1", psA[0])
            ln1 = k.sb(es, "ln1", [128, KC], F32)
            k.dma("sp", ln1[:, :], ln1_d[:, :], writes=[ln1], semres=ln1)
            gg = k.sb(es, "gg", [128, KC], F32)
            k.dve(stt(gg[:, :], mod1[:, 16:32], 1.0, ln1[:, :], ALU.add, ALU.mult), reads=[mod1, ln1], writes=[gg])
            sh1b = k.sb(es, "sh1b", [128, KC], BF16)
            k.dve(cp(sh1b[:, :], mod1[:, 0:16]), reads=[mod1], writes=[sh1b])
            sh1bc = k.sb(es, "sh1bc", [128, KC, 128], BF16)
            k.dve(cp(sh1bc[:, :, :], sh1b[:, :].unsqueeze(2).to_broadcast([128, KC, 128])), reads=[sh1b], writes=[sh1bc])

            wb = k.sb(es, "wb", [128, KC, 1920], BF16)
            wv = win_d.t.rearrange("(kc p) n -> p kc n", p=128)
            for kc4 in range(4):
                k.dma("pool", wb[:, kc4 * 4:(kc4 + 1) * 4, :], wv[:, kc4 * 4:(kc4 + 1) * 4, :], writes=[wb], semres=wb)
            b1col = k.sb(es, "b1col", [128, 8], F32)
            for f in range(8):
                for kc in range(KC):
                    k.pe(mm(psA[1][:, f:f + 1], wb[:, kc, f * 128:(f + 1) * 128], sh1b[:, kc:kc + 1], kc == 0, kc == KC - 1),
                         reads=[wb, sh1b], writes=[psA[1]])
            k.dve(cp(b1col[:, :], psA[1][:, 0:8]), reads=[psA[1]], writes=[b1col])
            b1bc = k.sb(es, "b1bc", [128, 896], F32)
            for j in range(2):
                for kc in range(KC):
                    k.pe(mm(psA[2 + j][:, 0:448], sh1bc[:, kc, :], wb[:, kc, 1024 + j * 448:1024 + (j + 1) * 448], kc == 0, kc == KC - 1),
                         reads=[wb, sh1bc], writes=[psA[2 + j]])
                k.dve(cp(b1bc[:, j * 448:(j + 1) * 448], psA[2 + j][:, 0:448]), reads=[psA[2 + j]], writes=[b1bc])
            for kc in range(KC):
                eng = k.dve if kc % 2 == 0 else k.pool
                eng(ts(wb[:, kc, :], wb[:, kc, :], gg[:, kc:kc + 1], ALU.mult), reads=[wb, gg], writes=[wb])

            qg = k.sb(es, "qg", [128, 1], F32)
            kg = k.sb(es, "kg", [128, 1], F32)
            k.dma("sp", qg[:, :], qg_d[:, :], writes=[qg], semres=qg)
            k.dma("sp", kg[:, :], kg_d[:, :], writes=[kg], semres=kg)
            k.dve(ts(qg[:, :], qg[:, :], 128.0 ** -0.5, ALU.mult), reads=[qg], writes=[qg])
            b1k = k.sb(es, "b1k", [128, 1], F32)
            k.dve(ts(b1k[:, :], b1col[:, 7:8], 128.0 ** -0.5, ALU.mult), reads=[b1col], writes=[b1k])
            rmf = k.sb(es, "rmf", [128, 128], F32)
            rmb = k.sb(es, "rmb", [128, 128], BF16)
            k.dma("sp", rmf[:, :], rm_d[:, :], writes=[rmf], semres=rmf)
            k.dve(cp(rmb[:, :], rmf[:, :]), reads=[rmf], writes=[rmb])
            epsr = k.sb(es, "epsr", [128, 1], F32)
            k.dve(lambda e: e.memset(epsr[:, :], RMS_EPS), writes=[epsr])

            if debug == "A0":
                dbg = k.dram("dbg", [128, 2048], F32, EO)
                stg = k.sb(es, "stg", [128, 2048], F32)
                k.dve(lambda e: e.memset(stg[:, :], 0.0), writes=[stg])
                k.dve(cp(stg[:, 0:32], mod1[:, :]), reads=[mod1], writes=[stg])
                k.dve(cp(stg[:, 32:48], gg[:, :]), reads=[gg], writes=[stg])
                k.dve(cp(stg[:, 48:56], b1col[:, :]), reads=[b1col], writes=[stg])
                k.dve(cp(stg[:, 64:960], b1bc[:, :]), reads=[b1bc], writes=[stg])
                k.dve(cp(stg[:, 960:976], cb[:, :]), reads=[cb], writes=[stg])
                k.dve(cp(stg[:, 1024:1536], wb[:, 3, 0:512]), reads=[wb], writes=[stg])
                k.dma("sp", dbg.t[:, :], stg[:, :], reads=[stg], writes=[dbg], semres=stg)
                k.finish()
                return nc
            NT = S // 512
            xbs = [k.sb(es, f"xb{i}", [128, KC, 512], BF16) for i in range(2)]
            sq = k.sb(es, "sq", [128, KC, 512], BF16)
            rstd = k.sb(es, "rstd", [128, 512], F32)
            rcol = k.sb(es, "rcol", [128, 4], F32)
            cs = [k.sb(es, f"cos{i}", [128, 512], F32) for i in range(2)]
            sn = [k.sb(es, f"sin{i}", [128, 512], F32) for i in range(2)]
            t1 = [k.sb(es, f"t1_{i}", [128, 512], F32) for i in range(2)]
            qf = [k.sb(es, f"qf_{i}", [128, 512], F32) for i in range(2)]
            qsq = [k.sb(es, f"qsq_{i}", [128, 512], BF16) for i in range(2)]
            rq = [k.sb(es, f"rq_{i}", [128, 512], F32) for i in range(2)]
            qo = [k.sb(es, f"qo_{i}", [128, 512], BF16) for i in range(4)]
            qfb = [k.sb(es, f"qfb_{i}", [128, 512], BF16) for i in range(2)]
            ra = [k.sb(es, f"ra_{i}", [128, 512], F32) for i in range(2)]
            vo = [k.sb(es, f"vo_{i}", [128, 896], BF16) for i in range(2)]
            xv = xT_d.t.rearrange("(kc p) s -> p kc s", p=128)
            nq = 0
            nv = 0

            def load_x(t):
                xb = xbs[t % 2]
                for h in range(4):
                    k.dma("pool", xb[:, h * 4:(h + 1) * 4, :], xv[:, h * 4:(h + 1) * 4, t * 512:(t + 1) * 512],
                          writes=[xb], semres=xb)

            load_x(0)
            for t in range(NT):
                xb = xbs[t % 2]
                if t + 1 < NT:
                    load_x(t + 1)
                c0 = t * 512
                k.dma("sp", cs[t % 2][:, :], cos_d[:, c0:c0 + 512], writes=[cs[t % 2]], semres=cs[t % 2])
                k.dma("sp", sn[t % 2][:, :], sin_d[:, c0:c0 + 512], writes=[sn[t % 2]], semres=sn[t % 2])
                for h in range(2):
                    k.act(actf(sq[:, h * 8:(h + 1) * 8, :], xb[:, h * 8:(h + 1) * 8, :], AF.Square), reads=[xb], writes=[sq.r(h)])
                for kc in range(KC):
                    k.pe(mm(psA[0][:, :], ones_b[:, :], sq[:, kc, :], kc == 0, kc == KC - 1),
                         reads=[sq.r(kc // 8), ones_b], writes=[psA[0]])
                k.act(actf(rstd[:, :], psA[0][:, :], AF.Sqrt, bias=epsr[:, :], scale=1.0 / D), reads=[psA[0], epsr], writes=[rstd])
                k.dve(recip(rstd[:, :], rstd[:, :]), reads=[rstd], writes=[rstd])
                for s4 in range(4):
                    k.pe(lambda e, s4=s4: e.transpose(psA[1][:, s4 * 128:(s4 + 1) * 128], rstd[:, s4 * 128:(s4 + 1) * 128], idf[:, :]),
                         reads=[rstd, idf], writes=[psA[1]])
                k.dve(cp(rcol[:, :], psA[1][:, :].rearrange("p (s n) -> p s n", n=128)[:, :, 0]), reads=[psA[1]], writes=[rcol])
                for f in range(8):
                    pb = psA[2 + (f % 2)]
                    for kc in range(KC):
                        k.pe(mm(pb[:, :], wb[:, kc, f * 128:(f + 1) * 128], xb[:, kc, :], kc == 0, kc == KC - 1),
                             reads=[wb, xb], writes=[pb])
                    a = nq % 2
                    nq += 1
                    k.dve(tt(t1[a][:, :], pb[:, :], rstd[:, :], ALU.mult), reads=[pb, rstd], writes=[t1[a]])
                    if f < 6:
                        gcol = qg if f % 2 == 0 else kg
                        k.act(actf(qsq[a][:, :], t1[a][:, :], AF.Square, bias=b1col[:, f:f + 1]), reads=[t1[a], b1col], writes=[qsq[a]])
                        k.act(actf(qf[a][:, :], t1[a][:, :], AF.Identity, bias=b1col[:, f:f + 1]), reads=[t1[a], b1col], writes=[qf[a]])
                        pn = psA[4 + a]
                        k.pe(mm(pn[:, :], ones_b[:, :], qsq[a][:, :], True, True), reads=[qsq[a], ones_b], writes=[pn])
                        k.act(actf(rq[a][:, :], pn[:, :], AF.Sqrt, bias=epsr[:, :], scale=1.0 / 128), reads=[pn, epsr], writes=[rq[a]])
                        k.dve(recip(rq[a][:, :], rq[a][:, :]), reads=[rq[a]], writes=[rq[a]])
                        o = qo[nq % 4]
                        k.dve(stt(o[:, :], qf[a][:, :], gcol[:, 0:1], rq[a][:, :], ALU.mult, ALU.mult), reads=[qf[a], gcol, rq[a]], writes=[o])
                        k.dma("sp", qk_s.t[f, :, c0:c0 + 512], o[:, :], reads=[o], writes=[qk_s.r((f, t))], semres=o)
                    else:
                        sc = 1.0 if f == 6 else 128.0 ** -0.5
                        bcol = b1col[:, 6:7] if f == 6 else b1k[:, 0:1]
                        bres = b1col if f == 6 else b1k
                        k.act(actf(qf[a][:, :], t1[a][:, :], AF.Identity, bias=bcol, scale=sc), reads=[t1[a], bres], writes=[qf[a]])
                        k.act(actf(qfb[a][:, :], t1[a][:, :], AF.Identity, bias=bcol, scale=sc), reads=[t1[a], bres], writes=[qfb[a]])
                        pn = psA[4 + a]
                        k.pe(mm(pn[:, :], rmb[:, :], qfb[a][:, :], True, True), reads=[qfb[a], rmb], writes=[pn])
                        k.dve(tt(ra[a][:, :], qf[a][:, :], cs[t % 2][:, :], ALU.mult), reads=[qf[a], cs[t % 2]], writes=[ra[a]])
                        k.dve(tt(rq[a][:, :], pn[:, :], sn[t % 2][:, :], ALU.mult), reads=[pn, sn[t % 2]], writes=[rq[a]])
                        o = qo[nq % 4]
                        k.pool(tt(o[:, :], ra[a][:, :], rq[a][:, :], ALU.add), reads=[ra[a], rq[a]], writes=[o])
                        k.dma("sp", qk_s.t[f, :, c0:c0 + 512], o[:, :], reads=[o], writes=[qk_s.r((f, t))], semres=o)
                for s4 in range(4):
                    v = vo[nv % 2]
                    nv += 1
                    for j in range(2):
                        pb = psA[6 + j]
                        for kc in range(KC):
                            k.pe(mm(pb[:, 0:448], xb[:, kc, s4 * 128:(s4 + 1) * 128], wb[:, kc, 1024 + j * 448:1024 + (j + 1) * 448],
                                    kc == 0, kc == KC - 1), reads=[wb, xb], writes=[pb])
                        k.dve(stt(v[:, j * 448:(j + 1) * 448], pb[:, 0:448], rcol[:, s4:s4 + 1], b1bc[:, j * 448:(j + 1) * 448], ALU.mult, ALU.add),
                              reads=[pb, rcol, b1bc], writes=[v.r(j)])
                    r0 = c0 + s4 * 128
                    k.dma("sp", vt_s.t[r0:r0 + 128, :], v[:, :], reads=[v.r(0), v.r(1)], writes=[vt_s.r((t, s4))], semres=v)
        k.barrier()
        if debug == "A":
            k.finish()
            return nc

        with ExitStack() as es:
            psS = [k.ps(es, f"psS{i}", [128, 512], F32) for i in range(2)]
            psO = [k.ps(es, f"psO{i}", [128, 512], F32) for i in range(2)]
            psL = [k.ps(es, f"psL{i}", [128, 512], F32) for i in range(2)]
            accO = k.sb(es, "accO", [128, S], F32)
            accL = k.sb(es, "accL", [128, S], F32)
            idx = k.sb(es, "idx", [128, 3 * 256], F32)
            negm = k.sb(es, "negm", [128, 256], F32)
            rb = k.sb(es, "rb", [128, 96], F32)
            k.dma("sp", idx[:, :], idx_d[:, :], writes=[idx], semres=idx)
            k.dma("sp", negm[:, :], neg_d[:, :], writes=[negm], semres=negm)
            k.dma("sp", rb[:, :], rb_d.t[0:1, :].partition_broadcast(128), writes=[rb], semres=rb)
            mbs = [k.sb(es, f"mb{g}", [128, 256], F32) for g in range(3)]
            tmpm = k.sb(es, "tmpm", [128, 256], F32)
            for g in range(3):
                k.dve(cp(mbs[g][:, :], negm[:, :]), reads=[negm], writes=[mbs[g]])
                for b in range(32):
                    k.dve(ts(tmpm[:, :], idx[:, g * 256:(g + 1) * 256], float(b), ALU.is_equal, rb[:, g * 32 + b:g * 32 + b + 1], ALU.mult),
                          reads=[idx, rb], writes=[tmpm])
                    k.dve(tt(mbs[g][:, :], mbs[g][:, :], tmpm[:, :], ALU.add), reads=[tmpm, mbs[g]], writes=[mbs[g]])
            qm = [k.sb(es, f"qm{i}", [128, 2048], BF16) for i in range(2)]
            km = [k.sb(es, f"km{i}", [128, 2048], BF16) for i in range(3)]
            vm = [k.sb(es, f"vm{i}", [128, 16, 128], BF16) for i in range(3)]
            sadd = [k.sb(es, f"sadd{i}", [128, 256], F32) for i in range(2)]
            pT = [k.sb(es, f"pT{i}", [128, 256], BF16) for i in range(3)]
            nld = 0
            nblk = 0
            nbatch = 0
            for g in range(3):
                d = DILS[g]
                nsb = 16 // d
                for m in range(4):
                    q = qm[nld % 2]
                    kk = km[nld % 3]
                    vv = vm[nld % 3]
                    kprev = km[(nld - 1) % 3]
                    vprev = vm[(nld - 1) % 3]
                    nld += 1
                    t0 = m * 2048
                    tiles = [(2 * g, tq) for tq in range(4 * m, 4 * m + 4)]
                    k.dma("sp", q[:, :], qk_s.t[2 * g, :, t0:t0 + 2048], reads=[qk_s.r((2 * g, tq)) for tq in range(4 * m, 4 * m + 4)],
                          writes=[q], semres=q)
                    k.dma("sp", kk[:, :], qk_s.t[2 * g + 1, :, t0:t0 + 2048],
                          reads=[qk_s.r((2 * g + 1, tq)) for tq in range(4 * m, 4 * m + 4)], writes=[kk], semres=kk)
                    vrd = [vt_s.r((tq, s4)) for tq in range(4 * m, 4 * m + 4) for s4 in range(4)]
                    if d == 1:
                        k.dma("sp", vv[:, :, :], vt_s.t[t0:t0 + 2048, g * 128:(g + 1) * 128].rearrange("(n c) e -> c n e", c=128),
                              reads=vrd, writes=[vv], semres=vv)
                    else:
                        for nl_ in range(nsb):
                            ta = t0 + nl_ * 128 * d
                            k.dma("sp", vv[:, nl_ * d:(nl_ + 1) * d, :],
                                  vt_s.t[ta:ta + 128 * d, g * 128:(g + 1) * 128].rearrange("(c r) e -> c r e", r=d),
                                  reads=vrd, writes=[vv], semres=vv)
                    for jb in range(4):
                        bo = psO[nbatch % 2]
                        bl = psL[nbatch % 2]
                        nbatch += 1
                        for ji in range(4):
                            j = jb * 4 + ji
                            nl, r = j // d, j % d
                            first = (m == 0 and nl == 0)
                            dsl = lambda st_: slice(st_, st_ + 127 * d + 1, d)
                            cols = dsl(nl * 128 * d + r)
                            if nl > 0:
                                pk, pv = kk, vv
                                pcols = dsl((nl - 1) * 128 * d + r)
                                pj = (nl - 1) * d + r
                            else:
                                pk, pv = kprev, vprev
                                pcols = dsl((nsb - 1) * 128 * d + r)
                                pj = (nsb - 1) * d + r
                            ps = psS[nblk % 2]
                            sa = sadd[nblk % 2]
                            p = pT[nblk % 3]
                            nblk += 1
                            qa = q[:, cols]
                            if not first:
                                k.pe(mm(ps[:, 0:128], pk[:, pcols], qa, True, True), reads=[pk, q], writes=[ps])
                            k.pe(mm(ps[:, 128:256], kk[:, cols], qa, True, True), reads=[kk, q], writes=[ps])
                            lo = 128 if first else 0
                            k.dve(tt(sa[:, lo:256], ps[:, lo:256], mbs[g][:, lo:256], ALU.add), reads=[ps, mbs[g]], writes=[sa])
                            k.act(actf(p[:, lo:256], sa[:, lo:256], AF.Exp), reads=[sa], writes=[p])
                            osl = bo[:, ji * 128:(ji + 1) * 128]
                            lsl = bl[:, ji * 128:(ji + 1) * 128]
                            if not first:
                                k.pe(mm(osl, pv[:, pj, :], p[:, 0:128], True, False), reads=[pv, p], writes=[bo])
                                k.pe(mm(lsl, ones_b[:, :], p[:, 0:128], True, False), reads=[ones_b, p], writes=[bl])
                            k.pe(mm(osl, vv[:, j, :], p[:, 128:256], first, True), reads=[vv, p], writes=[bo])
                            k.pe(mm(lsl, ones_b[:, :], p[:, 128:256], first, True), reads=[ones_b, p], writes=[bl])
                        j0 = jb * 4
                        nl0, r0 = j0 // d, j0 % d
                        if d == 1:
                            dst = slice(t0 + j0 * 128, t0 + j0 * 128 + 512)
                            dO = accO[:, dst]
                            dL = accL[:, dst]
                            sO = bo[:, :]
                            sL = bl[:, :]
                        else:
                            base = t0 + nl0 * 128 * d
                            dO = accO[:, base:base + 128 * d].rearrange("p (a r) -> p r a", r=d)[:, r0:r0 + 4, :]
                            dL = accL[:, base:base + 128 * d].rearrange("p (a r) -> p r a", r=d)[:, r0:r0 + 4, :]
                            sO = bo[:, :].rearrange("p (r a) -> p r a", a=128)
                            sL = bl[:, :].rearrange("p (r a) -> p r a", a=128)
                        if g == 0:
                            k.dve(cp(dO, sO), reads=[bo], writes=[accO.r(m)])
                            k.act(lambda e, dL=dL, sL=sL: e.copy(out=dL, in_=sL), reads=[bl], writes=[accL.r(m)])
                        else:
                            k.dve(tt(dO, sO, dO, ALU.add), reads=[bo, accO.r(m)], writes=[accO.r(m)])
                            k.dve(tt(dL, sL, dL, ALU.add), reads=[bl, accL.r(m)], writes=[accL.r(m)])
            yo = [k.sb(es, f"yo{i}", [128, 2048], BF16) for i in range(2)]
            for m in range(4):
                sl = slice(m * 2048, (m + 1) * 2048)
                k.dve(recip(accL[:, sl], accL[:, sl]), reads=[accL.r(m)], writes=[accL.r(m)])
                k.dve(tt(yo[m % 2][:, :], accO[:, sl], accL[:, sl], ALU.mult), reads=[accO.r(m), accL.r(m)], writes=[yo[m % 2]])
                k.dma("sp", yT_d.t[0:128, sl], yo[m % 2][:, :], reads=[yo[m % 2]], writes=[yT_d.r(("a", m))], semres=yo[m % 2])
        k.barrier()
        if debug == "B":
            k.finish()
            return nc

        with ExitStack() as es:
            psAT = [k.ps(es, f"psAT{i}", [128, 512], F32) for i in range(2)]
            psR = [k.ps(es, f"psR{i}", [128, 512], F32) for i in range(2)]
            psU = k.ps(es, "psU", [128, 512], F32)
            psK = k.ps(es, "psK", [128, 1024], BF16)
            psY = [k.ps(es, f"psY{i}", [128, 1024], BF16) for i in range(2)]
            dmat = k.sb(es, "dmat", [128, 128], F32)
            xib = k.sb(es, "xib", [128, 128], F32)
            zeta = k.sb(es, "zeta", [128, 1], F32)
            gch = k.sb(es, "gch", [128, 1], F32)
            gng = k.sb(es, "gng", [128, 256], F32)
            epsg = k.sb(es, "epsg", [128, 1], F32)
            k.dma("sp", dmat[:, :], dmat_d[:, :], writes=[dmat], semres=dmat)
            k.dma("sp", xib[:, :], xi_d[:, :], writes=[xib], semres=xib)
            k.dma("sp", zeta[:, :], zeta_d[:, :], writes=[zeta], semres=zeta)
            k.dma("sp", gch[:, :], gch_d[:, :], writes=[gch], semres=gch)
            k.dma("sp", gng[:, :], gng_d.t[0:1, :].partition_broadcast(128), writes=[gng], semres=gng)
            k.dve(lambda e: e.memset(epsg[:, :], GN_EPS), writes=[epsg])
            qm = [k.sb(es, f"rqm{i}", [128, 2048], BF16) for i in range(2)]
            km = [k.sb(es, f"rkm{i}", [128, 2048], BF16) for i in range(2)]
            vm = [k.sb(es, f"rvm{i}", [128, 16, 256], BF16) for i in range(2)]
            gm = [k.sb(es, f"rgm{i}", [128, 16, 256], BF16) for i in range(2)]
            St = k.sb(es, "St", [128, 256], F32)
            Sb = k.sb(es, "Sb", [128, 256], BF16)
            atd = [k.sb(es, f"atd{i}", [128, 128], BF16) for i in range(2)]
            qx = [k.sb(es, f"qx{i}", [128, 128], BF16) for i in range(2)]
            kz = [k.sb(es, f"kz{i}", [128, 128], BF16) for i in range(2)]
            s1 = [k.sb(es, f"s1_{i}", [128, 1], F32) for i in range(2)]
            ssq = [k.sb(es, f"ssq_{i}", [128, 1], F32) for i in range(2)]
            junk = [k.sb(es, f"junk{i}", [128, 256], F32) for i in range(2)]
            yn = [k.sb(es, f"yn{i}", [128, 256], F32) for i in range(2)]
            sg = [k.sb(es, f"sg{i}", [128, 256], F32) for i in range(2)]
            yb = [k.sb(es, f"yb{i}", [128, 256], BF16) for i in range(2)]
            ybT = [k.sb(es, f"ybT{i}", [128, 2, 2048], BF16) for i in range(2)]
            k.dve(lambda e: e.memset(St[:, :], 0.0), writes=[St])
            for m in range(4):
                t0 = m * 2048
                q, kk, vv, gg_ = qm[m % 2], km[m % 2], vm[m % 2], gm[m % 2]
                k.dma("sp", q[:, :], qk_s.t[6, :, t0:t0 + 2048], reads=[qk_s.r((6, tq)) for tq in range(4 * m, 4 * m + 4)], writes=[q], semres=q)
                k.dma("sp", kk[:, :], qk_s.t[7, :, t0:t0 + 2048], reads=[qk_s.r((7, tq)) for tq in range(4 * m, 4 * m + 4)], writes=[kk], semres=kk)
                vtr = [vt_s.r((tq, s4)) for tq in range(4 * m, 4 * m + 4) for s4 in range(4)]
                k.dma("sp", vv[:, :, :], vt_s.t[t0:t0 + 2048, 384:640].rearrange("(n c) e -> c n e", c=128), reads=vtr, writes=[vv], semres=vv)
                k.dma("sp", gg_[:, :, :], vt_s.t[t0:t0 + 2048, 640:896].rearrange("(n c) e -> c n e", c=128), reads=vtr, writes=[gg_], semres=gg_)
                yT = ybT[m % 2]
                for n in range(16):
                    gn = m * 16 + n
                    a = gn % 2
                    cols = slice(n * 128, (n + 1) * 128)
                    k.pe(mm(psAT[a][:, 0:128], kk[:, cols], q[:, cols], True, True), reads=[kk, q], writes=[psAT[a]])
                    k.dve(tt(atd[a][:, :], psAT[a][:, 0:128], dmat[:, :], ALU.mult), reads=[psAT[a], dmat], writes=[atd[a]])
                    k.pool(tt(qx[a][:, :], q[:, cols], xib[:, :], ALU.mult), reads=[q, xib], writes=[qx[a]])
                    k.pe(mm(psR[a][:, 0:256], atd[a][:, :], vv[:, n, :], True, gn == 0), reads=[atd[a], vv], writes=[psR[a]])
                    if gn > 0:
                        k.pe(mm(psR[a][:, 0:256], qx[a][:, :], Sb[:, :], False, True), reads=[qx[a], Sb], writes=[psR[a]])
                    k.pe(lambda e, a=a, cols=cols, kk=kk: e.transpose(psK[:, 0:128], kk[:, cols], idb[:, :]), reads=[kk, idb], writes=[psK])
                    k.act(actf(kz[a][:, :], psK[:, 0:128], AF.Copy, scale=zeta[:, 0:1]), reads=[psK, zeta], writes=[kz[a]])
                    k.pe(mm(psU[:, 0:256], kz[a][:, :], vv[:, n, :], True, True), reads=[kz[a], vv], writes=[psU])
                    k.dve(stt(St[:, :], St[:, :], gch[:, 0:1], psU[:, 0:256], ALU.mult, ALU.add), reads=[St, gch, psU], writes=[St])
                    k.act(lambda e: e.copy(out=Sb[:, :], in_=St[:, :]), reads=[St], writes=[Sb])
                    k.dve(lambda e, a=a: e.tensor_reduce(out=s1[a][:, :], in_=psR[a][:, 0:256], op=ALU.add, axis=AX.X), reads=[psR[a]], writes=[s1[a]])
                    k.dve(ts(s1[a][:, :], s1[a][:, :], -1.0 / 256, ALU.mult), reads=[s1[a]], writes=[s1[a]])
                    k.act(actf(junk[a][:, :], psR[a][:, 0:256], AF.Square, bias=s1[a][:, 0:1], accum_out=ssq[a][:, :]), reads=[psR[a], s1[a]], writes=[junk[a], ssq[a]])
                    k.act(actf(ssq[a][:, :], ssq[a][:, :], AF.Sqrt, bias=epsg[:, :], scale=1.0 / 256), reads=[ssq[a], epsg], writes=[ssq[a]])
                    k.dve(recip(ssq[a][:, :], ssq[a][:, :]), reads=[ssq[a]], writes=[ssq[a]])
                    k.dve(ts(yn[a][:, :], psR[a][:, 0:256], s1[a][:, 0:1], ALU.add, ssq[a][:, 0:1], ALU.mult), reads=[psR[a], s1[a], ssq[a]], writes=[yn[a]])
                    k.act(actf(sg[a][:, :], gg_[:, n, :], AF.Silu), reads=[gg_], writes=[sg[a]])
                    k.pool(tt(yn[a][:, :], yn[a][:, :], gng[:, :], ALU.mult), reads=[yn[a], gng], writes=[yn[a]])
                    k.pool(tt(yb[a][:, :], yn[a][:, :], sg[a][:, :], ALU.mult), reads=[yn[a], sg[a]], writes=[yb[a]])
                    for h in range(2):
                        k.pe(lambda e, a=a, h=h: e.transpose(psY[a][:, h * 128:(h + 1) * 128], yb[a][:, h * 128:(h + 1) * 128], idb[:, :]),
                             reads=[yb[a], idb], writes=[psY[a]])
                    k.dve(cp(yT[:, :, cols], psY[a][:, 0:256].rearrange("p (h t) -> p h t", h=2)), reads=[psY[a]], writes=[yT])
                for h in range(2):
                    k.dma("sp", yT_d.t[128 + h * 128:256 + h * 128, t0:t0 + 2048], yT[:, h, :], reads=[yT], writes=[yT_d.r(("b", m, h))], semres=yT)
        k.finish()
    return nc


def _t5_bucket(dist):
    nb, md = 32, 2048
    max_exact = nb // 2
    safe = np.maximum(dist, 1).astype(np.float32)
    large = max_exact + (np.log(safe / max_exact) / np.log(md / max_exact) * (nb - max_exact)).astype(np.int32)
    return np.where(dist < max_exact, dist, np.minimum(large, nb - 1)).astype(np.int32)


def static_tables():
    cc = np.arange(128)[:, None]
    aa = np.arange(128)[None, :]
    idx = np.zeros((128, 3, 2, 128), np.float32)
    neg = np.zeros((128, 2, 128), np.float32)
    for g, d in enumerate(DILS):
        d_prev = 128 + aa - cc
        d_cur = aa - cc
        v_prev = d_prev <= 128
        v_cur = d_cur >= 0
        idx[:, g, 0, :] = np.where(v_prev, _t5_bucket(np.maximum(d_prev, 0) * d), -1)
        idx[:, g, 1, :] = np.where(v_cur, _t5_bucket(np.maximum(d_cur, 0) * d), -1)
        neg[:, 0, :] = np.where(v_prev, 0.0, NEG)
        neg[:, 1, :] = np.where(v_cur, 0.0, NEG)
    inv = (10000.0 ** (-np.arange(0, 128, 2, dtype=np.float32) / np.float32(128))).astype(np.float32)
    ang = (np.arange(S, dtype=np.float32)[:, None] * inv[None, :]).astype(np.float32)
    cosT = np.concatenate([np.cos(ang).T, np.cos(ang).T], axis=0).astype(np.float32)
    sinT = np.concatenate([np.sin(ang).T, np.sin(ang).T], axis=0).astype(np.float32)
    rm = np.zeros((128, 128), np.float32)
    for m in range(64):
        rm[m + 64, m] = -1.0
        rm[m, m + 64] = 1.0
    ident = np.eye(128, dtype=np.float32)
    ret = []
    ii = np.arange(128, dtype=np.float32)
    for h in range(8):
        log_g = np.log1p(-np.exp2(np.float32(-5.0 - h))).astype(np.float32)
        diff = ii[None, :] - ii[:, None]
        dmatT = np.where(diff >= 0, np.exp(log_g * np.maximum(diff, 0.0)), 0.0).astype(np.float32)
        xi = np.exp(log_g * (ii + 1.0)).astype(np.float32)
        zeta = np.exp(log_g * (127.0 - ii)).astype(np.float32)
        gchunk = np.float32(np.exp(log_g * 128.0))
        ret.append(dict(dmatT=dmatT, xibc=np.tile(xi[None, :], (128, 1)).astype(np.float32),
                        zetacol=zeta[:, None].copy(), gchunk=np.full((128, 1), gchunk, np.float32)))
    zpow = np.zeros((128, 8, 56), np.float32)
    for h in range(8):
        log_g = np.log1p(-np.exp2(np.float32(-5.0 - h))).astype(np.float64)
        zeta64 = np.exp(log_g * (127.0 - ii.astype(np.float64)))
        for n in range(56):
            zpow[:, h, n] = (zeta64 * np.exp(log_g * 128.0 * (55 - n))).astype(np.float32)
    return dict(idxT=idx.reshape(128, 768), negT=neg.reshape(128, 256), cosT=cosT, sinT=sinT, rmat=rm, identf=ident, ret=ret,
                zpow=np.ascontiguousarray(zpow.reshape(128, 448)))


def pk16(v):
    return np.ascontiguousarray(np.asarray(v, np.float32).reshape(-1, 128).T)


def l1_inputs(inp, st):
    x = np.asarray(inp["x"], np.float32)[0]
    xT = np.ascontiguousarray(x.T)
    w_in = np.asarray(inp["w_in"], np.float32)[0]
    w_ada = np.asarray(inp["w_ada"], np.float32)[0]
    b_ada = np.asarray(inp["b_ada"], np.float32)[0]
    rel_bias = np.asarray(inp["rel_bias"], np.float32)
    wada1 = np.ascontiguousarray(w_ada[:, 0:4096])
    bada1 = pk16(b_ada[0:4096])
    maps = []
    for c in range(NCORES):
        cols = []
        for g in range(3):
            h = g * 8 + c
            cols += [np.arange(h * 128, (h + 1) * 128), 3072 + np.arange(h * 128, (h + 1) * 128)]
        cols += [9216 + np.arange(c * 128, (c + 1) * 128), 10240 + np.arange(c * 128, (c + 1) * 128)]
        for g in range(3):
            h = g * 8 + c
            cols += [6144 + np.arange(h * 128, (h + 1) * 128)]
        cols += [11264 + np.arange(c * 256, (c + 1) * 256), 13312 + np.arange(c * 256, (c + 1) * 256)]
        cols = np.concatenate(cols)
        rt = st["ret"][c]
        maps.append(dict(
            xT=xT, c_pk=pk16(inp["c"][0]), wada1=wada1, bada1=bada1, ln1_pk=pk16(inp["ln1_g"][0]),
            win=np.ascontiguousarray(w_in[:, cols]),
            qg=np.asarray(inp["q_norm_g"], np.float32)[0][:, None].copy(),
            kg=np.asarray(inp["k_norm_g"], np.float32)[0][:, None].copy(),
            rb=np.ascontiguousarray(np.stack([rel_bias[:, g * 8 + c] for g in range(3)]).reshape(1, 96)),
            idxT=st["idxT"], negT=st["negT"], cosT=st["cosT"], sinT=st["sinT"], rmat=st["rmat"], identf=st["identf"],
            dmatT=rt["dmatT"], xibc=rt["xibc"], zetacol=rt["zetacol"], gchunk=rt["gchunk"],
            gng=np.asarray(inp["ret_gn_g"], np.float32)[0][c * 256:(c + 1) * 256][None, :].copy(),
        ))
    return maps


def build_l2(n_exp=65, stop=None, nc=None, k=None, shared=None):
    if nc is None:
        nc = bass.Bass("TRN2", target_bir_lowering=False)
        k = K(nc)
    shared = shared or {}
    EI, EO = "ExternalInput", "ExternalOutput"
    TS = S // NCORES
    xT_d = k.dram("xTs", [D, TS], F32, EI)
    x_d = k.dram("xs", [TS, D], F32, EI)
    c_d = shared.get("c_pk") or k.dram("c_pk", [128, KC], F32, EI)
    wada_d = shared.get("wada") or k.dram("wada", [D, 12288], F32, EI)
    bada1_d = shared.get("bada1") or k.dram("bada1", [128, 32], F32, EI)
    badar_d = k.dram("badar", [1, 8192], F32, EI)
    ln1_d = shared.get("ln1_pk") or k.dram("ln1_pk", [128, KC], F32, EI)
    ln2_d = k.dram("ln2_row", [1, D], F32, EI)
    wg_d = k.dram("wgates", [D, 4096], F32, EI)
    pa_d = k.dram("pa", [1024, D], F32, EI)
    pb_d = k.dram("pb", [D, D], F32, EI)
    wo_d = k.dram("wo", [D, D], F32, EI)
    yT_d = shared.get("ys") or k.dram("yTs", [3072, TS], BF16, EI)
    rw_d = k.dram("rw", [D, 64], F32, EI)
    rbias_d = k.dram("rbias", [1, 64], F32, EI)
    if n_exp > 0:
        wge_d = k.dram("wge", [64, D, 512], F32, EI)
        wue_d = k.dram("wue", [64, D, 512], F32, EI)
        wde_d = k.dram("wde", [64, 512, D], F32, EI)
        wgs_d = k.dram("wgs", [D, 512], F32, EI)
        wus_d = k.dram("wus", [D, 512], F32, EI)
        wds_d = k.dram("wds", [512, D], F32, EI)
    idf_d = shared.get("identf") or k.dram("identf", [128, 128], F32, EI)
    out_d = k.dram("out", [TS, D], F32, EO)

    def sbr(name, shape, dt):
        k.nm += 1
        t = nc.alloc_sbuf_tensor(f"r{k.nm}_{name}", list(shape), dt, side="right")
        return Tl(k, t, name)

    with ExitStack() as es0:
        ones_b = k.sb(es0, "ones_b", [128, 128], BF16)
        idf = k.sb(es0, "idf", [128, 128], F32)
        epsr = k.sb(es0, "epsr", [128, 1], F32)
        k.dve(lambda e: e.memset(ones_b[:, :], 1.0), writes=[ones_b])
        k.dve(lambda e: e.memset(epsr[:, :], RMS_EPS), writes=[epsr])
        k.dma("sp", idf[:, :], idf_d[:, :], writes=[idf], semres=idf)
        g2bc = sbr("g2bc", [128, D], F32)

        with ExitStack() as esm:
            g1bc = k.sb(esm, "g1bc", [128, D], F32)
            gg2bc = k.sb(esm, "gg2bc", [128, D], F32)
            sh2bc = k.sb(esm, "sh2bc", [128, D], F32)
            gg = k.sb(esm, "gg", [128, KC], F32)
            sh1b = k.sb(esm, "sh1b", [128, KC], BF16)
            rstd = k.sb(esm, "rstd", [128, TS], F32)
            with ExitStack() as es:
                psA = [k.ps(es, f"psA{i}", [128, 512], F32) for i in range(4)]
                cb = emit_silu_c(k, es, c_d)
                mod1 = emit_mod_cols(k, es, cb, wada_d, bada1_d, 32, "mod1", psA[0])
                ln1 = k.sb(es, "ln1", [128, KC], F32)
                k.dma("sp", ln1[:, :], ln1_d[:, :], writes=[ln1], semres=ln1)
                k.dve(stt(gg[:, :], mod1[:, 16:32], 1.0, ln1[:, :], ALU.add, ALU.mult), reads=[mod1, ln1], writes=[gg])
                k.dve(cp(sh1b[:, :], mod1[:, 0:16]), reads=[mod1], writes=[sh1b])
                cbc = k.sb(es, "cbc", [128, KC, 128], BF16)
                k.dve(cp(cbc[:, :, :], cb[:, :].unsqueeze(2).to_broadcast([128, KC, 128])), reads=[cb], writes=[cbc])
                wch = [k.sb(es, f"wch{i}", [128, KC, 512], BF16) for i in range(2)]
                bch = [k.sb(es, f"bch{i}", [128, 512], F32) for i in range(2)]
                ln2bc = k.sb(es, "ln2bc", [128, D], F32)
                k.dma("sp", ln2bc[:, :], ln2_d.t[0:1, :].partition_broadcast(128), writes=[ln2bc], semres=ln2bc)
                wv = wada_d.t.rearrange("(kc p) n -> p kc n", p=128)
                dsts = [g1bc, sh2bc, gg2bc, g2bc]
                for ch in range(16):
                    w = wch[ch % 2]
                    bb = bch[ch % 2]
                    c0 = 4096 + ch * 512
                    for h in range(2):
                        k.dma("pool", w[:, h * 8:(h + 1) * 8, :], wv[:, h * 8:(h + 1) * 8, c0:c0 + 512], writes=[w], semres=w)
                    k.dma("sp", bb[:, :], badar_d.t[0:1, ch * 512:(ch + 1) * 512].partition_broadcast(128), writes=[bb], semres=bb)
                    pb_ = psA[1 + ch % 2]
                    for kc in range(KC):
                        k.pe(mm(pb_[:, :], cbc[:, kc, :], w[:, kc, :], kc == 0, kc == KC - 1), reads=[cbc, w], writes=[pb_])
                    dst = dsts[ch // 4]
                    k.dve(tt(dst[:, (ch % 4) * 512:(ch % 4 + 1) * 512], pb_[:, :], bb[:, :], ALU.add), reads=[pb_, bb], writes=[dst])
                k.dve(stt(gg2bc[:, :], gg2bc[:, :], 1.0, ln2bc[:, :], ALU.add, ALU.mult), reads=[gg2bc, ln2bc], writes=[gg2bc])
                k.barrier()
                if stop == "a":
                    k.finish()
                    return nc

            with ExitStack() as esg:
                mergedT = k.sb(esg, "mergedT", [128, KC, TS], BF16)
                with ExitStack() as es:
                    psG = [k.ps(es, f"psG{i}", [128, 512], F32) for i in range(4)]
                    psY = [k.ps(es, f"psYp{i}", [128, 512], F32) for i in range(2)]
                    psB = k.ps(es, "psB", [128, 512], F32)
                    psN = k.ps(es, "psN", [128, 512], F32)
                    xg = k.sb(es, "xg", [128, KC, TS], BF16)
                    yT = k.sb(es, "yT", [128, 24, TS], BF16)
                    sq = k.sb(es, "sq", [128, 8, 512], BF16)
                    xv = xT_d.t.rearrange("(kc p) s -> p kc s", p=128)
                    for h in range(4):
                        k.dma("pool", xg[:, h * 4:(h + 1) * 4, :], xv[:, h * 4:(h + 1) * 4, :], writes=[xg], semres=xg)
                    for h in range(3):
                        k.dma("sp", yT[:, h * 8:(h + 1) * 8, :], yT_d.t[h * 1024:(h + 1) * 1024, :].rearrange("(kc p) s -> p kc s", p=128),
                              writes=[yT], semres=yT)
                    for th in range(2):
                        for h in range(2):
                            k.act(actf(sq[:, :, :], xg[:, h * 8:(h + 1) * 8, th * 512:(th + 1) * 512], AF.Square), reads=[xg], writes=[sq])
                            for kc in range(8):
                                k.pe(mm(psN[:, :], ones_b[:, :], sq[:, kc, :], h == 0 and kc == 0, h == 1 and kc == 7), reads=[sq, ones_b], writes=[psN])
                        k.act(actf(rstd[:, th * 512:(th + 1) * 512], psN[:, :], AF.Sqrt, bias=epsr[:, :], scale=1.0 / D), reads=[psN, epsr], writes=[rstd])
                    k.dve(recip(rstd[:, :], rstd[:, :]), reads=[rstd], writes=[rstd])
                    for kc in range(KC):
                        k.dve(ts(xg[:, kc, :], xg[:, kc, :], gg[:, kc:kc + 1], ALU.mult), reads=[xg, gg], writes=[xg])
                    wga = [k.sb(es, f"wga{i}", [128, KC, 256], BF16) for i in range(1)]
                    wgb = [k.sb(es, f"wgb{i}", [128, KC, 256], BF16) for i in range(1)]
                    wpa = [k.sb(es, f"wpa{i}", [128, 8, 256], BF16) for i in range(1)]
                    wpb = [k.sb(es, f"wpb{i}", [128, KC, 256], BF16) for i in range(1)]
                    b1g = k.sb(es, "b1g", [128, 32], F32)
                    ta = [k.sb(es, f"ta{i}", [128, 512], F32) for i in range(2)]
                    tb = [k.sb(es, f"tb{i}", [128, 512], F32) for i in range(2)]
                    wgv = wg_d.t.rearrange("(kc p) n -> p kc n", p=128)
                    pav = pa_d.t.rearrange("(kc p) n -> p kc n", p=128)
                    pbv = pb_d.t.rearrange("(kc p) n -> p kc n", p=128)
                    ncnt = 0
                    for G in range(8):
                        i2 = 0
                        c0 = G * 256
                        k.dma("pool", wga[i2][:, :, :], wgv[:, :, c0:c0 + 256], writes=[wga[i2]], semres=wga[i2])
                        k.dma("pool", wgb[i2][:, :, :], wgv[:, :, 2048 + c0:2048 + c0 + 256], writes=[wgb[i2]], semres=wgb[i2])
                        k.dma("pool", wpa[i2][:, :, :], pav[:, :, c0:c0 + 256], writes=[wpa[i2]], semres=wpa[i2])
                        k.dma("pool", wpb[i2][:, :, :], pbv[:, :, c0:c0 + 256], writes=[wpb[i2]], semres=wpb[i2])
                        for ft in range(2):
                            f = G * 2 + ft
                            fs = slice(ft * 128, (ft + 1) * 128)
                            for kc in range(KC):
                                k.pe(mm(psB[:, 2 * f:2 * f + 1], wga[i2][:, kc, fs], sh1b[:, kc:kc + 1], kc == 0, kc == KC - 1), reads=[wga[i2], sh1b], writes=[psB])
                            for kc in range(KC):
                                k.pe(mm(psB[:, 2 * f + 1:2 * f + 2], wgb[i2][:, kc, fs], sh1b[:, kc:kc + 1], kc == 0, kc == KC - 1), reads=[wgb[i2], sh1b], writes=[psB])
                            k.dve(cp(b1g[:, 2 * f:2 * f + 2], psB[:, 2 * f:2 * f + 2]), reads=[psB], writes=[b1g])
                            for th in range(2):
                                tsl = slice(th * 512, (th + 1) * 512)
                                a = ncnt % 2
                                ncnt += 1
                                pga, pgb = psG[2 * a], psG[2 * a + 1]
                                for kc in range(KC):
                                    k.pe(mm(pga[:, :], wga[i2][:, kc, fs], xg[:, kc, tsl], kc == 0, kc == KC - 1), reads=[wga[i2], xg], writes=[pga])
                                for kc in range(KC):
                                    k.pe(mm(pgb[:, :], wgb[i2][:, kc, fs], xg[:, kc, tsl], kc == 0, kc == KC - 1), reads=[wgb[i2], xg], writes=[pgb])
                                k.dve(tt(ta[a][:, :], pga[:, :], rstd[:, tsl], ALU.mult), reads=[pga, rstd], writes=[ta[a]])
                                k.dve(tt(tb[a][:, :], pgb[:, :], rstd[:, tsl], ALU.mult), reads=[pgb, rstd], writes=[tb[a]])
                                k.act(actf(ta[a][:, :], ta[a][:, :], AF.Sigmoid, bias=b1g[:, 2 * f:2 * f + 1]), reads=[ta[a], b1g], writes=[ta[a]])
                                k.act(actf(tb[a][:, :], tb[a][:, :], AF.Sigmoid, bias=b1g[:, 2 * f + 1:2 * f + 2]), reads=[tb[a], b1g], writes=[tb[a]])
                                for kc in range(8):
                                    k.pe(mm(psY[0][:, :], wpa[i2][:, kc, fs], yT[:, kc, tsl], kc == 0, kc == 7), reads=[wpa[i2], yT], writes=[psY[0]])
                                k.dve(tt(ta[a][:, :], ta[a][:, :], psY[0][:, :], ALU.mult), reads=[ta[a], psY[0]], writes=[ta[a]])
                                for kc in range(KC):
                                    k.pe(mm(psY[1][:, :], wpb[i2][:, kc, fs], yT[:, 8 + kc, tsl], kc == 0, kc == KC - 1), reads=[wpb[i2], yT], writes=[psY[1]])
                                k.dve(tt(tb[a][:, :], tb[a][:, :], psY[1][:, :], ALU.mult), reads=[tb[a], psY[1]], writes=[tb[a]])
                                k.pool(tt(mergedT[:, f, tsl], ta[a][:, :], tb[a][:, :], ALU.add), reads=[ta[a], tb[a]], writes=[mergedT.r(th)])
                    k.barrier()
                    if stop == "b":
                        k.finish()
                        return nc

                acc = sbr("acc", [128, 8, D], F32)
                with ExitStack() as es:
                    psO = [k.ps(es, f"psO{i}", [128, 512], F32) for i in range(2)]
                    wo = [k.sb(es, f"wo{i}", [128, KC, 512], BF16) for i in range(2)]
                    tmp = [k.sb(es, f"tmpo{i}", [128, 512], F32) for i in range(2)]
                    wov = wo_d.t.rearrange("(kc p) n -> p kc n", p=128)
                    xv2 = x_d.t.rearrange("(t p) n -> p t n", p=128)
                    for tt_ in range(8):
                        k.dma("sp", acc[:, tt_, :], xv2[:, tt_, :], writes=[acc.r(tt_)], semres=acc.r(tt_))
                    no = 0
                    for n in range(4):
                        w = wo[n % 2]
                        for h in range(2):
                            k.dma("pool", w[:, h * 8:(h + 1) * 8, :], wov[:, h * 8:(h + 1) * 8, n * 512:(n + 1) * 512], writes=[w], semres=w)
                        for tt_ in range(8):
                            a = no % 2
                            no += 1
                            for kc in range(KC):
                                k.pe(mm(psO[a][:, :], mergedT[:, kc, tt_ * 128:(tt_ + 1) * 128], w[:, kc, :], kc == 0, kc == KC - 1),
                                     reads=[mergedT.r(tt_ // 4), w], writes=[psO[a]])
                            k.dve(tt(tmp[a][:, :], psO[a][:, :], g1bc[:, n * 512:(n + 1) * 512], ALU.mult), reads=[psO[a], g1bc], writes=[tmp[a]])
                            k.pool(tt(acc[:, tt_, n * 512:(n + 1) * 512], acc[:, tt_, n * 512:(n + 1) * 512], tmp[a][:, :], ALU.add),
                                   reads=[tmp[a], acc.r(tt_)], writes=[acc.r(tt_)])
                    k.barrier()
                    if stop == "c":
                        ov = out_d.t.rearrange("(t p) n -> p t n", p=128)
                        for tt_ in range(8):
                            k.dma("sp", ov[:, tt_, :], acc[:, tt_, :], reads=[acc.r(tt_)], writes=[out_d.r(tt_)], semres=acc.r(tt_))
                        k.finish()
                        return nc

            h2T = sbr("h2T", [128, KC, TS], BF16)
            Wc = sbr("Wc", [128, 8, 66], F32)
            with ExitStack() as es:
                psT = [k.ps(es, f"psT{i}", [128, 512], F32) for i in range(4)]
                psR = k.ps(es, "psRt", [128, 512], F32)
                rwf = k.sb(es, "rwf", [128, KC, 64], F32)
                rbb = k.sb(es, "rbb", [128, 64], F32)
                k.dma("sp", rwf[:, :, :], rw_d.t.rearrange("(kc p) e -> p kc e", p=128), writes=[rwf], semres=rwf)
                k.dma("sp", rbb[:, :], rbias_d.t[0:1, :].partition_broadcast(128), writes=[rbb], semres=rbb)
                rwhi = k.sb(es, "rwhi", [128, KC, 64], BF16)
                rwlo = k.sb(es, "rwlo", [128, KC, 64], BF16)
                h2lo = k.sb(es, "h2lo", [128, KC, 128], BF16)
                k.dve(cp(rwhi[:, :, :], rwf[:, :, :]), reads=[rwf], writes=[rwhi])
                k.dve(tt(rwlo[:, :, :], rwf[:, :, :], rwhi[:, :, :], ALU.subtract), reads=[rwf, rwhi], writes=[rwlo])
                h2 = [k.sb(es, f"h2_{i}", [128, D], F32) for i in range(2)]
                h2Tf = [k.sb(es, f"h2Tf{i}", [128, KC, 128], F32) for i in range(2)]
                junk = k.sb(es, "junk2", [128, D], BF16)
                ss = [k.sb(es, f"ss{i}", [128, 1], F32) for i in range(2)]
                sc_ = [k.sb(es, f"sc{i}", [128, 64], F32) for i in range(2)]
                sel = [k.sb(es, f"sel{i}", [128, 64], F32) for i in range(2)]
                eq = k.sb(es, "eq", [128, 64], F32)
                sel2 = k.sb(es, "sel2", [128, 64], F32)
                m1 = k.sb(es, "m1", [128, 8], F32)
                m2 = k.sb(es, "m2", [128, 8], F32)
                gs = k.sb(es, "gs", [128, 8], F32)
                mx = k.sb(es, "mx", [128, 8], F32)
                gmask = k.sb(es, "gmask", [128, 8], F32)
                selm = k.sb(es, "selm", [128, 64], F32)
                mx2 = k.sb(es, "mx2", [128, 8], F32)
                tw = k.sb(es, "tw", [128, 64], F32)
                wsum = k.sb(es, "wsum", [128, 1], F32)
                k.dve(lambda e: e.memset(Wc[:, :, :], 1.0), writes=[Wc])
                for tt_ in range(8):
                    a = tt_ % 2
                    x2 = acc[:, tt_, :]
                    k.act(actf(junk[:, :], x2, AF.Square, accum_out=ss[a][:, :]), reads=[acc.r(tt_)], writes=[junk, ss[a]])
                    k.act(actf(ss[a][:, :], ss[a][:, :], AF.Sqrt, bias=epsr[:, :], scale=1.0 / D), reads=[ss[a], epsr], writes=[ss[a]])
                    k.dve(recip(ss[a][:, :], ss[a][:, :]), reads=[ss[a]], writes=[ss[a]])
                    k.dve(stt(h2[a][:, :], x2, ss[a][:, 0:1], gg2bc[:, :], ALU.mult, ALU.mult), reads=[acc.r(tt_), ss[a], gg2bc], writes=[h2[a]])
                    k.pool(tt(h2[a][:, :], h2[a][:, :], sh2bc[:, :], ALU.add), reads=[h2[a], sh2bc], writes=[h2[a]])
                    if stop == "d1":
                        k.barrier()
                        k.finish()
                        return nc
                    for q4 in range(4):
                        pt = psT[q4]
                        for j in range(4):
                            kc = q4 * 4 + j
                            k.pe(lambda e, pt=pt, j=j, kc=kc, a=a: e.transpose(pt[:, j * 128:(j + 1) * 128], h2[a][:, kc * 128:(kc + 1) * 128], idf[:, :]),
                                 reads=[h2[a], idf], writes=[pt])
                        k.dve(cp(h2Tf[a][:, q4 * 4:(q4 + 1) * 4, :], pt[:, :].rearrange("p (j t) -> p j t", j=4)), reads=[pt], writes=[h2Tf[a]])
                        k.dve(cp(h2T[:, q4 * 4:(q4 + 1) * 4, tt_ * 128:(tt_ + 1) * 128], pt[:, :].rearrange("p (j t) -> p j t", j=4)),
                              reads=[pt], writes=[h2T.r(tt_)])
                    if stop == "d2":
                        k.barrier()
                        k.finish()
                        return nc
                    hi = h2T[:, :, tt_ * 128:(tt_ + 1) * 128]
                    k.dve(tt(h2lo[:, :, :], h2Tf[a][:, :, :], hi, ALU.subtract), reads=[h2Tf[a], h2T.r(tt_)], writes=[h2lo])
                    nmm = 0
                    for kc in range(KC):
                        for (l_, r_, lres) in ((hi[:, kc, :], rwhi[:, kc, :], h2T.r(tt_)), (h2lo[:, kc, :], rwhi[:, kc, :], h2lo), (hi[:, kc, :], rwlo[:, kc, :], h2T.r(tt_))):
                            k.pe(mm(psR[:, 0:64], l_, r_, nmm == 0, nmm == 3 * KC - 1), reads=[lres, rwhi, rwlo], writes=[psR])
                            nmm += 1
                    k.act(actf(sc_[a][:, :], psR[:, 0:64], AF.Sigmoid), reads=[psR], writes=[sc_[a]])
                    k.dve(tt(sel[a][:, :], sc_[a][:, :], rbb[:, :], ALU.add), reads=[sc_[a], rbb], writes=[sel[a]])
                    if stop == "d3":
                        k.barrier()
                        k.finish()
                        return nc
                    def red(out_ap, in_ap, op, rd, wr):
                        k.dve(lambda e: e.tensor_reduce(out=out_ap, in_=in_ap, axis=AX.X, op=op), reads=rd, writes=wr)

                    def kth_thr(work, n, kth, thr):
                        for _ in range(kth - 1):
                            red(thr[:, 0:1], work[:, 0:n], ALU.max, [work], [thr])
                            k.dve(ts(eq[:, 0:n], work[:, 0:n], thr[:, 0:1], ALU.is_ge, -2e9, ALU.mult), reads=[work, thr], writes=[eq])
                            k.dve(tt(work[:, 0:n], work[:, 0:n], eq[:, 0:n], ALU.add), reads=[work, eq], writes=[work])
                        red(thr[:, 0:1], work[:, 0:n], ALU.max, [work], [thr])

                    for g_ in range(8):
                        gsl = slice(g_ * 8, (g_ + 1) * 8)
                        red(m1[:, g_:g_ + 1], sel[a][:, gsl], ALU.max, [sel[a]], [m1])
                        k.dve(ts(sel2[:, gsl], sel[a][:, gsl], m1[:, g_:g_ + 1], ALU.is_equal, -1e9, ALU.mult), reads=[sel[a], m1], writes=[sel2])
                        k.dve(tt(sel2[:, gsl], sel2[:, gsl], sel[a][:, gsl], ALU.add), reads=[sel2, sel[a]], writes=[sel2])
                        red(m2[:, g_:g_ + 1], sel2[:, gsl], ALU.max, [sel2], [m2])
                    k.dve(tt(gs[:, :], m1[:, :], m2[:, :], ALU.add), reads=[m1, m2], writes=[gs])
                    k.dve(cp(mx[:, :], gs[:, :]), reads=[gs], writes=[mx])
                    kth_thr(mx, 8, 4, mx2)
                    k.dve(ts(gmask[:, :], gs[:, :], mx2[:, 0:1], ALU.is_ge), reads=[gs, mx2], writes=[gmask])
                    k.dve(ts(gmask[:, :], gmask[:, :], 1e9, ALU.mult, -1e9, ALU.add), reads=[gmask], writes=[gmask])
                    for g_ in range(8):
                        gsl = slice(g_ * 8, (g_ + 1) * 8)
                        k.dve(ts(selm[:, gsl], sel[a][:, gsl], gmask[:, g_:g_ + 1], ALU.add), reads=[sel[a], gmask], writes=[selm])
                    k.dve(cp(sel2[:, :], selm[:, :]), reads=[selm], writes=[sel2])
                    kth_thr(sel2, 64, 8, mx2)
                    k.dve(ts(eq[:, :], selm[:, :], mx2[:, 0:1], ALU.is_ge), reads=[selm, mx2], writes=[eq])
                    k.dve(tt(tw[:, :], sc_[a][:, :], eq[:, :], ALU.mult), reads=[sc_[a], eq], writes=[tw])
                    k.dve(lambda e: e.tensor_reduce(out=wsum[:, :], in_=tw[:, :], axis=AX.X, op=ALU.add), reads=[tw], writes=[wsum])
                    k.dve(recip(wsum[:, :], wsum[:, :]), reads=[wsum], writes=[wsum])
                    k.dve(ts(Wc[:, tt_, 0:64], tw[:, :], wsum[:, 0:1], ALU.mult, 2.5, ALU.mult), reads=[tw, wsum], writes=[Wc])
                k.barrier()
                if stop == "d":
                    k.dve(cp(acc[:, :, 0:66], Wc[:, :, :]), reads=[Wc, acc.r(0)], writes=[acc.r(0)])
                    k.barrier()
                    ov = out_d.t.rearrange("(t p) n -> p t n", p=128)
                    for tt_ in range(8):
                        k.dma("sp", ov[:, tt_, :], acc[:, tt_, :], reads=[acc.r(tt_)], writes=[out_d.r(tt_)], semres=acc.r(tt_))
                    k.finish()
                    return nc

        with ExitStack() as es:
            psg = [k.ps(es, f"psg{i}", [128, 512], F32) for i in range(2)]
            psu = [k.ps(es, f"psu{i}", [128, 512], F32) for i in range(2)]
            psd = [k.ps(es, f"psd{i}", [128, 512], F32) for i in range(2)]
            WG = [k.sb(es, f"WG{i}", [128, KC, 512], BF16) for i in range(2)]
            WU = [k.sb(es, f"WU{i}", [128, KC, 512], BF16) for i in range(2)]
            WD = [k.sb(es, f"WD{i}", [128, 4, D], BF16) for i in range(1)]
            actT = [k.sb(es, f"actT{i}", [128, 4, 512], BF16) for i in range(2)]
            sg = [k.sb(es, f"sgm{i}", [128, 512], F32) for i in range(2)]
            ng = 0
            nd = 0
            for e in range(n_exp):
                if e < 64:
                    gsrc, usrc, dsrc = wge_d.t[e], wue_d.t[e], wde_d.t[e]
                else:
                    gsrc, usrc, dsrc = wgs_d.t, wus_d.t, wds_d.t
                wg_, wu_, wd_ = WG[e % 2], WU[e % 2], WD[0]
                gv = gsrc.rearrange("(kc p) f -> p kc f", p=128)
                uv = usrc.rearrange("(kc p) f -> p kc f", p=128)
                dv = dsrc.rearrange("(fc p) n -> p fc n", p=128)
                for h in range(2):
                    k.dma("pool", wg_[:, h * 8:(h + 1) * 8, :], gv[:, h * 8:(h + 1) * 8, :], writes=[wg_], semres=wg_)
                    k.dma("pool", wu_[:, h * 8:(h + 1) * 8, :], uv[:, h * 8:(h + 1) * 8, :], writes=[wu_], semres=wu_)
                for h in range(2):
                    k.dma("pool", wd_[:, h * 2:(h + 1) * 2, :], dv[:, h * 2:(h + 1) * 2, :], writes=[wd_], semres=wd_)
                k.dve(tt(wd_[:, :, :], wd_[:, :, :], g2bc[:, :].unsqueeze(1).to_broadcast([128, 4, D]), ALU.mult), reads=[wd_, g2bc], writes=[wd_])
                for th in range(2):
                    tsl = slice(th * 512, (th + 1) * 512)
                    at = actT[th]
                    for ft in range(4):
                        a = ng % 2
                        ng += 1
                        fs = slice(ft * 128, (ft + 1) * 128)
                        for kc in range(KC):
                            k.pe(mm(psg[a][:, :], wg_[:, kc, fs], h2T[:, kc, tsl], kc == 0, kc == KC - 1), reads=[wg_, h2T], writes=[psg[a]])
                        for kc in range(KC):
                            k.pe(mm(psu[a][:, :], wu_[:, kc, fs], h2T[:, kc, tsl], kc == 0, kc == KC - 1), reads=[wu_, h2T], writes=[psu[a]])
                        k.act(actf(sg[a][:, :], psg[a][:, :], AF.Silu), reads=[psg[a]], writes=[sg[a]])
                        k.dve(tt(at[:, ft, :], sg[a][:, :], psu[a][:, :], ALU.mult), reads=[sg[a], psu[a]], writes=[at])
                for th in range(2):
                    at = actT[th]
                    for t4 in range(4):
                        tt_ = th * 4 + t4
                        for n in range(4):
                            a = nd % 2
                            nd += 1
                            for fc in range(4):
                                k.pe(mm(psd[a][:, :], at[:, fc, t4 * 128:(t4 + 1) * 128], wd_[:, fc, n * 512:(n + 1) * 512], fc == 0, fc == 3),
                                     reads=[at, wd_], writes=[psd[a]])
                            k.dve(stt(acc[:, tt_, n * 512:(n + 1) * 512], psd[a][:, :], Wc[:, tt_, e:e + 1], acc[:, tt_, n * 512:(n + 1) * 512], ALU.mult, ALU.add),
                                  reads=[psd[a], Wc, acc.r(tt_)], writes=[acc.r(tt_)])
            ov = out_d.t.rearrange("(t p) n -> p t n", p=128)
            for tt_ in range(8):
                k.dma("sp", ov[:, tt_, :], acc[:, tt_, :], reads=[acc.r(tt_)], writes=[out_d.r(tt_)], semres=acc.r(tt_))
        k.finish()
    return nc


def l2_inputs(inp, st, yT_all):
    x = np.asarray(inp["x"], np.float32)[0]
    w_in = np.asarray(inp["w_in"], np.float32)[0]
    w_ada = np.asarray(inp["w_ada"], np.float32)[0]
    b_ada = np.asarray(inp["b_ada"], np.float32)[0]
    ya = np.concatenate([np.asarray(y)[0:128] for y in yT_all], axis=0)
    yb = np.concatenate([np.asarray(y)[128:384] for y in yT_all], axis=0)
    yfull = np.concatenate([ya, yb], axis=0)
    wgates = np.ascontiguousarray(w_in[:, 15360:19456])
    shared = dict(
        c_pk=pk16(inp["c"][0]), wada=w_ada, bada1=pk16(b_ada[0:4096]), badar=np.ascontiguousarray(b_ada[4096:12288][None, :]),
        ln1_pk=pk16(inp["ln1_g"][0]), ln2_row=np.asarray(inp["ln2_g"], np.float32)[0][None, :].copy(),
        wgates=wgates, pa=np.asarray(inp["p_a"], np.float32)[0], pb=np.asarray(inp["p_b"], np.float32)[0],
        wo=np.asarray(inp["w_o"], np.float32)[0], rw=np.asarray(inp["router_w"], np.float32)[0],
        rbias=np.asarray(inp["router_bias"], np.float32)[0][None, :].copy(),
        wge=np.asarray(inp["w_gate_e"], np.float32)[0], wue=np.asarray(inp["w_up_e"], np.float32)[0],
        wde=np.asarray(inp["w_down_e"], np.float32)[0], wgs=np.asarray(inp["w_gate_s"], np.float32)[0],
        wus=np.asarray(inp["w_up_s"], np.float32)[0], wds=np.asarray(inp["w_down_s"], np.float32)[0],
        identf=st["identf"],
    )
    maps = []
    TS = S // NCORES
    for c in range(NCORES):
        m = dict(shared)
        m["xTs"] = np.ascontiguousarray(x[c * TS:(c + 1) * TS].T)
        m["xs"] = np.ascontiguousarray(x[c * TS:(c + 1) * TS])
        m["yTs"] = np.ascontiguousarray(yfull[:, c * TS:(c + 1) * TS])
        maps.append(m)
    return maps


_CACHE = {}


def kernel(**inputs):
    st = static_tables()
    if "l1" not in _CACHE:
        _CACHE["l1"] = build_l1()
    res1 = run_bass_kernel_spmd(_CACHE["l1"], l1_inputs(inputs, st), core_ids=list(range(NCORES)))
    yT_all = [res1.results[c]["yT"] for c in range(NCORES)]
    if "l2" not in _CACHE:
        _CACHE["l2"] = build_l2()
    res2 = run_bass_kernel_spmd(_CACHE["l2"], l2_inputs(inputs, st, yT_all), core_ids=list(range(NCORES)))
    out = np.concatenate([np.asarray(res2.results[c]["out"], np.float32) for c in range(NCORES)], axis=0)
    return out[None, :, :].astype(np.float32)


def build_fused(n_exp=65):
    nc = bass.Bass("TRN2", target_bir_lowering=False)
    k = K(nc)
    EI, EO, IN = "ExternalInput", "ExternalOutput", "Internal"
    NH = 8
    xT_d = k.dram("xT", [D, S], F32, EI)
    c_d = k.dram("c_pk", [128, KC], F32, EI)
    wada_d = k.dram("wada", [D, 12288], F32, EI)
    bada_d = k.dram("bada1", [128, 32], F32, EI)
    ln1_d = k.dram("ln1_pk", [128, KC], F32, EI)
    win_d = k.dram("win", [NH, D, 1920], F32, EI)
    qg_d = k.dram("qg", [128, 1], F32, EI)
    kg_d = k.dram("kg", [128, 1], F32, EI)
    rb_d = k.dram("rb", [NH, 96], F32, EI)
    idx_d = k.dram("idxT", [128, 3 * 256], F32, EI)
    neg_d = k.dram("negT", [128, 256], F32, EI)
    cos_d = k.dram("cosT", [128, S], F32, EI)
    sin_d = k.dram("sinT", [128, S], F32, EI)
    rm_d = k.dram("rmat", [128, 128], F32, EI)
    idf_d = k.dram("identf", [128, 128], F32, EI)
    dmat_d = k.dram("dmatT", [NH, 128, 128], F32, EI)
    xi_d = k.dram("xibc", [NH, 128, 128], F32, EI)
    zeta_d = k.dram("zetacol", [128, NH], F32, EI)
    gch_d = k.dram("gchunk", [128, NH], F32, EI)
    gng_d = k.dram("gng", [NH, 256], F32, EI)
    kbias_d = k.dram("kbias", [128, 192], F32, EI)
    vmask_d = k.dram("vmask", [128, 64], F32, EI)
    qk_s = k.dram("qk_s", [8, 128, S], BF16, IN)
    vt_s = k.dram("vt_s", [S, 896], BF16, IN)
    ys = k.dram("ys", [3072, 1024], BF16, IN)
    xb_s = k.dram("xb_s", [S // 512, 128, KC * 512], BF16, IN)
    zpow_d = k.dram("zpow", [128, NH * 56], F32, EI)

    with ExitStack() as es0:
        rstd_all = k.sb(es0, "rstd_all", [128, S], F32)
        rcol_all = k.sb(es0, "rcol_all", [128, 64], F32)
        zpow = k.sb(es0, "zpow", [128, NH * 56], F32)
        k.dma("sp", zpow[:, :], zpow_d[:, :], writes=[zpow], semres=zpow)
        ones_b = k.sb(es0, "ones_b", [128, 128], BF16)
        idf = k.sb(es0, "idf", [128, 128], F32)
        idb = k.sb(es0, "idb", [128, 128], BF16)
        k.dve(lambda e: e.memset(ones_b[:, :], 1.0), writes=[ones_b])
        k.dma("sp", idf[:, :], idf_d[:, :], writes=[idf], semres=idf)
        k.dve(cp(idb[:, :], idf[:, :]), reads=[idf], writes=[idb])
        gg = k.sb(es0, "gg", [128, KC], F32)
        sh1b = k.sb(es0, "sh1b", [128, KC], BF16)
        sh1bc = k.sb(es0, "sh1bc", [128, KC, 128], BF16)
        qg = k.sb(es0, "qg", [128, 1], F32)
        kg = k.sb(es0, "kg", [128, 1], F32)
        rmb = k.sb(es0, "rmb", [128, 128], BF16)
        epsr = k.sb(es0, "epsr", [128, 1], F32)
        epsg = k.sb(es0, "epsg", [128, 1], F32)
        kbias = k.sb(es0, "kbias", [128, 192], F32)
        vmask = k.sb(es0, "vmask", [128, 64], F32)
        zeta = k.sb(es0, "zeta", [128, NH], F32)
        gch = k.sb(es0, "gch", [128, NH], F32)
        k.dma("sp", kbias[:, :], kbias_d[:, :], writes=[kbias], semres=kbias)
        k.dma("sp", vmask[:, :], vmask_d[:, :], writes=[vmask], semres=vmask)
        k.dma("sp", zeta[:, :], zeta_d[:, :], writes=[zeta], semres=zeta)
        k.dma("sp", gch[:, :], gch_d[:, :], writes=[gch], semres=gch)
        k.dve(lambda e: e.memset(epsr[:, :], RMS_EPS), writes=[epsr])
        k.dve(lambda e: e.memset(epsg[:, :], GN_EPS), writes=[epsg])
        with ExitStack() as es:
            psA0 = k.ps(es, "psM", [128, 512], F32)
            cb = emit_silu_c(k, es, c_d)
            mod1 = emit_mod_cols(k, es, cb, wada_d, bada_d, 32, "mod1", psA0)
            ln1 = k.sb(es, "ln1", [128, KC], F32)
            k.dma("sp", ln1[:, :], ln1_d[:, :], writes=[ln1], semres=ln1)
            k.dve(stt(gg[:, :], mod1[:, 16:32], 1.0, ln1[:, :], ALU.add, ALU.mult), reads=[mod1, ln1], writes=[gg])
            k.dve(cp(sh1b[:, :], mod1[:, 0:16]), reads=[mod1], writes=[sh1b])
            k.dve(cp(sh1bc[:, :, :], sh1b[:, :].unsqueeze(2).to_broadcast([128, KC, 128])), reads=[sh1b], writes=[sh1bc])
            k.dma("sp", qg[:, :], qg_d[:, :], writes=[qg], semres=qg)
            k.dma("sp", kg[:, :], kg_d[:, :], writes=[kg], semres=kg)
            k.dve(ts(qg[:, :], qg[:, :], 128.0 ** -0.5, ALU.mult), reads=[qg], writes=[qg])
            rmf = k.sb(es, "rmf", [128, 128], F32)
            k.dma("sp", rmf[:, :], rm_d[:, :], writes=[rmf], semres=rmf)
            k.dve(cp(rmb[:, :], rmf[:, :]), reads=[rmf], writes=[rmb])
            k.barrier()

        k.nm += 1
        wb = Tl(k, es0.enter_context(nc.sbuf_tensor(f"r{k.nm}_wb", [128, KC, 1920], BF16, side="right")), "wb")

        def load_w(hv_):
            wv = win_d.t[hv_].rearrange("(kc p) n -> p kc n", p=128)
            for kc4 in range(4):
                k.dma("pool", wb[:, kc4 * 4:(kc4 + 1) * 4, :], wv[:, kc4 * 4:(kc4 + 1) * 4, :], writes=[wb], semres=wb)

        load_w(0)
        for hv in range(NH):
            with ExitStack() as es:
                psA = [k.ps(es, f"psA{i}", [128, 512], F32) for i in range(8)]
                b1col = k.sb(es, "b1col", [128, 8], F32)
                for f in range(8):
                    for kc in range(KC):
                        k.pe(mm(psA[1][:, f:f + 1], wb[:, kc, f * 128:(f + 1) * 128], sh1b[:, kc:kc + 1], kc == 0, kc == KC - 1),
                             reads=[wb, sh1b], writes=[psA[1]])
                k.dve(cp(b1col[:, :], psA[1][:, 0:8]), reads=[psA[1]], writes=[b1col])
                b1bc = k.sb(es, "b1bc", [128, 896], F32)
                for j in range(2):
                    for kc in range(KC):
                        k.pe(mm(psA[2 + j][:, 0:448], sh1bc[:, kc, :], wb[:, kc, 1024 + j * 448:1024 + (j + 1) * 448], kc == 0, kc == KC - 1),
                             reads=[wb, sh1bc], writes=[psA[2 + j]])
                    k.dve(cp(b1bc[:, j * 448:(j + 1) * 448], psA[2 + j][:, 0:448]), reads=[psA[2 + j]], writes=[b1bc])
                for kc in range(KC):
                    k.dve(ts(wb[:, kc, :], wb[:, kc, :], gg[:, kc:kc + 1], ALU.mult), reads=[wb, gg], writes=[wb])
                b1k = k.sb(es, "b1k", [128, 1], F32)
                k.dve(ts(b1k[:, :], b1col[:, 7:8], 128.0 ** -0.5, ALU.mult), reads=[b1col], writes=[b1k])

                NT = S // 512
                xbs = [k.sb(es, f"xb{i}", [128, KC, 512], BF16) for i in range(2)]
                sq = k.sb(es, "sq", [128, KC, 512], BF16) if hv == 0 else None
                cs = [k.sb(es, f"cos{i}", [128, 512], F32) for i in range(2)]
                sn = [k.sb(es, f"sin{i}", [128, 512], F32) for i in range(2)]
                t1 = [k.sb(es, f"t1_{i}", [128, 512], F32) for i in range(2)]
                qf = [k.sb(es, f"qf_{i}", [128, 512], F32) for i in range(2)]
                qsq = [k.sb(es, f"qsq_{i}", [128, 512], BF16) for i in range(2)]
                rq = [k.sb(es, f"rq_{i}", [128, 512], F32) for i in range(2)]
                qo = [k.sb(es, f"qo_{i}", [128, 512], BF16) for i in range(4)]
                qfb = [k.sb(es, f"qfb_{i}", [128, 512], BF16) for i in range(2)]
                ra = [k.sb(es, f"ra_{i}", [128, 512], F32) for i in range(2)]
                vo = [k.sb(es, f"vo_{i}", [128, 896], BF16) for i in range(2)]
                xv = xT_d.t.rearrange("(kc p) s -> p kc s", p=128)
                nq = 0
                nv = 0

                def load_x(t):
                    xb = xbs[t % 2]
                    if hv == 0:
                        for h in range(4):
                            k.dma("pool", xb[:, h * 4:(h + 1) * 4, :], xv[:, h * 4:(h + 1) * 4, t * 512:(t + 1) * 512],
                                  writes=[xb], semres=xb)
                    else:
                        k.dma("sp", xb[:, :, :].rearrange("p a b -> p (a b)"), xb_s.t[t], writes=[xb], semres=xb)

                load_x(0)
                for t in range(NT):
                    xb = xbs[t % 2]
                    if t + 1 < NT:
                        load_x(t + 1)
                    c0 = t * 512
                    if t >= 14:
                        flist = list(range(8))
                        segs = [(0, 384), (384, 896)]
                    elif t == 13:
                        flist = [1, 3, 5, 7]
                        segs = [(0, 384), (384, 640)]
                    elif t >= 8:
                        flist = [5, 7]
                        segs = [(256, 384), (384, 640)]
                    else:
                        flist = [7]
                        segs = [(384, 640)]
                    k.dma("sp", cs[t % 2][:, :], cos_d[:, c0:c0 + 512], writes=[cs[t % 2]], semres=cs[t % 2])
                    k.dma("sp", sn[t % 2][:, :], sin_d[:, c0:c0 + 512], writes=[sn[t % 2]], semres=sn[t % 2])
                    rstd_t = rstd_all[:, c0:c0 + 512]
                    rres = rstd_all.r(t)
                    if hv == 0:
                        k.dma("sp", xb_s.t[t], xb[:, :, :].rearrange("p a b -> p (a b)"), reads=[xb], writes=[], semres=xb)
                        for h in range(2):
                            k.act(actf(sq[:, h * 8:(h + 1) * 8, :], xb[:, h * 8:(h + 1) * 8, :], AF.Square), reads=[xb], writes=[sq.r(h)])
                        for kc in range(KC):
                            k.pe(mm(psA[0][:, :], ones_b[:, :], sq[:, kc, :], kc == 0, kc == KC - 1),
                                 reads=[sq.r(kc // 8), ones_b], writes=[psA[0]])
                        k.act(actf(rstd_t, psA[0][:, :], AF.Sqrt, bias=epsr[:, :], scale=1.0 / D), reads=[psA[0], epsr], writes=[rres])
                        k.dve(recip(rstd_t, rstd_t), reads=[rres], writes=[rres])
                        for s4 in range(4):
                            k.pe(lambda e, s4=s4: e.transpose(psA[1][:, s4 * 128:(s4 + 1) * 128], rstd_all[:, c0 + s4 * 128:c0 + (s4 + 1) * 128], idf[:, :]),
                                 reads=[rres, idf], writes=[psA[1]])
                        k.dve(cp(rcol_all[:, t * 4:(t + 1) * 4], psA[1][:, :].rearrange("p (s n) -> p s n", n=128)[:, :, 0]), reads=[psA[1]], writes=[rres])
                    for fi, f in enumerate(flist):
                        pb = psA[2 + (fi % 2)]
                        for kc in range(KC):
                            k.pe(mm(pb[:, :], wb[:, kc, f * 128:(f + 1) * 128], xb[:, kc, :], kc == 0, kc == KC - 1),
                                 reads=[wb, xb], writes=[pb])
                        a = nq % 2
                        nq += 1
                        k.dve(tt(t1[a][:, :], pb[:, :], rstd_t, ALU.mult), reads=[pb, rres], writes=[t1[a]])
                        o = qo[nq % 4]
                        if f < 6:
                            gcol = qg if f % 2 == 0 else kg
                            k.act(actf(qsq[a][:, :], t1[a][:, :], AF.Square, bias=b1col[:, f:f + 1]), reads=[t1[a], b1col], writes=[qsq[a]])
                            k.act(actf(qf[a][:, :], t1[a][:, :], AF.Identity, bias=b1col[:, f:f + 1]), reads=[t1[a], b1col], writes=[qf[a]])
                            pn = psA[4 + a]
                            k.pe(mm(pn[:, :], ones_b[:, :], qsq[a][:, :], True, True), reads=[qsq[a], ones_b], writes=[pn])
                            k.act(actf(rq[a][:, :], pn[:, :], AF.Sqrt, bias=epsr[:, :], scale=1.0 / 128), reads=[pn, epsr], writes=[rq[a]])
                            k.dve(recip(rq[a][:, :], rq[a][:, :]), reads=[rq[a]], writes=[rq[a]])
                            k.dve(stt(o[:, :], qf[a][:, :], gcol[:, 0:1], rq[a][:, :], ALU.mult, ALU.mult), reads=[qf[a], gcol, rq[a]], writes=[o])
                        else:
                            sc = 1.0 if f == 6 else 128.0 ** -0.5
                            bcol = b1col[:, 6:7] if f == 6 else b1k[:, 0:1]
                            bres = b1col if f == 6 else b1k
                            k.act(actf(qf[a][:, :], t1[a][:, :], AF.Identity, bias=bcol, scale=sc), reads=[t1[a], bres], writes=[qf[a]])
                            k.act(actf(qfb[a][:, :], t1[a][:, :], AF.Identity, bias=bcol, scale=sc), reads=[t1[a], bres], writes=[qfb[a]])
                            pn = psA[4 + a]
                            k.pe(mm(pn[:, :], rmb[:, :], qfb[a][:, :], True, True), reads=[qfb[a], rmb], writes=[pn])
                            k.dve(tt(ra[a][:, :], qf[a][:, :], cs[t % 2][:, :], ALU.mult), reads=[qf[a], cs[t % 2]], writes=[ra[a]])
                            k.dve(tt(rq[a][:, :], pn[:, :], sn[t % 2][:, :], ALU.mult), reads=[pn, sn[t % 2]], writes=[rq[a]])
                            k.pool(tt(o[:, :], ra[a][:, :], rq[a][:, :], ALU.add), reads=[ra[a], rq[a]], writes=[o])
                        k.dma("sp", qk_s.t[f, :, c0:c0 + 512], o[:, :], reads=[o], writes=[], semres=o)
                    for s4 in range(4):
                        v = vo[nv % 2]
                        nv += 1
                        for si, (a0, a1) in enumerate(segs):
                            pb = psA[6 + si]
                            for kc in range(KC):
                                k.pe(mm(pb[:, 0:a1 - a0], xb[:, kc, s4 * 128:(s4 + 1) * 128], wb[:, kc, 1024 + a0:1024 + a1],
                                        kc == 0, kc == KC - 1), reads=[wb, xb], writes=[pb])
                            k.dve(stt(v[:, a0:a1], pb[:, 0:a1 - a0], rcol_all[:, t * 4 + s4:t * 4 + s4 + 1], b1bc[:, a0:a1], ALU.mult, ALU.add),
                                  reads=[pb, rres, b1bc], writes=[v])
                        tix = t * 4 + s4
                        k.dve(ts(v[:, 384:640], v[:, 384:640], vmask[:, tix:tix + 1], ALU.mult), reads=[v, vmask], writes=[v])
                        r0 = c0 + s4 * 128
                        lo_, hi_ = segs[0][0], segs[-1][1]
                        k.dma("sp", vt_s.t[r0:r0 + 128, lo_:hi_], v[:, lo_:hi_], reads=[v], writes=[], semres=v)
                k.barrier()
            if hv + 1 < NH:
                load_w(hv + 1)

            with ExitStack() as es:
                psS = [k.ps(es, f"psS{i}", [128, 512], F32) for i in range(2)]
                psO = [k.ps(es, f"psO{i}", [128, 512], F32) for i in range(2)]
                psL = [k.ps(es, f"psL{i}", [128, 512], F32) for i in range(2)]
                accO = k.sb(es, "accO", [128, 2048], F32)
                accL = k.sb(es, "accL", [128, 2048], F32)
                idx = k.sb(es, "idx", [128, 3 * 256], F32)
                negm = k.sb(es, "negm", [128, 256], F32)
                rb = k.sb(es, "rb", [128, 96], F32)
                k.dma("sp", idx[:, :], idx_d[:, :], writes=[idx], semres=idx)
                k.dma("sp", negm[:, :], neg_d[:, :], writes=[negm], semres=negm)
                k.dma("sp", rb[:, :], rb_d.t[hv:hv + 1, :].partition_broadcast(128), writes=[rb], semres=rb)
                mbs = [k.sb(es, f"mb{g}", [128, 256], F32) for g in range(3)]
                tmpm = k.sb(es, "tmpm", [128, 256], F32)
                for g in range(3):
                    k.dve(cp(mbs[g][:, :], negm[:, :]), reads=[negm], writes=[mbs[g]])
                    for b in range(32):
                        k.dve(ts(tmpm[:, :], idx[:, g * 256:(g + 1) * 256], float(b), ALU.is_equal, rb[:, g * 32 + b:g * 32 + b + 1], ALU.mult),
                              reads=[idx, rb], writes=[tmpm])
                        k.dve(tt(mbs[g][:, :], mbs[g][:, :], tmpm[:, :], ALU.add), reads=[tmpm, mbs[g]], writes=[mbs[g]])
                qm = [k.sb(es, f"qm{i}", [128, 2048], BF16) for i in range(2)]
                km = [k.sb(es, f"km{i}", [128, 2048], BF16) for i in range(2)]
                vm = [k.sb(es, f"vm{i}", [128, 16, 128], BF16) for i in range(2)]
                kpv = k.sb(es, "kpv", [128, 2048], BF16)
                vpv = k.sb(es, "vpv", [128, 16, 128], BF16)
                sadd = [k.sb(es, f"sadd{i}", [128, 256], F32) for i in range(2)]
                pT = [k.sb(es, f"pT{i}", [128, 256], BF16) for i in range(3)]
                nblk = 0
                nbatch = 0

                def load_v(dst, g, t0, d, nsb):
                    if d == 1:
                        k.dma("sp", dst[:, :, :], vt_s.t[t0:t0 + 2048, g * 128:(g + 1) * 128].rearrange("(n c) e -> c n e", c=128),
                              writes=[dst], semres=dst)
                    else:
                        for nl_ in range(nsb):
                            ta_ = t0 + nl_ * 128 * d
                            k.dma("sp", dst[:, nl_ * d:(nl_ + 1) * d, :],
                                  vt_s.t[ta_:ta_ + 128 * d, g * 128:(g + 1) * 128].rearrange("(c r) e -> c r e", r=d),
                                  writes=[dst], semres=dst)

                for g in range(3):
                    d = DILS[g]
                    nsb = 16 // d
                    q, kk, vv = qm[g % 2], km[g % 2], vm[g % 2]
                    t0 = 3 * 2048
                    k.dma("sp", q[:, :], qk_s.t[2 * g, :, t0:t0 + 2048], writes=[q], semres=q)
                    k.dma("sp", kk[:, :], qk_s.t[2 * g + 1, :, t0:t0 + 2048], writes=[kk], semres=kk)
                    load_v(vv, g, t0, d, nsb)
                    if g == 2:
                        k.dma("sp", kpv[:, :], qk_s.t[2 * g + 1, :, t0 - 2048:t0], writes=[kpv], semres=kpv)
                        load_v(vpv, g, t0 - 2048, d, nsb)
                    for jb in ((2, 3) if g < 2 else (0, 1, 2, 3)):
                        bo = psO[nbatch % 2]
                        bl = psL[nbatch % 2]
                        nbatch += 1
                        for ji in range(4):
                            j = jb * 4 + ji
                            nl, r = j // d, j % d
                            dsl = lambda st_: slice(st_, st_ + 127 * d + 1, d)
                            cols = dsl(nl * 128 * d + r)
                            N = 3 * nsb + nl
                            if nl > 0:
                                pk, pv = kk, vv
                                pcols = dsl((nl - 1) * 128 * d + r)
                                pj = (nl - 1) * d + r
                            else:
                                pk, pv = kpv, vpv
                                pcols = dsl((nsb - 1) * 128 * d + r)
                                pj = (nsb - 1) * d + r
                            ps = psS[nblk % 2]
                            sa = sadd[nblk % 2]
                            p = pT[nblk % 3]
                            nblk += 1
                            qa = q[:, cols]
                            k.pe(mm(ps[:, 0:128], pk[:, pcols], qa, True, True), reads=[pk, q], writes=[ps])
                            k.pe(mm(ps[:, 128:256], kk[:, cols], qa, True, True), reads=[kk, q], writes=[ps])
                            k.dve(tt(sa[:, 0:256], ps[:, 0:256], mbs[g][:, 0:256], ALU.add), reads=[ps, mbs[g]], writes=[sa])
                            k.act(actf(p[:, 0:128], sa[:, 0:128], AF.Exp, bias=kbias[:, g * 64 + N - 1:g * 64 + N]), reads=[sa, kbias], writes=[p.r(0)])
                            k.act(actf(p[:, 128:256], sa[:, 128:256], AF.Exp, bias=kbias[:, g * 64 + N:g * 64 + N + 1]), reads=[sa, kbias], writes=[p.r(1)])
                            osl = bo[:, ji * 128:(ji + 1) * 128]
                            lsl = bl[:, ji * 128:(ji + 1) * 128]
                            k.pe(mm(osl, pv[:, pj, :], p[:, 0:128], True, False), reads=[pv, p.r(0)], writes=[bo])
                            k.pe(mm(lsl, ones_b[:, :], p[:, 0:128], True, False), reads=[ones_b, p.r(0)], writes=[bl])
                            k.pe(mm(osl, vv[:, j, :], p[:, 128:256], False, True), reads=[vv, p.r(1)], writes=[bo])
                            k.pe(mm(lsl, ones_b[:, :], p[:, 128:256], False, True), reads=[ones_b, p.r(1)], writes=[bl])
                        j0 = jb * 4
                        nl0, r0 = j0 // d, j0 % d
                        if d == 1:
                            dst = slice(j0 * 128, j0 * 128 + 512)
                            dO, dL, sO, sL = accO[:, dst], accL[:, dst], bo[:, :], bl[:, :]
                        else:
                            base = nl0 * 128 * d
                            dO = accO[:, base:base + 128 * d].rearrange("p (a r) -> p r a", r=d)[:, r0:r0 + 4, :]
                            dL = accL[:, base:base + 128 * d].rearrange("p (a r) -> p r a", r=d)[:, r0:r0 + 4, :]
                            sO = bo[:, :].rearrange("p (r a) -> p r a", a=128)
                            sL = bl[:, :].rearrange("p (r a) -> p r a", a=128)
                        if g == 0:
                            k.dve(cp(dO, sO), reads=[bo], writes=[accO])
                            k.dve(cp(dL, sL), reads=[bl], writes=[accL])
                        else:
                            k.dve(tt(dO, sO, dO, ALU.add), reads=[bo, accO], writes=[accO])
                            k.dve(tt(dL, sL, dL, ALU.add), reads=[bl, accL], writes=[accL])
                yo = k.sb(es, "yo", [128, 1024], BF16)
                own = slice(1024, 2048)
                k.dve(recip(accL[:, own], accL[:, own]), reads=[accL], writes=[accL])
                k.dve(tt(yo[:, :], accO[:, own], accL[:, own], ALU.mult), reads=[accO, accL], writes=[yo])
                k.dma("sp", ys.t[hv * 128:(hv + 1) * 128, :], yo[:, :], reads=[yo], writes=[], semres=yo)
                k.barrier()

            with ExitStack() as es:
                psAT = [k.ps(es, f"psAT{i}", [128, 512], F32) for i in range(2)]
                psR = [k.ps(es, f"psR{i}", [128, 512], F32) for i in range(2)]
                psU = k.ps(es, "psU", [128, 512], F32)
                psK2 = [k.ps(es, f"psK{i}", [128, 1024], BF16) for i in range(2)]
                psY = [k.ps(es, f"psY{i}", [128, 1024], BF16) for i in range(1)] * 2
                dmat = k.sb(es, "dmat", [128, 128], F32)
                xib = k.sb(es, "xib", [128, 128], F32)
                gng = k.sb(es, "gng", [128, 256], F32)
                k.dma("sp", dmat[:, :], dmat_d.t[hv], writes=[dmat], semres=dmat)
                k.dma("sp", xib[:, :], xi_d.t[hv], writes=[xib], semres=xib)
                k.dma("sp", gng[:, :], gng_d.t[hv:hv + 1, :].partition_broadcast(128), writes=[gng], semres=gng)
                qm_ = k.sb(es, "rqm", [128, 2048], BF16)
                km = [k.sb(es, f"rkm{i}", [128, 2048], BF16) for i in range(2)]
                vm = [k.sb(es, f"rvm{i}", [128, 16, 256], BF16) for i in range(2)]
                gm_ = k.sb(es, "rgm", [128, 16, 256], BF16)
                St = k.sb(es, "St", [128, 256], F32)
                Sb = k.sb(es, "Sb", [128, 256], BF16)
                atd = [k.sb(es, f"atd{i}", [128, 128], BF16) for i in range(2)]
                qx = [k.sb(es, f"qx{i}", [128, 128], BF16) for i in range(2)]
                kz = [k.sb(es, f"kz{i}", [128, 128], BF16) for i in range(2)]
                s1 = [k.sb(es, f"s1_{i}", [128, 1], F32) for i in range(2)]
                ssq = [k.sb(es, f"ssq_{i}", [128, 1], F32) for i in range(2)]
                junk = [k.sb(es, f"junk{i}", [128, 256], F32) for i in range(2)]
                yn = [k.sb(es, f"yn{i}", [128, 256], F32) for i in range(2)]
                sg = [k.sb(es, f"sg{i}", [128, 256], F32) for i in range(2)]
                yb = [k.sb(es, f"yb{i}", [128, 256], BF16) for i in range(2)]
                ybT = k.sb(es, "ybT", [128, 2, 1024], BF16)
                k.dve(lambda e: e.memset(St[:, :], 0.0), writes=[St])
                k.dve(lambda e: e.memset(Sb[:, :], 0.0), writes=[Sb])
                for m in range(4):
                    t0 = m * 2048
                    kk, vv = km[m % 2], vm[m % 2]
                    k.dma("sp", kk[:, :], qk_s.t[7, :, t0:t0 + 2048], writes=[kk], semres=kk)
                    k.dma("sp", vv[:, :, :], vt_s.t[t0:t0 + 2048, 384:640].rearrange("(n c) e -> c n e", c=128), writes=[vv], semres=vv)
                    if m == 3:
                        k.dma("sp", qm_[:, :], qk_s.t[6, :, t0:t0 + 2048], writes=[qm_], semres=qm_)
                        k.dma("sp", gm_[:, :, :], vt_s.t[t0:t0 + 2048, 640:896].rearrange("(n c) e -> c n e", c=128), writes=[gm_], semres=gm_)
                    for n in range(16):
                        gn = m * 16 + n
                        a = gn % 2
                        cols = slice(n * 128, (n + 1) * 128)
                        own_c = gn >= 56
                        if own_c:
                            k.pe(mm(psAT[a][:, 0:128], kk[:, cols], qm_[:, cols], True, True), reads=[kk, qm_], writes=[psAT[a]])
                            k.dve(tt(atd[a][:, :], psAT[a][:, 0:128], dmat[:, :], ALU.mult), reads=[psAT[a], dmat], writes=[atd[a]])
                            k.pool(tt(qx[a][:, :], qm_[:, cols], xib[:, :], ALU.mult), reads=[qm_, xib], writes=[qx[a]])
                            k.pe(mm(psR[a][:, 0:256], atd[a][:, :], vv[:, n, :], True, False), reads=[atd[a], vv], writes=[psR[a]])
                            k.pe(mm(psR[a][:, 0:256], qx[a][:, :], Sb[:, :], False, True), reads=[qx[a], Sb], writes=[psR[a]])
                        if gn < 56:
                            pk_ = psK2[gn % 2]
                            k.pe(lambda e, cols=cols, kk=kk, pk_=pk_: e.transpose(pk_[:, 0:128], kk[:, cols], idb[:, :]), reads=[kk, idb], writes=[pk_])
                            k.act(actf(kz[a][:, :], pk_[:, 0:128], AF.Copy, scale=zpow[:, hv * 56 + gn:hv * 56 + gn + 1]), reads=[pk_, zpow], writes=[kz[a]])
                            k.pe(mm(psU[:, 0:256], kz[a][:, :], vv[:, n, :], gn == 0, gn == 55), reads=[kz[a], vv], writes=[psU])
                            if gn == 55:
                                k.dve(cp(St[:, :], psU[:, 0:256]), reads=[psU], writes=[St])
                                k.act(lambda e: e.copy(out=Sb[:, :], in_=St[:, :]), reads=[St], writes=[Sb])
                        elif gn < 63:
                            pk_ = psK2[gn % 2]
                            k.pe(lambda e, cols=cols, kk=kk, pk_=pk_: e.transpose(pk_[:, 0:128], kk[:, cols], idb[:, :]), reads=[kk, idb], writes=[pk_])
                            k.act(actf(kz[a][:, :], pk_[:, 0:128], AF.Copy, scale=zeta[:, hv:hv + 1]), reads=[pk_, zeta], writes=[kz[a]])
                            k.pe(mm(psU[:, 0:256], kz[a][:, :], vv[:, n, :], True, True), reads=[kz[a], vv], writes=[psU])
                            k.dve(stt(St[:, :], St[:, :], gch[:, hv:hv + 1], psU[:, 0:256], ALU.mult, ALU.add), reads=[St, gch, psU], writes=[St])
                            k.act(lambda e: e.copy(out=Sb[:, :], in_=St[:, :]), reads=[St], writes=[Sb])
                        if own_c:
                            k.dve(lambda e, a=a: e.tensor_reduce(out=s1[a][:, :], in_=psR[a][:, 0:256], op=ALU.add, axis=AX.X), reads=[psR[a]], writes=[s1[a]])
                            k.dve(ts(s1[a][:, :], s1[a][:, :], -1.0 / 256, ALU.mult), reads=[s1[a]], writes=[s1[a]])
                            k.act(actf(junk[a][:, :], psR[a][:, 0:256], AF.Square, bias=s1[a][:, 0:1], accum_out=ssq[a][:, :]), reads=[psR[a], s1[a]], writes=[junk[a], ssq[a]])
                            k.act(actf(ssq[a][:, :], ssq[a][:, :], AF.Sqrt, bias=epsg[:, :], scale=1.0 / 256), reads=[ssq[a], epsg], writes=[ssq[a]])
                            k.dve(recip(ssq[a][:, :], ssq[a][:, :]), reads=[ssq[a]], writes=[ssq[a]])
                            k.dve(ts(yn[a][:, :], psR[a][:, 0:256], s1[a][:, 0:1], ALU.add, ssq[a][:, 0:1], ALU.mult), reads=[psR[a], s1[a], ssq[a]], writes=[yn[a]])
                            k.act(actf(sg[a][:, :], gm_[:, n, :], AF.Silu), reads=[gm_], writes=[sg[a]])
                            k.pool(tt(yn[a][:, :], yn[a][:, :], gng[:, :], ALU.mult), reads=[yn[a], gng], writes=[yn[a]])
                            k.pool(tt(yb[a][:, :], yn[a][:, :], sg[a][:, :], ALU.mult), reads=[yn[a], sg[a]], writes=[yb[a]])
                            for h in range(2):
                                k.pe(lambda e, a=a, h=h: e.transpose(psY[a][:, h * 128:(h + 1) * 128], yb[a][:, h * 128:(h + 1) * 128], idb[:, :]),
                                     reads=[yb[a], idb], writes=[psY[a]])
                            lc = slice((n - 8) * 128, (n - 7) * 128)
                            k.dve(cp(ybT[:, :, lc], psY[a][:, 0:256].rearrange("p (h t) -> p h t", h=2)), reads=[psY[a]], writes=[ybT])
                for h in range(2):
                    r0 = 1024 + hv * 256 + h * 128
                    k.dma("sp", ys.t[r0:r0 + 128, :], ybT[:, h, :], reads=[ybT], writes=[], semres=ybT)
                k.barrier()
    shared = {"c_pk": c_d, "wada": wada_d, "bada1": bada_d, "ln1_pk": ln1_d, "identf": idf_d, "ys": ys}
    return build_l2(n_exp=n_exp, nc=nc, k=k, shared=shared)


def fused_inputs(inp, st):
    x = np.asarray(inp["x"], np.float32)[0]
    w_in = np.asarray(inp["w_in"], np.float32)[0]
    w_ada = np.asarray(inp["w_ada"], np.float32)[0]
    b_ada = np.asarray(inp["b_ada"], np.float32)[0]
    rel_bias = np.asarray(inp["rel_bias"], np.float32)
    TS = S // NCORES
    wins = []
    for c in range(NCORES):
        cols = []
        for g in range(3):
            h = g * 8 + c
            cols += [np.arange(h * 128, (h + 1) * 128), 3072 + np.arange(h * 128, (h + 1) * 128)]
        cols += [9216 + np.arange(c * 128, (c + 1) * 128), 10240 + np.arange(c * 128, (c + 1) * 128)]
        for g in range(3):
            h = g * 8 + c
            cols += [6144 + np.arange(h * 128, (h + 1) * 128)]
        cols += [11264 + np.arange(c * 256, (c + 1) * 256), 13312 + np.arange(c * 256, (c + 1) * 256)]
        wins.append(w_in[:, np.concatenate(cols)])
    win = np.ascontiguousarray(np.stack(wins))
    rb = np.ascontiguousarray(np.stack([np.concatenate([rel_bias[:, g * 8 + c] for g in range(3)]) for c in range(NCORES)]))
    shared = dict(
        c_pk=pk16(inp["c"][0]), wada=w_ada, bada1=pk16(b_ada[0:4096]), badar=np.ascontiguousarray(b_ada[4096:12288][None, :]),
        ln1_pk=pk16(inp["ln1_g"][0]), ln2_row=np.asarray(inp["ln2_g"], np.float32)[0][None, :].copy(),
        win=win, qg=np.asarray(inp["q_norm_g"], np.float32)[0][:, None].copy(), kg=np.asarray(inp["k_norm_g"], np.float32)[0][:, None].copy(),
        rb=rb, idxT=st["idxT"], negT=st["negT"], rmat=st["rmat"], identf=st["identf"],
        dmatT=np.ascontiguousarray(np.stack([r["dmatT"] for r in st["ret"]])),
        xibc=np.ascontiguousarray(np.stack([r["xibc"] for r in st["ret"]])),
        zetacol=np.ascontiguousarray(np.concatenate([r["zetacol"] for r in st["ret"]], axis=1)),
        gchunk=np.ascontiguousarray(np.concatenate([r["gchunk"] for r in st["ret"]], axis=1)),
        gng=np.ascontiguousarray(np.asarray(inp["ret_gn_g"], np.float32)[0].reshape(8, 256)),
        zpow=st["zpow"],
        wgates=np.ascontiguousarray(w_in[:, 15360:19456]), pa=np.asarray(inp["p_a"], np.float32)[0], pb=np.asarray(inp["p_b"], np.float32)[0],
        wo=np.asarray(inp["w_o"], np.float32)[0], rw=np.asarray(inp["router_w"], np.float32)[0],
        rbias=np.asarray(inp["router_bias"], np.float32)[0][None, :].copy(),
        wge=np.asarray(inp["w_gate_e"], np.float32)[0], wue=np.asarray(inp["w_up_e"], np.float32)[0],
        wde=np.asarray(inp["w_down_e"], np.float32)[0], wgs=np.asarray(inp["w_gate_s"], np.float32)[0],
        wus=np.asarray(inp["w_up_s"], np.float32)[0], wds=np.asarray(inp["w_down_s"], np.float32)[0],
    )
    maps = []
    cpart = np.arange(128)[:, None]
    for c in range(NCORES):
        shift = S - TS * (c + 1)
        xs_ = np.zeros((S, D), np.float32)
        xs_[shift:] = x[0:S - shift]
        cosT = np.zeros((128, S), np.float32)
        sinT = np.zeros((128, S), np.float32)
        cosT[:, shift:] = st["cosT"][:, 0:S - shift]
        sinT[:, shift:] = st["sinT"][:, 0:S - shift]
        kbias = np.zeros((128, 3, 64), np.float32)
        for g, d in enumerate(DILS):
            for N in range(64 // d):
                kbias[:, g, N] = np.where((N * 128 + cpart[:, 0]) * d >= shift, 0.0, NEG)
        vmask = np.tile(((np.arange(64) * 128) >= shift).astype(np.float32)[None, :], (128, 1))
        m = dict(shared)
        m.update(xT=np.ascontiguousarray(xs_.T), cosT=cosT, sinT=sinT, kbias=np.ascontiguousarray(kbias.reshape(128, 192)), vmask=vmask,
                 xTs=np.ascontiguousarray(x[c * TS:(c + 1) * TS].T), xs=np.ascontiguousarray(x[c * TS:(c + 1) * TS]))
        maps.append(m)
    return maps


def kernel(**inputs):
    st = static_tables()
    if "fused" not in _CACHE:
        _CACHE["fused"] = build_fused()
    res = run_bass_kernel_spmd(_CACHE["fused"], fused_inputs(inputs, st), core_ids=list(range(NCORES)))
    out = np.concatenate([np.asarray(res.results[c]["out"], np.float32) for c in range(NCORES)], axis=0)
    return out[None, :, :].astype(np.float32)
```

```python
import math
from contextlib import ExitStack

import numpy as np
import ml_dtypes
import concourse.bass as bass
import concourse.mybir as mybir
from concourse.bass_utils import run_bass_kernel_spmd

F32 = mybir.dt.float32
BF16 = mybir.dt.bfloat16
ALU = mybir.AluOpType
AF = mybir.ActivationFunctionType
AX = mybir.AxisListType

NCORES = 8
S = 8192
D = 2048
KC = 16
NEG = -30000.0
RMS_EPS = 1e-6
GN_EPS = 1e-5
DILS = (1, 4, 16)
NDSEM = 90


class DSem:
    def __init__(self, sem):
        self.sem = sem
        self.total = 0


class Res:
    def __init__(self, name):
        self.name = name
        self.lw = []
        self.rd = []
        self.dsem = None


class Tl:
    def __init__(self, k, t, name):
        self.k = k
        self.t = t
        self.name = name
        self.whole = Res(name)
        self.subs = {}

    def r(self, key):
        if key not in self.subs:
            self.subs[key] = Res(f"{self.name}.{key}")
        return self.subs[key]

    def __getitem__(self, idx):
        return self.t[idx]


def _res(x):
    return x.whole if isinstance(x, Tl) else x


class K:
    def __init__(self, nc):
        self.nc = nc
        self.eng = {"pe": nc.tensor, "dve": nc.vector, "act": nc.scalar, "pool": nc.gpsimd, "sp": nc.sync}
        self.esem = {}
        self.ecnt = {}
        for q in ("pe", "dve", "act", "pool"):
            self.esem[q] = nc.alloc_semaphore(name="e_" + q)
            self.ecnt[q] = 0
        self.waited = {}
        self.dsems = []
        self.nm = 0
        self.dsems = [DSem(nc.alloc_semaphore(name=f"d{i}")) for i in range(NDSEM)]
        self.dfree = list(self.dsems)
        self.bound = []
        for q in self.esem:
            nc.gpsimd.sem_clear(self.esem[q])
        for d in self.dsems:
            nc.gpsimd.sem_clear(d.sem)
        nc.all_engine_barrier()

    def sb(self, es, name, shape, dt):
        self.nm += 1
        t = es.enter_context(self.nc.sbuf_tensor(f"s{self.nm}_{name}", list(shape), dt))
        return Tl(self, t, name)

    def ps(self, es, name, shape, dt):
        self.nm += 1
        t = es.enter_context(self.nc.psum_tensor(f"p{self.nm}_{name}", list(shape), dt))
        return Tl(self, t, name)

    def dram(self, name, shape, dt, kind):
        t = self.nc.dram_tensor(name, list(shape), dt, kind=kind).ap()
        return Tl(self, t, name)

    def _wait(self, q, toks):
        best = {}
        for tok in toks:
            s, v = tok
            if isinstance(s, DSem):
                v = s.total
                key = id(s)
                semh = s.sem
            else:
                key = s
                semh = self.esem[s]
                if s == q and q == "pe":
                    continue
            if self.waited.get((q, key), 0) >= v:
                continue
            if key not in best or best[key][1] < v:
                best[key] = (semh, v)
        for key, (semh, v) in best.items():
            self.eng[q].wait_ge(semh, v)
            self.waited[(q, key)] = v

    def _deps(self, reads, writes):
        deps = []
        for r in reads:
            deps += _res(r).lw
        for w in writes:
            w = _res(w)
            deps += w.lw
            deps += w.rd
        return deps

    def _commit(self, tok, reads, writes):
        for r in reads:
            _res(r).rd.append(tok)
        for w in writes:
            w = _res(w)
            w.lw = [tok]
            w.rd = []

    def op(self, q, fn, reads=(), writes=()):
        self._wait(q, self._deps(reads, writes))
        ins = fn(self.eng[q])
        self.ecnt[q] += 1
        ins.then_inc(self.esem[q], 1)
        self._commit((q, self.ecnt[q]), reads, writes)

    def pe(self, fn, reads=(), writes=()):
        self.op("pe", fn, reads, writes)

    def dve(self, fn, reads=(), writes=()):
        self.op("dve", fn, reads, writes)

    def act(self, fn, reads=(), writes=()):
        self.op("act", fn, reads, writes)

    def pool(self, fn, reads=(), writes=()):
        self.op("pool", fn, reads, writes)

    def dma(self, q, out, in_, reads=(), writes=(), semres=None, **kw):
        r = _res(semres)
        if r.dsem is None:
            r.dsem = self.dfree.pop(0)
            self.bound.append(r)
        self._wait(q, self._deps(reads, writes))
        ins = self.eng[q].dma_start(out=out, in_=in_, **kw)
        ins.then_inc(r.dsem.sem, 16)
        r.dsem.total += 16
        self._commit((r.dsem, r.dsem.total), reads, writes)

    def barrier(self):
        toks = [(q, self.ecnt[q]) for q in self.esem if self.ecnt[q] > 0]
        toks += [(d, d.total) for d in self.dsems if d.total > 0]
        for q in self.eng:
            self._wait(q, toks)
        for r in self.bound:
            self.dfree.append(r.dsem)
            r.dsem = None
        self.bound = []

    def finish(self):
        toks = [(q, self.ecnt[q]) for q in self.esem if self.ecnt[q] > 0]
        toks += [(d, d.total) for d in self.dsems if d.total > 0]
        self._wait("sp", toks)
        self._wait("pool", toks)
        self.nc.all_engine_barrier()
        for q in self.esem:
            self.nc.gpsimd.sem_clear(self.esem[q])
        for d in self.dsems:
            self.nc.gpsimd.sem_clear(d.sem)


def mm(out, lhsT, rhs, start, stop):
    return lambda e: e.matmul(out, lhsT, rhs, start=start, stop=stop)


def tt(out, a, b, op):
    return lambda e: e.tensor_tensor(out=out, in0=a, in1=b, op=op)


def ts(out, a, s1, op0, s2=None, op1=None):
    if op1 is None:
        return lambda e: e.tensor_scalar(out=out, in0=a, scalar1=s1, scalar2=None, op0=op0)
    return lambda e: e.tensor_scalar(out=out, in0=a, scalar1=s1, scalar2=s2, op0=op0, op1=op1)


def stt(out, a, sc, b, op0, op1):
    return lambda e: e.scalar_tensor_tensor(out=out, in0=a, scalar=sc, in1=b, op0=op0, op1=op1)


def actf(out, in_, func, bias=None, scale=1.0, accum_out=None):
    kw = {}
    if bias is not None:
        kw["bias"] = bias
    if accum_out is not None:
        kw["accum_out"] = accum_out
    return lambda e: e.activation(out=out, in_=in_, func=func, scale=scale, **kw)


def cp(out, in_):
    return lambda e: e.tensor_copy(out=out, in_=in_)


def recip(out, in_):
    return lambda e: e.reciprocal(out=out, in_=in_)


def emit_silu_c(k, es, c_d):
    cf = k.sb(es, "c_f", [128, KC], F32)
    cb = k.sb(es, "c_b", [128, KC], BF16)
    k.dma("sp", cf[:, :], c_d[:, :], writes=[cf], semres=cf)
    k.act(actf(cb[:, :], cf[:, :], AF.Silu), reads=[cf], writes=[cb])
    return cb


def emit_mod_cols(k, es, cb, wada_d, bada_d, ncols_tiles, name, ps_bank):
    nt = ncols_tiles
    out = k.sb(es, name, [128, nt], F32)
    bt = k.sb(es, name + "_b", [128, nt], F32)
    k.dma("sp", bt[:, :], bada_d[:, :], writes=[bt], semres=bt)
    wv = wada_d.t.rearrange("(kc p) n -> p kc n", p=128)
    with ExitStack() as es2:
        wbufs = [k.sb(es2, f"{name}_w{i}", [128, KC, 512], BF16) for i in range(2)]
        ngrp = (nt * 128 + 511) // 512
        for g in range(ngrp):
            wb = wbufs[g % 2]
            c0 = g * 512
            cw = min(512, nt * 128 - c0)
            for h in range(2):
                k.dma("pool", wb[:, h * 8:(h + 1) * 8, 0:cw], wv[:, h * 8:(h + 1) * 8, c0:c0 + cw],
                      writes=[wb], semres=wb)
            for j in range(cw // 128):
                col = g * 4 + j
                for kc in range(KC):
                    k.pe(mm(ps_bank[:, col:col + 1], wb[:, kc, j * 128:(j + 1) * 128], cb[:, kc:kc + 1],
                            kc == 0, kc == KC - 1), reads=[wb, cb], writes=[ps_bank])
        k.dve(tt(out[:, :], ps_bank[:, 0:nt], bt[:, :], ALU.add), reads=[ps_bank, bt], writes=[out])
        k.barrier()
    return out


def build_l1(debug=False):
    nc = bass.Bass("TRN2", target_bir_lowering=False)
    k = K(nc)
    EI, EO, IN = "ExternalInput", "ExternalOutput", "Internal"
    xT_d = k.dram("xT", [D, S], F32, EI)
    c_d = k.dram("c_pk", [128, KC], F32, EI)
    wada_d = k.dram("wada1", [D, 4096], F32, EI)
    bada_d = k.dram("bada1", [128, 32], F32, EI)
    ln1_d = k.dram("ln1_pk", [128, KC], F32, EI)
    win_d = k.dram("win", [D, 1920], F32, EI)
    qg_d = k.dram("qg", [128, 1], F32, EI)
    kg_d = k.dram("kg", [128, 1], F32, EI)
    rb_d = k.dram("rb", [1, 96], F32, EI)
    idx_d = k.dram("idxT", [128, 3 * 256], F32, EI)
    neg_d = k.dram("negT", [128, 256], F32, EI)
    cos_d = k.dram("cosT", [128, S], F32, EI)
    sin_d = k.dram("sinT", [128, S], F32, EI)
    rm_d = k.dram("rmat", [128, 128], F32, EI)
    idf_d = k.dram("identf", [128, 128], F32, EI)
    dmat_d = k.dram("dmatT", [128, 128], F32, EI)
    xi_d = k.dram("xibc", [128, 128], F32, EI)
    zeta_d = k.dram("zetacol", [128, 1], F32, EI)
    gch_d = k.dram("gchunk", [128, 1], F32, EI)
    gng_d = k.dram("gng", [1, 256], F32, EI)
    yT_d = k.dram("yT", [384, S], BF16, EO)
    qk_s = k.dram("qk_s", [8, 128, S], BF16, EO if debug else IN)
    vt_s = k.dram("vt_s", [S, 896], BF16, EO if debug else IN)

    with ExitStack() as es0:
        ones_b = k.sb(es0, "ones_b", [128, 128], BF16)
        idf = k.sb(es0, "idf", [128, 128], F32)
        idb = k.sb(es0, "idb", [128, 128], BF16)
        k.dve(lambda e: e.memset(ones_b[:, :], 1.0), writes=[ones_b])
        k.dma("sp", idf[:, :], idf_d[:, :], writes=[idf], semres=idf)
        k.dve(cp(idb[:, :], idf[:, :]), reads=[idf], writes=[idb])

        with ExitStack() as es:
            psA = [k.ps(es, f"psA{i}", [128, 512], F32) for i in range(8)]
            cb = emit_silu_c(k, es, c_d)
            mod1 = emit_mod_cols(k, es, cb, wada_d, bada_d, 32, "mod1", psA[0])
            ln1 = k.sb(es, "ln1", [128, KC], F32)
            k.dma("sp", ln1[:, :], ln1_d[:, :], writes=[ln1], semres=ln1)
            gg = k.sb(es, "gg", [128, KC], F32)
            k.dve(stt(gg[:, :], mod1[:, 16:32], 1.0, ln1[:, :], ALU.add, ALU.mult), reads=[mod1, ln1], writes=[gg])
            sh1b = k.sb(es, "sh1b", [128, KC], BF16)
            k.dve(cp(sh1b[:, :], mod1[:, 0:16]), reads=[mod1], writes=[sh1b])
            sh1bc = k.sb(es, "sh1bc", [128, KC, 128], BF16)
            k.dve(cp(sh1bc[:, :, :], sh1b[:, :].unsqueeze(2).to_broadcast([128, KC, 128])), reads=[sh1b], writes=[sh1bc])

            wb = k.sb(es, "wb", [128, KC, 1920], BF16)
            wv = win_d.t.rearrange("(kc p) n -> p kc n", p=128)
            for kc4 in range(4):
                k.dma("pool", wb[:, kc4 * 4:(kc4 + 1) * 4, :], wv[:, kc4 * 4:(kc4 + 1) * 4, :], writes=[wb], semres=wb)
            b1col = k.sb(es, "b1col", [128, 8], F32)
            for f in range(8):
                for kc in range(KC):
                    k.pe(mm(psA[1][:, f:f + 1], wb[:, kc, f * 128:(f + 1) * 128], sh1b[:, kc:kc + 1], kc == 0, kc == KC - 1),
                         reads=[wb, sh1b], writes=[psA[1]])
            k.dve(cp(b1col[:, :], psA[1][:, 0:8]), reads=[psA[1]], writes=[b1col])
            b1bc = k.sb(es, "b1bc", [128, 896], F32)
            for j in range(2):
                for kc in range(KC):
                    k.pe(mm(psA[2 + j][:, 0:448], sh1bc[:, kc, :], wb[:, kc, 1024 + j * 448:1024 + (j + 1) * 448], kc == 0, kc == KC - 1),
                         reads=[wb, sh1bc], writes=[psA[2 + j]])
                k.dve(cp(b1bc[:, j * 448:(j + 1) * 448], psA[2 + j][:, 0:448]), reads=[psA[2 + j]], writes=[b1bc])
            for kc in range(KC):
                eng = k.dve if kc % 2 == 0 else k.pool
                eng(ts(wb[:, kc, :], wb[:, kc, :], gg[:, kc:kc + 1], ALU.mult), reads=[wb, gg], writes=[wb])

            qg = k.sb(es, "qg", [128, 1], F32)
            kg = k.sb(es, "kg", [128, 1], F32)
            k.dma("sp", qg[:, :], qg_d[:, :], writes=[qg], semres=qg)
            k.dma("sp", kg[:, :], kg_d[:, :], writes=[kg], semres=kg)
            k.dve(ts(qg[:, :], qg[:, :], 128.0 ** -0.5, ALU.mult), reads=[qg], writes=[qg])
            b1k = k.sb(es, "b1k", [128, 1], F32)
            k.dve(ts(b1k[:, :], b1col[:, 7:8], 128.0 ** -0.5, ALU.mult), reads=[b1col], writes=[b1k])
            rmf = k.sb(es, "rmf", [128, 128], F32)
            rmb = k.sb(es, "rmb", [128, 128], BF16)
            k.dma("sp", rmf[:, :], rm_d[:, :], writes=[rmf], semres=rmf)
            k.dve(cp(rmb[:, :], rmf[:, :]), reads=[rmf], writes=[rmb])
            epsr = k.sb(es, "epsr", [128, 1], F32)
            k.dve(lambda e: e.memset(epsr[:, :], RMS_EPS), writes=[epsr])

            if debug == "A0":
                dbg = k.dram("dbg", [128, 2048], F32, EO)
                stg = k.sb(es, "stg", [128, 2048], F32)
                k.dve(lambda e: e.memset(stg[:, :], 0.0), writes=[stg])
                k.dve(cp(stg[:, 0:32], mod1[:, :]), reads=[mod1], writes=[stg])
                k.dve(cp(stg[:, 32:48], gg[:, :]), reads=[gg], writes=[stg])
                k.dve(cp(stg[:, 48:56], b1col[:, :]), reads=[b1col], writes=[stg])
                k.dve(cp(stg[:, 64:960], b1bc[:, :]), reads=[b1bc], writes=[stg])
                k.dve(cp(stg[:, 960:976], cb[:, :]), reads=[cb], writes=[stg])
                k.dve(cp(stg[:, 1024:1536], wb[:, 3, 0:512]), reads=[wb], writes=[stg])
                k.dma("sp", dbg.t[:, :], stg[:, :], reads=[stg], writes=[dbg], semres=stg)
                k.finish()
                return nc
            NT = S // 512
            xbs = [k.sb(es, f"xb{i}", [128, KC, 512], BF16) for i in range(2)]
            sq = k.sb(es, "sq", [128, KC, 512], BF16)
            rstd = k.sb(es, "rstd", [128, 512], F32)
            rcol = k.sb(es, "rcol", [128, 4], F32)
            cs = [k.sb(es, f"cos{i}", [128, 512], F32) for i in range(2)]
            sn = [k.sb(es, f"sin{i}", [128, 512], F32) for i in range(2)]
            t1 = [k.sb(es, f"t1_{i}", [128, 512], F32) for i in range(2)]
            qf = [k.sb(es, f"qf_{i}", [128, 512], F32) for i in range(2)]
            qsq = [k.sb(es, f"qsq_{i}", [128, 512], BF16) for i in range(2)]
            rq = [k.sb(es, f"rq_{i}", [128, 512], F32) for i in range(2)]
            qo = [k.sb(es, f"qo_{i}", [128, 512], BF16) for i in range(4)]
            qfb = [k.sb(es, f"qfb_{i}", [128, 512], BF16) for i in range(2)]
            ra = [k.sb(es, f"ra_{i}", [128, 512], F32) for i in range(2)]
            vo = [k.sb(es, f"vo_{i}", [128, 896], BF16) for i in range(2)]
            xv = xT_d.t.rearrange("(kc p) s -> p kc s", p=128)
            nq = 0
            nv = 0

            def load_x(t):
                xb = xbs[t % 2]
                for h in range(4):
                    k.dma("pool", xb[:, h * 4:(h + 1) * 4, :], xv[:, h * 4:(h + 1) * 4, t * 512:(t + 1) * 512],
                          writes=[xb], semres=xb)

            load_x(0)
            for t in range(NT):
                xb = xbs[t % 2]
                if t + 1 < NT:
                    load_x(t + 1)
                c0 = t * 512
                k.dma("sp", cs[t % 2][:, :], cos_d[:, c0:c0 + 512], writes=[cs[t % 2]], semres=cs[t % 2])
                k.dma("sp", sn[t % 2][:, :], sin_d[:, c0:c0 + 512], writes=[sn[t % 2]], semres=sn[t % 2])
                for h in range(2):
                    k.act(actf(sq[:, h * 8:(h + 1) * 8, :], xb[:, h * 8:(h + 1) * 8, :], AF.Square), reads=[xb], writes=[sq.r(h)])
                for kc in range(KC):
                    k.pe(mm(psA[0][:, :], ones_b[:, :], sq[:, kc, :], kc == 0, kc == KC - 1),
                         reads=[sq.r(kc // 8), ones_b], writes=[psA[0]])
                k.act(actf(rstd[:, :], psA[0][:, :], AF.Sqrt, bias=epsr[:, :], scale=1.0 / D), reads=[psA[0], epsr], writes=[rstd])
                k.dve(recip(rstd[:, :], rstd[:, :]), reads=[rstd], writes=[rstd])
                for s4 in range(4):
                    k.pe(lambda e, s4=s4: e.transpose(psA[1][:, s4 * 128:(s4 + 1) * 128], rstd[:, s4 * 128:(s4 + 1) * 128], idf[:, :]),
                         reads=[rstd, idf], writes=[psA[1]])
                k.dve(cp(rcol[:, :], psA[1][:, :].rearrange("p (s n) -> p s n", n=128)[:, :, 0]), reads=[psA[1]], writes=[rcol])
                for f in range(8):
                    pb = psA[2 + (f % 2)]
                    for kc in range(KC):
                        k.pe(mm(pb[:, :], wb[:, kc, f * 128:(f + 1) * 128], xb[:, kc, :], kc == 0, kc == KC - 1),
                             reads=[wb, xb], writes=[pb])
                    a = nq % 2
                    nq += 1
                    k.dve(tt(t1[a][:, :], pb[:, :], rstd[:, :], ALU.mult), reads=[pb, rstd], writes=[t1[a]])
                    if f < 6:
                        gcol = qg if f % 2 == 0 else kg
                        k.act(actf(qsq[a][:, :], t1[a][:, :], AF.Square, bias=b1col[:, f:f + 1]), reads=[t1[a], b1col], writes=[qsq[a]])
                        k.act(actf(qf[a][:, :], t1[a][:, :], AF.Identity, bias=b1col[:, f:f + 1]), reads=[t1[a], b1col], writes=[qf[a]])
                        pn = psA[4 + a]
                        k.pe(mm(pn[:, :], ones_b[:, :], qsq[a][:, :], True, True), reads=[qsq[a], ones_b], writes=[pn])
                        k.act(actf(rq[a][:, :], pn[:, :], AF.Sqrt, bias=epsr[:, :], scale=1.0 / 128), reads=[pn, epsr], writes=[rq[a]])
                        k.dve(recip(rq[a][:, :], rq[a][:, :]), reads=[rq[a]], writes=[rq[a]])
                        o = qo[nq % 4]
                        k.dve(stt(o[:, :], qf[a][:, :], gcol[:, 0:1], rq[a][:, :], ALU.mult, ALU.mult), reads=[qf[a], gcol, rq[a]], writes=[o])
                        k.dma("sp", qk_s.t[f, :, c0:c0 + 512], o[:, :], reads=[o], writes=[qk_s.r((f, t))], semres=o)
                    else:
                        sc = 1.0 if f == 6 else 128.0 ** -0.5
                        bcol = b1col[:, 6:7] if f == 6 else b1k[:, 0:1]
                        bres = b1col if f == 6 else b1k
                        k.act(actf(qf[a][:, :], t1[a][:, :], AF.Identity, bias=bcol, scale=sc), reads=[t1[a], bres], writes=[qf[a]])
                        k.act(actf(qfb[a][:, :], t1[a][:, :], AF.Identity, bias=bcol, scale=sc), reads=[t1[a], bres], writes=[qfb[a]])
                        pn = psA[4 + a]
                        k.pe(mm(pn[:, :], rmb[:, :], qfb[a][:, :], True, True), reads=[qfb[a], rmb], writes=[pn])
                        k.dve(tt(ra[a][:, :], qf[a][:, :], cs[t % 2][:, :], ALU.mult), reads=[qf[a], cs[t % 2]], writes=[ra[a]])
                        k.dve(tt(rq[a][:, :], pn[:, :], sn[t % 2][:, :], ALU.mult), reads=[pn, sn[t % 2]], writes=[rq[a]])
                        o = qo[nq % 4]
                        k.pool(tt(o[:, :], ra[a][:, :], rq[a][:, :], ALU.add), reads=[ra[a], rq[a]], writes=[o])
                        k.dma("sp", qk_s.t[f, :, c0:c0 + 512], o[:, :], reads=[o], writes=[qk_s.r((f, t))], semres=o)
                for s4 in range(4):
                    v = vo[nv % 2]
                    nv += 1
                    for j in range(2):
                        pb = psA[6 + j]
                        for kc in range(KC):
                            k.pe(mm(pb[:, 0:448], xb[:, kc, s4 * 128:(s4 + 1) * 128], wb[:, kc, 1024 + j * 448:1024 + (j + 1) * 448],
                                    kc == 0, kc == KC - 1), reads=[wb, xb], writes=[pb])
                        k.dve(stt(v[:, j * 448:(j + 1) * 448], pb[:, 0:448], rcol[:, s4:s4 + 1], b1bc[:, j * 448:(j + 1) * 448], ALU.mult, ALU.add),
                              reads=[pb, rcol, b1bc], writes=[v.r(j)])
                    r0 = c0 + s4 * 128
                    k.dma("sp", vt_s.t[r0:r0 + 128, :], v[:, :], reads=[v.r(0), v.r(1)], writes=[vt_s.r((t, s4))], semres=v)
        k.barrier()
        if debug == "A":
            k.finish()
            return nc

        with ExitStack() as es:
            psS = [k.ps(es, f"psS{i}", [128, 512], F32) for i in range(2)]
            psO = [k.ps(es, f"psO{i}", [128, 512], F32) for i in range(2)]
            psL = [k.ps(es, f"psL{i}", [128, 512], F32) for i in range(2)]
            accO = k.sb(es, "accO", [128, S], F32)
            accL = k.sb(es, "accL", [128, S], F32)
            idx = k.sb(es, "idx", [128, 3 * 256], F32)
            negm = k.sb(es, "negm", [128, 256], F32)
            rb = k.sb(es, "rb", [128, 96], F32)
            k.dma("sp", idx[:, :], idx_d[:, :], writes=[idx], semres=idx)
            k.dma("sp", negm[:, :], neg_d[:, :], writes=[negm], semres=negm)
            k.dma("sp", rb[:, :], rb_d.t[0:1, :].partition_broadcast(128), writes=[rb], semres=rb)
            mbs = [k.sb(es, f"mb{g}", [128, 256], F32) for g in range(3)]
            tmpm = k.sb(es, "tmpm", [128, 256], F32)
            for g in range(3):
                k.dve(cp(mbs[g][:, :], negm[:, :]), reads=[negm], writes=[mbs[g]])
                for b in range(32):
                    k.dve(ts(tmpm[:, :], idx[:, g * 256:(g + 1) * 256], float(b), ALU.is_equal, rb[:, g * 32 + b:g * 32 + b + 1], ALU.mult),
                          reads=[idx, rb], writes=[tmpm])
                    k.dve(tt(mbs[g][:, :], mbs[g][:, :], tmpm[:, :], ALU.add), reads=[tmpm, mbs[g]], writes=[mbs[g]])
            qm = [k.sb(es, f"qm{i}", [128, 2048], BF16) for i in range(2)]
            km = [k.sb(es, f"km{i}", [128, 2048], BF16) for i in range(3)]
            vm = [k.sb(es, f"vm{i}", [128, 16, 128], BF16) for i in range(3)]
            sadd = [k.sb(es, f"sadd{i}", [128, 256], F32) for i in range(2)]
            pT = [k.sb(es, f"pT{i}", [128, 256], BF16) for i in range(3)]
            nld = 0
            nblk = 0
            nbatch = 0
            for g in range(3):
                d = DILS[g]
                nsb = 16 // d
                for m in range(4):
                    q = qm[nld % 2]
                    kk = km[nld % 3]
                    vv = vm[nld % 3]
                    kprev = km[(nld - 1) % 3]
                    vprev = vm[(nld - 1) % 3]
                    nld += 1
                    t0 = m * 2048
                    tiles = [(2 * g, tq) for tq in range(4 * m, 4 * m + 4)]
                    k.dma("sp", q[:, :], qk_s.t[2 * g, :, t0:t0 + 2048], reads=[qk_s.r((2 * g, tq)) for tq in range(4 * m, 4 * m + 4)],
                          writes=[q], semres=q)
                    k.dma("sp", kk[:, :], qk_s.t[2 * g + 1, :, t0:t0 + 2048],
                          reads=[qk_s.r((2 * g + 1, tq)) for tq in range(4 * m, 4 * m + 4)], writes=[kk], semres=kk)
                    vrd = [vt_s.r((tq, s4)) for tq in range(4 * m, 4 * m + 4) for s4 in range(4)]
                    if d == 1:
                        k.dma("sp", vv[:, :, :], vt_s.t[t0:t0 + 2048, g * 128:(g + 1) * 128].rearrange("(n c) e -> c n e", c=128),
                              reads=vrd, writes=[vv], semres=vv)
                    else:
                        for nl_ in range(nsb):
                            ta = t0 + nl_ * 128 * d
                            k.dma("sp", vv[:, nl_ * d:(nl_ + 1) * d, :],
                                  vt_s.t[ta:ta + 128 * d, g * 128:(g + 1) * 128].rearrange("(c r) e -> c r e", r=d),
                                  reads=vrd, writes=[vv], semres=vv)
                    for jb in range(4):
                        bo = psO[nbatch % 2]
                        bl = psL[nbatch % 2]
                        nbatch += 1
                        for ji in range(4):
                            j = jb * 4 + ji
                            nl, r = j // d, j % d
                            first = (m == 0 and nl == 0)
                            dsl = lambda st_: slice(st_, st_ + 127 * d + 1, d)
                            cols = dsl(nl * 128 * d + r)
                            if nl > 0:
                                pk, pv = kk, vv
                                pcols = dsl((nl - 1) * 128 * d + r)
                                pj = (nl - 1) * d + r
                            else:
                                pk, pv = kprev, vprev
                                pcols = dsl((nsb - 1) * 128 * d + r)
                                pj = (nsb - 1) * d + r
                            ps = psS[nblk % 2]
                            sa = sadd[nblk % 2]
                            p = pT[nblk % 3]
                            nblk += 1
                            qa = q[:, cols]
                            if not first:
                                k.pe(mm(ps[:, 0:128], pk[:, pcols], qa, True, True), reads=[pk, q], writes=[ps])
                            k.pe(mm(ps[:, 128:256], kk[:, cols], qa, True, True), reads=[kk, q], writes=[ps])
                            lo = 128 if first else 0
                            k.dve(tt(sa[:, lo:256], ps[:, lo:256], mbs[g][:, lo:256], ALU.add), reads=[ps, mbs[g]], writes=[sa])
                            k.act(actf(p[:, lo:256], sa[:, lo:256], AF.Exp), reads=[sa], writes=[p])
                            osl = bo[:, ji * 128:(ji + 1) * 128]
                            lsl = bl[:, ji * 128:(ji + 1) * 128]
                            if not first:
                                k.pe(mm(osl, pv[:, pj, :], p[:, 0:128], True, False), reads=[pv, p], writes=[bo])
                                k.pe(mm(lsl, ones_b[:, :], p[:, 0:128], True, False), reads=[ones_b, p], writes=[bl])
                            k.pe(mm(osl, vv[:, j, :], p[:, 128:256], first, True), reads=[vv, p], writes=[bo])
                            k.pe(mm(lsl, ones_b[:, :], p[:, 128:256], first, True), reads=[ones_b, p], writes=[bl])
                        j0 = jb * 4
                        nl0, r0 = j0 // d, j0 % d
                        if d == 1:
                            dst = slice(t0 + j0 * 128, t0 + j0 * 128 + 512)
                            dO = accO[:, dst]
                            dL = accL[:, dst]
                            sO = bo[:, :]
                            sL = bl[:, :]
                        else:
                            base = t0 + nl0 * 128 * d
                            dO = accO[:, base:base + 128 * d].rearrange("p (a r) -> p r a", r=d)[:, r0:r0 + 4, :]
                            dL = accL[:, base:base + 128 * d].rearrange("p (a r) -> p r a", r=d)[:, r0:r0 + 4, :]
                            sO = bo[:, :].rearrange("p (r a) -> p r a", a=128)
                            sL = bl[:, :].rearrange("p (r a) -> p r a", a=128)
                        if g == 0:
                            k.dve(cp(dO, sO), reads=[bo], writes=[accO.r(m)])
                            k.act(lambda e, dL=dL, sL=sL: e.copy(out=dL, in_=sL), reads=[bl], writes=[accL.r(m)])
                        else:
                            k.dve(tt(dO, sO, dO, ALU.add), reads=[bo, accO.r(m)], writes=[accO.r(m)])
                            k.dve(tt(dL, sL, dL, ALU.add), reads=[bl, accL.r(m)], writes=[accL.r(m)])
            yo = [k.sb(es, f"yo{i}", [128, 2048], BF16) for i in range(2)]
            for m in range(4):
                sl = slice(m * 2048, (m + 1) * 2048)
                k.dve(recip(accL[:, sl], accL[:, sl]), reads=[accL.r(m)], writes=[accL.r(m)])
                k.dve(tt(yo[m % 2][:, :], accO[:, sl], accL[:, sl], ALU.mult), reads=[accO.r(m), accL.r(m)], writes=[yo[m % 2]])
                k.dma("sp", yT_d.t[0:128, sl], yo[m % 2][:, :], reads=[yo[m % 2]], writes=[yT_d.r(("a", m))], semres=yo[m % 2])
        k.barrier()
        if debug == "B":
            k.finish()
            return nc

        with ExitStack() as es:
            psAT = [k.ps(es, f"psAT{i}", [128, 512], F32) for i in range(2)]
            psR = [k.ps(es, f"psR{i}", [128, 512], F32) for i in range(2)]
            psU = k.ps(es, "psU", [128, 512], F32)
            psK = k.ps(es, "psK", [128, 1024], BF16)
            psY = [k.ps(es, f"psY{i}", [128, 1024], BF16) for i in range(2)]
            dmat = k.sb(es, "dmat", [128, 128], F32)
            xib = k.sb(es, "xib", [128, 128], F32)
            zeta = k.sb(es, "zeta", [128, 1], F32)
            gch = k.sb(es, "gch", [128, 1], F32)
            gng = k.sb(es, "gng", [128, 256], F32)
            epsg = k.sb(es, "epsg", [128, 1], F32)
            k.dma("sp", dmat[:, :], dmat_d[:, :], writes=[dmat], semres=dmat)
            k.dma("sp", xib[:, :], xi_d[:, :], writes=[xib], semres=xib)
            k.dma("sp", zeta[:, :], zeta_d[:, :], writes=[zeta], semres=zeta)
            k.dma("sp", gch[:, :], gch_d[:, :], writes=[gch], semres=gch)
            k.dma("sp", gng[:, :], gng_d.t[0:1, :].partition_broadcast(128), writes=[gng], semres=gng)
            k.dve(lambda e: e.memset(epsg[:, :], GN_EPS), writes=[epsg])
            qm = [k.sb(es, f"rqm{i}", [128, 2048], BF16) for i in range(2)]
            km = [k.sb(es, f"rkm{i}", [128, 2048], BF16) for i in range(2)]
            vm = [k.sb(es, f"rvm{i}", [128, 16, 256], BF16) for i in range(2)]
            gm = [k.sb(es, f"rgm{i}", [128, 16, 256], BF16) for i in range(2)]
            St = k.sb(es, "St", [128, 256], F32)
            Sb = k.sb(es, "Sb", [128, 256], BF16)
            atd = [k.sb(es, f"atd{i}", [128, 128], BF16) for i in range(2)]
            qx = [k.sb(es, f"qx{i}", [128, 128], BF16) for i in range(2)]
            kz = [k.sb(es, f"kz{i}", [128, 128], BF16) for i in range(2)]
            s1 = [k.sb(es, f"s1_{i}", [128, 1], F32) for i in range(2)]
            ssq = [k.sb(es, f"ssq_{i}", [128, 1], F32) for i in range(2)]
            junk = [k.sb(es, f"junk{i}", [128, 256], F32) for i in range(2)]
            yn = [k.sb(es, f"yn{i}", [128, 256], F32) for i in range(2)]
            sg = [k.sb(es, f"sg{i}", [128, 256], F32) for i in range(2)]
            yb = [k.sb(es, f"yb{i}", [128, 256], BF16) for i in range(2)]
            ybT = [k.sb(es, f"ybT{i}", [128, 2, 2048], BF16) for i in range(2)]
            k.dve(lambda e: e.memset(St[:, :], 0.0), writes=[St])
            for m in range(4):
                t0 = m * 2048
                q, kk, vv, gg_ = qm[m % 2], km[m % 2], vm[m % 2], gm[m % 2]
                k.dma("sp", q[:, :], qk_s.t[6, :, t0:t0 + 2048], reads=[qk_s.r((6, tq)) for tq in range(4 * m, 4 * m + 4)], writes=[q], semres=q)
                k.dma("sp", kk[:, :], qk_s.t[7, :, t0:t0 + 2048], reads=[qk_s.r((7, tq)) for tq in range(4 * m, 4 * m + 4)], writes=[kk], semres=kk)
                vtr = [vt_s.r((tq, s4)) for tq in range(4 * m, 4 * m + 4) for s4 in range(4)]
                k.dma("sp", vv[:, :, :], vt_s.t[t0:t0 + 2048, 384:640].rearrange("(n c) e -> c n e", c=128), reads=vtr, writes=[vv], semres=vv)
                k.dma("sp", gg_[:, :, :], vt_s.t[t0:t0 + 2048, 640:896].rearrange("(n c) e -> c n e", c=128), reads=vtr, writes=[gg_], semres=gg_)
                yT = ybT[m % 2]
                for n in range(16):
                    gn = m * 16 + n
                    a = gn % 2
                    cols = slice(n * 128, (n + 1) * 128)
                    k.pe(mm(psAT[a][:, 0:128], kk[:, cols], q[:, cols], True, True), reads=[kk, q], writes=[psAT[a]])
                    k.dve(tt(atd[a][:, :], psAT[a][:, 0:128], dmat[:, :], ALU.mult), reads=[psAT[a], dmat], writes=[atd[a]])
                    k.pool(tt(qx[a][:, :], q[:, cols], xib[:, :], ALU.mult), reads=[q, xib], writes=[qx[a]])
                    k.pe(mm(psR[a][:, 0:256], atd[a][:, :], vv[:, n, :], True, gn == 0), reads=[atd[a], vv], writes=[psR[a]])
                    if gn > 0:
                        k.pe(mm(psR[a][:, 0:256], qx[a][:, :], Sb[:, :], False, True), reads=[qx[a], Sb], writes=[psR[a]])
                    k.pe(lambda e, a=a, cols=cols, kk=kk: e.transpose(psK[:, 0:128], kk[:, cols], idb[:, :]), reads=[kk, idb], writes=[psK])
                    k.act(actf(kz[a][:, :], psK[:, 0:128], AF.Copy, scale=zeta[:, 0:1]), reads=[psK, zeta], writes=[kz[a]])
                    k.pe(mm(psU[:, 0:256], kz[a][:, :], vv[:, n, :], True, True), reads=[kz[a], vv], writes=[psU])
                    k.dve(stt(St[:, :], St[:, :], gch[:, 0:1], psU[:, 0:256], ALU.mult, ALU.add), reads=[St, gch, psU], writes=[St])
                    k.act(lambda e: e.copy(out=Sb[:, :], in_=St[:, :]), reads=[St], writes=[Sb])
                    k.dve(lambda e, a=a: e.tensor_reduce(out=s1[a][:, :], in_=psR[a][:, 0:256], op=ALU.add, axis=AX.X), reads=[psR[a]], writes=[s1[a]])
                    k.dve(ts(s1[a][:, :], s1[a][:, :], -1.0 / 256, ALU.mult), reads=[s1[a]], writes=[s1[a]])
                    k.act(actf(junk[a][:, :], psR[a][:, 0:256], AF.Square, bias=s1[a][:, 0:1], accum_out=ssq[a][:, :]), reads=[psR[a], s1[a]], writes=[junk[a], ssq[a]])
                    k.act(actf(ssq[a][:, :], ssq[a][:, :], AF.Sqrt, bias=epsg[:, :], scale=1.0 / 256), reads=[ssq[a], epsg], writes=[ssq[a]])
                    k.dve(recip(ssq[a][:, :], ssq[a][:, :]), reads=[ssq[a]], writes=[ssq[a]])
                    k.dve(ts(yn[a][:, :], psR[a][:, 0:256], s1[a][:, 0:1], ALU.add, ssq[a][:, 0:1], ALU.mult), reads=[psR[a], s1[a], ssq[a]], writes=[yn[a]])
                    k.act(actf(sg[a][:, :], gg_[:, n, :], AF.Silu), reads=[gg_], writes=[sg[a]])
                    k.pool(tt(yn[a][:, :], yn[a][:, :], gng[:, :], ALU.mult), reads=[yn[a], gng], writes=[yn[a]])
                    k.pool(tt(yb[a][:, :], yn[a][:, :], sg[a][:, :], ALU.mult), reads=[yn[a], sg[a]], writes=[yb[a]])
                    for h in range(2):
                        k.pe(lambda e, a=a, h=h: e.transpose(psY[a][:, h * 128:(h + 1) * 128], yb[a][:, h * 128:(h + 1) * 128], idb[:, :]),
                             reads=[yb[a], idb], writes=[psY[a]])
                    k.dve(cp(yT[:, :, cols], psY[a][:, 0:256].rearrange("p (h t) -> p h t", h=2)), reads=[psY[a]], writes=[yT])
                for h in range(2):
                    k.dma("sp", yT_d.t[128 + h * 128:256 + h * 128, t0:t0 + 2048], yT[:, h, :], reads=[yT], writes=[yT_d.r(("b", m, h))], semres=yT)
        k.finish()
    return nc


def _t5_bucket(dist):
    nb, md = 32, 2048
    max_exact = nb // 2
    safe = np.maximum(dist, 1).astype(np.float32)
    large = max_exact + (np.log(safe / max_exact) / np.log(md / max_exact) * (nb - max_exact)).astype(np.int32)
    return np.where(dist < max_exact, dist, np.minimum(large, nb - 1)).astype(np.int32)


def static_tables():
    cc = np.arange(128)[:, None]
    aa = np.arange(128)[None, :]
    idx = np.zeros((128, 3, 2, 128), np.float32)
    neg = np.zeros((128, 2, 128), np.float32)
    for g, d in enumerate(DILS):
        d_prev = 128 + aa - cc
        d_cur = aa - cc
        v_prev = d_prev <= 128
        v_cur = d_cur >= 0
        idx[:, g, 0, :] = np.where(v_prev, _t5_bucket(np.maximum(d_prev, 0) * d), -1)
        idx[:, g, 1, :] = np.where(v_cur, _t5_bucket(np.maximum(d_cur, 0) * d), -1)
        neg[:, 0, :] = np.where(v_prev, 0.0, NEG)
        neg[:, 1, :] = np.where(v_cur, 0.0, NEG)
    inv = (10000.0 ** (-np.arange(0, 128, 2, dtype=np.float32) / np.float32(128))).astype(np.float32)
    ang = (np.arange(S, dtype=np.float32)[:, None] * inv[None, :]).astype(np.float32)
    cosT = np.concatenate([np.cos(ang).T, np.cos(ang).T], axis=0).astype(np.float32)
    sinT = np.concatenate([np.sin(ang).T, np.sin(ang).T], axis=0).astype(np.float32)
    rm = np.zeros((128, 128), np.float32)
    for m in range(64):
        rm[m + 64, m] = -1.0
        rm[m, m + 64] = 1.0
    ident = np.eye(128, dtype=np.float32)
    ret = []
    ii = np.arange(128, dtype=np.float32)
    for h in range(8):
        log_g = np.log1p(-np.exp2(np.float32(-5.0 - h))).astype(np.float32)
        diff = ii[None, :] - ii[:, None]
        dmatT = np.where(diff >= 0, np.exp(log_g * np.maximum(diff, 0.0)), 0.0).astype(np.float32)
        xi = np.exp(log_g * (ii + 1.0)).astype(np.float32)
        zeta = np.exp(log_g * (127.0 - ii)).astype(np.float32)
        gchunk = np.float32(np.exp(log_g * 128.0))
        ret.append(dict(dmatT=dmatT, xibc=np.tile(xi[None, :], (128, 1)).astype(np.float32),
                        zetacol=zeta[:, None].copy(), gchunk=np.full((128, 1), gchunk, np.float32)))
    zpow = np.zeros((128, 8, 56), np.float32)
    for h in range(8):
        log_g = np.log1p(-np.exp2(np.float32(-5.0 - h))).astype(np.float64)
        zeta64 = np.exp(log_g * (127.0 - ii.astype(np.float64)))
        for n in range(56):
            zpow[:, h, n] = (zeta64 * np.exp(log_g * 128.0 * (55 - n))).astype(np.float32)
    return dict(idxT=idx.reshape(128, 768), negT=neg.reshape(128, 256), cosT=cosT, sinT=sinT, rmat=rm, identf=ident, ret=ret,
                zpow=np.ascontiguousarray(zpow.reshape(128, 448)))


def pk16(v):
    return np.ascontiguousarray(np.asarray(v, np.float32).reshape(-1, 128).T)


def l1_inputs(inp, st):
    x = np.asarray(inp["x"], np.float32)[0]
    xT = np.ascontiguousarray(x.T)
    w_in = np.asarray(inp["w_in"], np.float32)[0]
    w_ada = np.asarray(inp["w_ada"], np.float32)[0]
    b_ada = np.asarray(inp["b_ada"], np.float32)[0]
    rel_bias = np.asarray(inp["rel_bias"], np.float32)
    wada1 = np.ascontiguousarray(w_ada[:, 0:4096])
    bada1 = pk16(b_ada[0:4096])
    maps = []
    for c in range(NCORES):
        cols = []
        for g in range(3):
            h = g * 8 + c
            cols += [np.arange(h * 128, (h + 1) * 128), 3072 + np.arange(h * 128, (h + 1) * 128)]
        cols += [9216 + np.arange(c * 128, (c + 1) * 128), 10240 + np.arange(c * 128, (c + 1) * 128)]
        for g in range(3):
            h = g * 8 + c
            cols += [6144 + np.arange(h * 128, (h + 1) * 128)]
        cols += [11264 + np.arange(c * 256, (c + 1) * 256), 13312 + np.arange(c * 256, (c + 1) * 256)]
        cols = np.concatenate(cols)
        rt = st["ret"][c]
        maps.append(dict(
            xT=xT, c_pk=pk16(inp["c"][0]), wada1=wada1, bada1=bada1, ln1_pk=pk16(inp["ln1_g"][0]),
            win=np.ascontiguousarray(w_in[:, cols]),
            qg=np.asarray(inp["q_norm_g"], np.float32)[0][:, None].copy(),
            kg=np.asarray(inp["k_norm_g"], np.float32)[0][:, None].copy(),
            rb=np.ascontiguousarray(np.stack([rel_bias[:, g * 8 + c] for g in range(3)]).reshape(1, 96)),
            idxT=st["idxT"], negT=st["negT"], cosT=st["cosT"], sinT=st["sinT"], rmat=st["rmat"], identf=st["identf"],
            dmatT=rt["dmatT"], xibc=rt["xibc"], zetacol=rt["zetacol"], gchunk=rt["gchunk"],
            gng=np.asarray(inp["ret_gn_g"], np.float32)[0][c * 256:(c + 1) * 256][None, :].copy(),
        ))
    return maps


def build_l2(n_exp=65, stop=None, nc=None, k=None, shared=None):
    if nc is None:
        nc = bass.Bass("TRN2", target_bir_lowering=False)
        k = K(nc)
    shared = shared or {}
    EI, EO = "ExternalInput", "ExternalOutput"
    TS = S // NCORES
    xT_d = k.dram("xTs", [D, TS], F32, EI)
    x_d = k.dram("xs", [TS, D], F32, EI)
    c_d = shared.get("c_pk") or k.dram("c_pk", [128, KC], F32, EI)
    wada_d = shared.get("wada") or k.dram("wada", [D, 12288], F32, EI)
    bada1_d = shared.get("bada1") or k.dram("bada1", [128, 32], F32, EI)
    badar_d = k.dram("badar", [1, 8192], F32, EI)
    ln1_d = shared.get("ln1_pk") or k.dram("ln1_pk", [128, KC], F32, EI)
    ln2_d = k.dram("ln2_row", [1, D], F32, EI)
    wg_d = k.dram("wgates", [D, 4096], F32, EI)
    pa_d = k.dram("pa", [1024, D], F32, EI)
    pb_d = k.dram("pb", [D, D], F32, EI)
    wo_d = k.dram("wo", [D, D], F32, EI)
    yT_d = shared.get("ys") or k.dram("yTs", [3072, TS], BF16, EI)
    rw_d = k.dram("rw", [D, 64], F32, EI)
    rbias_d = k.dram("rbias", [1, 64], F32, EI)
    if n_exp > 0:
        wge_d = k.dram("wge", [64, D, 512], F32, EI)
        wue_d = k.dram("wue", [64, D, 512], F32, EI)
        wde_d = k.dram("wde", [64, 512, D], F32, EI)
        wgs_d = k.dram("wgs", [D, 512], F32, EI)
        wus_d = k.dram("wus", [D, 512], F32, EI)
        wds_d = k.dram("wds", [512, D], F32, EI)
    idf_d = shared.get("identf") or k.dram("identf", [128, 128], F32, EI)
    out_d = k.dram("out", [TS, D], F32, EO)

    def sbr(name, shape, dt):
        k.nm += 1
        t = nc.alloc_sbuf_tensor(f"r{k.nm}_{name}", list(shape), dt, side="right")
        return Tl(k, t, name)

    with ExitStack() as es0:
        ones_b = k.sb(es0, "ones_b", [128, 128], BF16)
        idf = k.sb(es0, "idf", [128, 128], F32)
        epsr = k.sb(es0, "epsr", [128, 1], F32)
        k.dve(lambda e: e.memset(ones_b[:, :], 1.0), writes=[ones_b])
        k.dve(lambda e: e.memset(epsr[:, :], RMS_EPS), writes=[epsr])
        k.dma("sp", idf[:, :], idf_d[:, :], writes=[idf], semres=idf)
        g2bc = sbr("g2bc", [128, D], F32)

        with ExitStack() as esm:
            g1bc = k.sb(esm, "g1bc", [128, D], F32)
            gg2bc = k.sb(esm, "gg2bc", [128, D], F32)
            sh2bc = k.sb(esm, "sh2bc", [128, D], F32)
            gg = k.sb(esm, "gg", [128, KC], F32)
            sh1b = k.sb(esm, "sh1b", [128, KC], BF16)
            rstd = k.sb(esm, "rstd", [128, TS], F32)
            with ExitStack() as es:
                psA = [k.ps(es, f"psA{i}", [128, 512], F32) for i in range(4)]
                cb = emit_silu_c(k, es, c_d)
                mod1 = emit_mod_cols(k, es, cb, wada_d, bada1_d, 32, "mod1", psA[0])
                ln1 = k.sb(es, "ln1", [128, KC], F32)
                k.dma("sp", ln1[:, :], ln1_d[:, :], writes=[ln1], semres=ln1)
                k.dve(stt(gg[:, :], mod1[:, 16:32], 1.0, ln1[:, :], ALU.add, ALU.mult), reads=[mod1, ln1], writes=[gg])
                k.dve(cp(sh1b[:, :], mod1[:, 0:16]), reads=[mod1], writes=[sh1b])
                cbc = k.sb(es, "cbc", [128, KC, 128], BF16)
                k.dve(cp(cbc[:, :, :], cb[:, :].unsqueeze(2).to_broadcast([128, KC, 128])), reads=[cb], writes=[cbc])
                wch = [k.sb(es, f"wch{i}", [128, KC, 512], BF16) for i in range(2)]
                bch = [k.sb(es, f"bch{i}", [128, 512], F32) for i in range(2)]
                ln2bc = k.sb(es, "ln2bc", [128, D], F32)
                k.dma("sp", ln2bc[:, :], ln2_d.t[0:1, :].partition_broadcast(128), writes=[ln2bc], semres=ln2bc)
                wv = wada_d.t.rearrange("(kc p) n -> p kc n", p=128)
                dsts = [g1bc, sh2bc, gg2bc, g2bc]
                for ch in range(16):
                    w = wch[ch % 2]
                    bb = bch[ch % 2]
                    c0 = 4096 + ch * 512
                    for h in range(2):
                        k.dma("pool", w[:, h * 8:(h + 1) * 8, :], wv[:, h * 8:(h + 1) * 8, c0:c0 + 512], writes=[w], semres=w)
                    k.dma("sp", bb[:, :], badar_d.t[0:1, ch * 512:(ch + 1) * 512].partition_broadcast(128), writes=[bb], semres=bb)
                    pb_ = psA[1 + ch % 2]
                    for kc in range(KC):
                        k.pe(mm(pb_[:, :], cbc[:, kc, :], w[:, kc, :], kc == 0, kc == KC - 1), reads=[cbc, w], writes=[pb_])
                    dst = dsts[ch // 4]
                    k.dve(tt(dst[:, (ch % 4) * 512:(ch % 4 + 1) * 512], pb_[:, :], bb[:, :], ALU.add), reads=[pb_, bb], writes=[dst])
                k.dve(stt(gg2bc[:, :], gg2bc[:, :], 1.0, ln2bc[:, :], ALU.add, ALU.mult), reads=[gg2bc, ln2bc], writes=[gg2bc])
                k.barrier()
                if stop == "a":
                    k.finish()
                    return nc

            with ExitStack() as esg:
                mergedT = k.sb(esg, "mergedT", [128, KC, TS], BF16)
                with ExitStack() as es:
                    psG = [k.ps(es, f"psG{i}", [128, 512], F32) for i in range(4)]
                    psY = [k.ps(es, f"psYp{i}", [128, 512], F32) for i in range(2)]
                    psB = k.ps(es, "psB", [128, 512], F32)
                    psN = k.ps(es, "psN", [128, 512], F32)
                    xg = k.sb(es, "xg", [128, KC, TS], BF16)
                    yT = k.sb(es, "yT", [128, 24, TS], BF16)
                    sq = k.sb(es, "sq", [128, 8, 512], BF16)
                    xv = xT_d.t.rearrange("(kc p) s -> p kc s", p=128)
                    for h in range(4):
                        k.dma("pool", xg[:, h * 4:(h + 1) * 4, :], xv[:, h * 4:(h + 1) * 4, :], writes=[xg], semres=xg)
                    for h in range(3):
                        k.dma("sp", yT[:, h * 8:(h + 1) * 8, :], yT_d.t[h * 1024:(h + 1) * 1024, :].rearrange("(kc p) s -> p kc s", p=128),
                              writes=[yT], semres=yT)
                    for th in range(2):
                        for h in range(2):
                            k.act(actf(sq[:, :, :], xg[:, h * 8:(h + 1) * 8, th * 512:(th + 1) * 512], AF.Square), reads=[xg], writes=[sq])
                            for kc in range(8):
                                k.pe(mm(psN[:, :], ones_b[:, :], sq[:, kc, :], h == 0 and kc == 0, h == 1 and kc == 7), reads=[sq, ones_b], writes=[psN])
                        k.act(actf(rstd[:, th * 512:(th + 1) * 512], psN[:, :], AF.Sqrt, bias=epsr[:, :], scale=1.0 / D), reads=[psN, epsr], writes=[rstd])
                    k.dve(recip(rstd[:, :], rstd[:, :]), reads=[rstd], writes=[rstd])
                    for kc in range(KC):
                        k.dve(ts(xg[:, kc, :], xg[:, kc, :], gg[:, kc:kc + 1], ALU.mult), reads=[xg, gg], writes=[xg])
                    wga = [k.sb(es, f"wga{i}", [128, KC, 256], BF16) for i in range(1)]
                    wgb = [k.sb(es, f"wgb{i}", [128, KC, 256], BF16) for i in range(1)]
                    wpa = [k.sb(es, f"wpa{i}", [128, 8, 256], BF16) for i in range(1)]
                    wpb = [k.sb(es, f"wpb{i}", [128, KC, 256], BF16) for i in range(1)]
                    b1g = k.sb(es, "b1g", [128, 32], F32)
                    ta = [k.sb(es, f"ta{i}", [128, 512], F32) for i in range(2)]
                    tb = [k.sb(es, f"tb{i}", [128, 512], F32) for i in range(2)]
                    wgv = wg_d.t.rearrange("(kc p) n -> p kc n", p=128)
                    pav = pa_d.t.rearrange("(kc p) n -> p kc n", p=128)
                    pbv = pb_d.t.rearrange("(kc p) n -> p kc n", p=128)
                    ncnt = 0
                    for G in range(8):
                        i2 = 0
                        c0 = G * 256
                        k.dma("pool", wga[i2][:, :, :], wgv[:, :, c0:c0 + 256], writes=[wga[i2]], semres=wga[i2])
                        k.dma("pool", wgb[i2][:, :, :], wgv[:, :, 2048 + c0:2048 + c0 + 256], writes=[wgb[i2]], semres=wgb[i2])
                        k.dma("pool", wpa[i2][:, :, :], pav[:, :, c0:c0 + 256], writes=[wpa[i2]], semres=wpa[i2])
                        k.dma("pool", wpb[i2][:, :, :], pbv[:, :, c0:c0 + 256], writes=[wpb[i2]], semres=wpb[i2])
                        for ft in range(2):
                            f = G * 2 + ft
                            fs = slice(ft * 128, (ft + 1) * 128)
                            for kc in range(KC):
                                k.pe(mm(psB[:, 2 * f:2 * f + 1], wga[i2][:, kc, fs], sh1b[:, kc:kc + 1], kc == 0, kc == KC - 1), reads=[wga[i2], sh1b], writes=[psB])
                            for kc in range(KC):
                                k.pe(mm(psB[:, 2 * f + 1:2 * f + 2], wgb[i2][:, kc, fs], sh1b[:, kc:kc + 1], kc == 0, kc == KC - 1), reads=[wgb[i2], sh1b], writes=[psB])
                            k.dve(cp(b1g[:, 2 * f:2 * f + 2], psB[:, 2 * f:2 * f + 2]), reads=[psB], writes=[b1g])
                            for th in range(2):
                                tsl = slice(th * 512, (th + 1) * 512)
                                a = ncnt % 2
                                ncnt += 1
                                pga, pgb = psG[2 * a], psG[2 * a + 1]
                                for kc in range(KC):
                                    k.pe(mm(pga[:, :], wga[i2][:, kc, fs], xg[:, kc, tsl], kc == 0, kc == KC - 1), reads=[wga[i2], xg], writes=[pga])
                                for kc in range(KC):
                                    k.pe(mm(pgb[:, :], wgb[i2][:, kc, fs], xg[:, kc, tsl], kc == 0, kc == KC - 1), reads=[wgb[i2], xg], writes=[pgb])
                                k.dve(tt(ta[a][:, :], pga[:, :], rstd[:, tsl], ALU.mult), reads=[pga, rstd], writes=[ta[a]])
                                k.dve(tt(tb[a][:, :], pgb[:, :], rstd[:, tsl], ALU.mult), reads=[pgb, rstd], writes=[tb[a]])
                                k.act(actf(ta[a][:, :], ta[a][:, :], AF.Sigmoid, bias=b1g[:, 2 * f:2 * f + 1]), reads=[ta[a], b1g], writes=[ta[a]])
                                k.act(actf(tb[a][:, :], tb[a][:, :], AF.Sigmoid, bias=b1g[:, 2 * f + 1:2 * f + 2]), reads=[tb[a], b1g], writes=[tb[a]])
                                for kc in range(8):
                                    k.pe(mm(psY[0][:, :], wpa[i2][:, kc, fs], yT[:, kc, tsl], kc == 0, kc == 7), reads=[wpa[i2], yT], writes=[psY[0]])
                                k.dve(tt(ta[a][:, :], ta[a][:, :], psY[0][:, :], ALU.mult), reads=[ta[a], psY[0]], writes=[ta[a]])
                                for kc in range(KC):
                                    k.pe(mm(psY[1][:, :], wpb[i2][:, kc, fs], yT[:, 8 + kc, tsl], kc == 0, kc == KC - 1), reads=[wpb[i2], yT], writes=[psY[1]])
                                k.dve(tt(tb[a][:, :], tb[a][:, :], psY[1][:, :], ALU.mult), reads=[tb[a], psY[1]], writes=[tb[a]])
                                k.pool(tt(mergedT[:, f, tsl], ta[a][:, :], tb[a][:, :], ALU.add), reads=[ta[a], tb[a]], writes=[mergedT.r(th)])
                    k.barrier()
                    if stop == "b":
                        k.finish()
                        return nc

                acc = sbr("acc", [128, 8, D], F32)
                with ExitStack() as es:
                    psO = [k.ps(es, f"psO{i}", [128, 512], F32) for i in range(2)]
                    wo = [k.sb(es, f"wo{i}", [128, KC, 512], BF16) for i in range(2)]
                    tmp = [k.sb(es, f"tmpo{i}", [128, 512], F32) for i in range(2)]
                    wov = wo_d.t.rearrange("(kc p) n -> p kc n", p=128)
                    xv2 = x_d.t.rearrange("(t p) n -> p t n", p=128)
                    for tt_ in range(8):
                        k.dma("sp", acc[:, tt_, :], xv2[:, tt_, :], writes=[acc.r(tt_)], semres=acc.r(tt_))
                    no = 0
                    for n in range(4):
                        w = wo[n % 2]
                        for h in range(2):
                            k.dma("pool", w[:, h * 8:(h + 1) * 8, :], wov[:, h * 8:(h + 1) * 8, n * 512:(n + 1) * 512], writes=[w], semres=w)
                        for tt_ in range(8):
                            a = no % 2
                            no += 1
                            for kc in range(KC):
                                k.pe(mm(psO[a][:, :], mergedT[:, kc, tt_ * 128:(tt_ + 1) * 128], w[:, kc, :], kc == 0, kc == KC - 1),
                                     reads=[mergedT.r(tt_ // 4), w], writes=[psO[a]])
                            k.dve(tt(tmp[a][:, :], psO[a][:, :], g1bc[:, n * 512:(n + 1) * 512], ALU.mult), reads=[psO[a], g1bc], writes=[tmp[a]])
                            k.pool(tt(acc[:, tt_, n * 512:(n + 1) * 512], acc[:, tt_, n * 512:(n + 1) * 512], tmp[a][:, :], ALU.add),
                                   reads=[tmp[a], acc.r(tt_)], writes=[acc.r(tt_)])
                    k.barrier()
                    if stop == "c":
                        ov = out_d.t.rearrange("(t p) n -> p t n", p=128)
                        for tt_ in range(8):
                            k.dma("sp", ov[:, tt_, :], acc[:, tt_, :], reads=[acc.r(tt_)], writes=[out_d.r(tt_)], semres=acc.r(tt_))
                        k.finish()
                        return nc

            h2T = sbr("h2T", [128, KC, TS], BF16)
            Wc = sbr("Wc", [128, 8, 66], F32)
            with ExitStack() as es:
                psT = [k.ps(es, f"psT{i}", [128, 512], F32) for i in range(4)]
                psR = k.ps(es, "psRt", [128, 512], F32)
                rwf = k.sb(es, "rwf", [128, KC, 64], F32)
                rbb = k.sb(es, "rbb", [128, 64], F32)
                k.dma("sp", rwf[:, :, :], rw_d.t.rearrange("(kc p) e -> p kc e", p=128), writes=[rwf], semres=rwf)
                k.dma("sp", rbb[:, :], rbias_d.t[0:1, :].partition_broadcast(128), writes=[rbb], semres=rbb)
                rwhi = k.sb(es, "rwhi", [128, KC, 64], BF16)
                rwlo = k.sb(es, "rwlo", [128, KC, 64], BF16)
                h2lo = k.sb(es, "h2lo", [128, KC, 128], BF16)
                k.dve(cp(rwhi[:, :, :], rwf[:, :, :]), reads=[rwf], writes=[rwhi])
                k.dve(tt(rwlo[:, :, :], rwf[:, :, :], rwhi[:, :, :], ALU.subtract), reads=[rwf, rwhi], writes=[rwlo])
                h2 = [k.sb(es, f"h2_{i}", [128, D], F32) for i in range(2)]
                h2Tf = [k.sb(es, f"h2Tf{i}", [128, KC, 128], F32) for i in range(2)]
                junk = k.sb(es, "junk2", [128, D], BF16)
                ss = [k.sb(es, f"ss{i}", [128, 1], F32) for i in range(2)]
                sc_ = [k.sb(es, f"sc{i}", [128, 64], F32) for i in range(2)]
                sel = [k.sb(es, f"sel{i}", [128, 64], F32) for i in range(2)]
                eq = k.sb(es, "eq", [128, 64], F32)
                sel2 = k.sb(es, "sel2", [128, 64], F32)
                m1 = k.sb(es, "m1", [128, 8], F32)
                m2 = k.sb(es, "m2", [128, 8], F32)
                gs = k.sb(es, "gs", [128, 8], F32)
                mx = k.sb(es, "mx", [128, 8], F32)
                gmask = k.sb(es, "gmask", [128, 8], F32)
                selm = k.sb(es, "selm", [128, 64], F32)
                mx2 = k.sb(es, "mx2", [128, 8], F32)
                tw = k.sb(es, "tw", [128, 64], F32)
                wsum = k.sb(es, "wsum", [128, 1], F32)
                k.dve(lambda e: e.memset(Wc[:, :, :], 1.0), writes=[Wc])
                for tt_ in range(8):
                    a = tt_ % 2
                    x2 = acc[:, tt_, :]
                    k.act(actf(junk[:, :], x2, AF.Square, accum_out=ss[a][:, :]), reads=[acc.r(tt_)], writes=[junk, ss[a]])
                    k.act(actf(ss[a][:, :], ss[a][:, :], AF.Sqrt, bias=epsr[:, :], scale=1.0 / D), reads=[ss[a], epsr], writes=[ss[a]])
                    k.dve(recip(ss[a][:, :], ss[a][:, :]), reads=[ss[a]], writes=[ss[a]])
                    k.dve(stt(h2[a][:, :], x2, ss[a][:, 0:1], gg2bc[:, :], ALU.mult, ALU.mult), reads=[acc.r(tt_), ss[a], gg2bc], writes=[h2[a]])
                    k.pool(tt(h2[a][:, :], h2[a][:, :], sh2bc[:, :], ALU.add), reads=[h2[a], sh2bc], writes=[h2[a]])
                    if stop == "d1":
                        k.barrier()
                        k.finish()
                        return nc
                    for q4 in range(4):
                        pt = psT[q4]
                        for j in range(4):
                            kc = q4 * 4 + j
                            k.pe(lambda e, pt=pt, j=j, kc=kc, a=a: e.transpose(pt[:, j * 128:(j + 1) * 128], h2[a][:, kc * 128:(kc + 1) * 128], idf[:, :]),
                                 reads=[h2[a], idf], writes=[pt])
                        k.dve(cp(h2Tf[a][:, q4 * 4:(q4 + 1) * 4, :], pt[:, :].rearrange("p (j t) -> p j t", j=4)), reads=[pt], writes=[h2Tf[a]])
                        k.dve(cp(h2T[:, q4 * 4:(q4 + 1) * 4, tt_ * 128:(tt_ + 1) * 128], pt[:, :].rearrange("p (j t) -> p j t", j=4)),
                              reads=[pt], writes=[h2T.r(tt_)])
                    if stop == "d2":
                        k.barrier()
                        k.finish()
                        return nc
                    hi = h2T[:, :, tt_ * 128:(tt_ + 1) * 128]
                    k.dve(tt(h2lo[:, :, :], h2Tf[a][:, :, :], hi, ALU.subtract), reads=[h2Tf[a], h2T.r(tt_)], writes=[h2lo])
                    nmm = 0
                    for kc in range(KC):
                        for (l_, r_, lres) in ((hi[:, kc, :], rwhi[:, kc, :], h2T.r(tt_)), (h2lo[:, kc, :], rwhi[:, kc, :], h2lo), (hi[:, kc, :], rwlo[:, kc, :], h2T.r(tt_))):
                            k.pe(mm(psR[:, 0:64], l_, r_, nmm == 0, nmm == 3 * KC - 1), reads=[lres, rwhi, rwlo], writes=[psR])
                            nmm += 1
                    k.act(actf(sc_[a][:, :], psR[:, 0:64], AF.Sigmoid), reads=[psR], writes=[sc_[a]])
                    k.dve(tt(sel[a][:, :], sc_[a][:, :], rbb[:, :], ALU.add), reads=[sc_[a], rbb], writes=[sel[a]])
                    if stop == "d3":
                        k.barrier()
                        k.finish()
                        return nc
                    def red(out_ap, in_ap, op, rd, wr):
                        k.dve(lambda e: e.tensor_reduce(out=out_ap, in_=in_ap, axis=AX.X, op=op), reads=rd, writes=wr)

                    def kth_thr(work, n, kth, thr):
                        for _ in range(kth - 1):
                            red(thr[:, 0:1], work[:, 0:n], ALU.max, [work], [thr])
                            k.dve(ts(eq[:, 0:n], work[:, 0:n], thr[:, 0:1], ALU.is_ge, -2e9, ALU.mult), reads=[work, thr], writes=[eq])
                            k.dve(tt(work[:, 0:n], work[:, 0:n], eq[:, 0:n], ALU.add), reads=[work, eq], writes=[work])
                        red(thr[:, 0:1], work[:, 0:n], ALU.max, [work], [thr])

                    for g_ in range(8):
                        gsl = slice(g_ * 8, (g_ + 1) * 8)
                        red(m1[:, g_:g_ + 1], sel[a][:, gsl], ALU.max, [sel[a]], [m1])
                        k.dve(ts(sel2[:, gsl], sel[a][:, gsl], m1[:, g_:g_ + 1], ALU.is_equal, -1e9, ALU.mult), reads=[sel[a], m1], writes=[sel2])
                        k.dve(tt(sel2[:, gsl], sel2[:, gsl], sel[a][:, gsl], ALU.add), reads=[sel2, sel[a]], writes=[sel2])
                        red(m2[:, g_:g_ + 1], sel2[:, gsl], ALU.max, [sel2], [m2])
                    k.dve(tt(gs[:, :], m1[:, :], m2[:, :], ALU.add), reads=[m1, m2], writes=[gs])
                    k.dve(cp(mx[:, :], gs[:, :]), reads=[gs], writes=[mx])
                    kth_thr(mx, 8, 4, mx2)
                    k.dve(ts(gmask[:, :], gs[:, :], mx2[:, 0:1], ALU.is_ge), reads=[gs, mx2], writes=[gmask])
                    k.dve(ts(gmask[:, :], gmask[:, :], 1e9, ALU.mult, -1e9, ALU.add), reads=[gmask], writes=[gmask])
                    for g_ in range(8):
                        gsl = slice(g_ * 8, (g_ + 1) * 8)
                        k.dve(ts(selm[:, gsl], sel[a][:, gsl], gmask[:, g_:g_ + 1], ALU.add), reads=[sel[a], gmask], writes=[selm])
                    k.dve(cp(sel2[:, :], selm[:, :]), reads=[selm], writes=[sel2])
                    kth_thr(sel2, 64, 8, mx2)
                    k.dve(ts(eq[:, :], selm[:, :], mx2[:, 0:1], ALU.is_ge), reads=[selm, mx2], writes=[eq])
                    k.dve(tt(tw[:, :], sc_[a][:, :], eq[:, :], ALU.mult), reads=[sc_[a], eq], writes=[tw])
                    k.dve(lambda e: e.tensor_reduce(out=wsum[:, :], in_=tw[:, :], axis=AX.X, op=ALU.add), reads=[tw], writes=[wsum])
                    k.dve(recip(wsum[:, :], wsum[:, :]), reads=[wsum], writes=[wsum])
                    k.dve(ts(Wc[:, tt_, 0:64], tw[:, :], wsum[:, 0:1], ALU.mult, 2.5, ALU.mult), reads=[tw, wsum], writes=[Wc])
                k.barrier()
                if stop == "d":
                    k.dve(cp(acc[:, :, 0:66], Wc[:, :, :]), reads=[Wc, acc.r(0)], writes=[acc.r(0)])
                    k.barrier()
                    ov = out_d.t.rearrange("(t p) n -> p t n", p=128)
                    for tt_ in range(8):
                        k.dma("sp", ov[:, tt_, :], acc[:, tt_, :], reads=[acc.r(tt_)], writes=[out_d.r(tt_)], semres=acc.r(tt_))
                    k.finish()
                    return nc

        with ExitStack() as es:
            psg = [k.ps(es, f"psg{i}", [128, 512], F32) for i in range(2)]
            psu = [k.ps(es, f"psu{i}", [128, 512], F32) for i in range(2)]
            psd = [k.ps(es, f"psd{i}", [128, 512], F32) for i in range(4)]
            WG = [k.sb(es, f"WG{i}", [128, KC, 512], BF16) for i in range(2)]
            WU = [k.sb(es, f"WU{i}", [128, KC, 512], BF16) for i in range(2)]
            WD = [k.sb(es, f"WD{i}", [128, 4, D], BF16) for i in range(1)]
            actT = [k.sb(es, f"actT{i}", [128, 4, 512], BF16) for i in range(2)]
            sg = [k.sb(es, f"sgm{i}", [128, 512], F32) for i in range(2)]
            ng = 0
            nd = 0
            for e in range(n_exp):
                if e < 64:
                    gsrc, usrc, dsrc = wge_d.t[e], wue_d.t[e], wde_d.t[e]
                else:
                    gsrc, usrc, dsrc = wgs_d.t, wus_d.t, wds_d.t
                wg_, wu_, wd_ = WG[e % 2], WU[e % 2], WD[0]
                gv = gsrc.rearrange("(kc p) f -> p kc f", p=128)
                uv = usrc.rearrange("(kc p) f -> p kc f", p=128)
                dv = dsrc.rearrange("(fc p) n -> p fc n", p=128)
                for h in range(2):
                    k.dma("pool", wg_[:, h * 8:(h + 1) * 8, :], gv[:, h * 8:(h + 1) * 8, :], writes=[wg_], semres=wg_)
                    k.dma("pool", wu_[:, h * 8:(h + 1) * 8, :], uv[:, h * 8:(h + 1) * 8, :], writes=[wu_], semres=wu_)
                for h in range(2):
                    k.dma("pool", wd_[:, h * 2:(h + 1) * 2, :], dv[:, h * 2:(h + 1) * 2, :], writes=[wd_], semres=wd_)
                k.dve(tt(wd_[:, :, :], wd_[:, :, :], g2bc[:, :].unsqueeze(1).to_broadcast([128, 4, D]), ALU.mult), reads=[wd_, g2bc], writes=[wd_])
                for th in range(2):
                    tsl = slice(th * 512, (th + 1) * 512)
                    at = actT[th]
                    for ft in range(4):
                        a = ng % 2
                        ng += 1
                        fs = slice(ft * 128, (ft + 1) * 128)
                        for kc in range(KC):
                            k.pe(mm(psg[a][:, :], wg_[:, kc, fs], h2T[:, kc, tsl], kc == 0, kc == KC - 1), reads=[wg_, h2T], writes=[psg[a]])
                        for kc in range(KC):
                            k.pe(mm(psu[a][:, :], wu_[:, kc, fs], h2T[:, kc, tsl], kc == 0, kc == KC - 1), reads=[wu_, h2T], writes=[psu[a]])
                        k.act(actf(sg[a][:, :], psg[a][:, :], AF.Silu), reads=[psg[a]], writes=[sg[a]])
                        k.dve(tt(at[:, ft, :], sg[a][:, :], psu[a][:, :], ALU.mult), reads=[sg[a], psu[a]], writes=[at])
                for th in range(2):
                    at = actT[th]
                    for t4 in range(4):
                        tt_ = th * 4 + t4
                        for n in range(4):
                            a = nd % 4
                            nd += 1
                            for fc in range(4):
                                k.pe(mm(psd[a][:, :], at[:, fc, t4 * 128:(t4 + 1) * 128], wd_[:, fc, n * 512:(n + 1) * 512], fc == 0, fc == 3),
                                     reads=[at, wd_], writes=[psd[a]])
                            k.dve(stt(acc[:, tt_, n * 512:(n + 1) * 512], psd[a][:, :], Wc[:, tt_, e:e + 1], acc[:, tt_, n * 512:(n + 1) * 512], ALU.mult, ALU.add),
                                  reads=[psd[a], Wc, acc.r(tt_)], writes=[acc.r(tt_)])
            ov = out_d.t.rearrange("(t p) n -> p t n", p=128)
            for tt_ in range(8):
                k.dma("sp", ov[:, tt_, :], acc[:, tt_, :], reads=[acc.r(tt_)], writes=[out_d.r(tt_)], semres=acc.r(tt_))
        k.finish()
    return nc


def l2_inputs(inp, st, yT_all):
    x = np.asarray(inp["x"], np.float32)[0]
    w_in = np.asarray(inp["w_in"], np.float32)[0]
    w_ada = np.asarray(inp["w_ada"], np.float32)[0]
    b_ada = np.asarray(inp["b_ada"], np.float32)[0]
    ya = np.concatenate([np.asarray(y)[0:128] for y in yT_all], axis=0)
    yb = np.concatenate([np.asarray(y)[128:384] for y in yT_all], axis=0)
    yfull = np.concatenate([ya, yb], axis=0)
    wgates = np.ascontiguousarray(w_in[:, 15360:19456])
    shared = dict(
        c_pk=pk16(inp["c"][0]), wada=w_ada, bada1=pk16(b_ada[0:4096]), badar=np.ascontiguousarray(b_ada[4096:12288][None, :]),
        ln1_pk=pk16(inp["ln1_g"][0]), ln2_row=np.asarray(inp["ln2_g"], np.float32)[0][None, :].copy(),
        wgates=wgates, pa=np.asarray(inp["p_a"], np.float32)[0], pb=np.asarray(inp["p_b"], np.float32)[0],
        wo=np.asarray(inp["w_o"], np.float32)[0], rw=np.asarray(inp["router_w"], np.float32)[0],
        rbias=np.asarray(inp["router_bias"], np.float32)[0][None, :].copy(),
        wge=np.asarray(inp["w_gate_e"], np.float32)[0], wue=np.asarray(inp["w_up_e"], np.float32)[0],
        wde=np.asarray(inp["w_down_e"], np.float32)[0], wgs=np.asarray(inp["w_gate_s"], np.float32)[0],
        wus=np.asarray(inp["w_up_s"], np.float32)[0], wds=np.asarray(inp["w_down_s"], np.float32)[0],
        identf=st["identf"],
    )
    maps = []
    TS = S // NCORES
    for c in range(NCORES):
        m = dict(shared)
        m["xTs"] = np.ascontiguousarray(x[c * TS:(c + 1) * TS].T)
        m["xs"] = np.ascontiguousarray(x[c * TS:(c + 1) * TS])
        m["yTs"] = np.ascontiguousarray(yfull[:, c * TS:(c + 1) * TS])
        maps.append(m)
    return maps


_CACHE = {}


def kernel(**inputs):
    st = static_tables()
    if "l1" not in _CACHE:
        _CACHE["l1"] = build_l1()
    res1 = run_bass_kernel_spmd(_CACHE["l1"], l1_inputs(inputs, st), core_ids=list(range(NCORES)))
    yT_all = [res1.results[c]["yT"] for c in range(NCORES)]
    if "l2" not in _CACHE:
        _CACHE["l2"] = build_l2()
    res2 = run_bass_kernel_spmd(_CACHE["l2"], l2_inputs(inputs, st, yT_all), core_ids=list(range(NCORES)))
    out = np.concatenate([np.asarray(res2.results[c]["out"], np.float32) for c in range(NCORES)], axis=0)
    return out[None, :, :].astype(np.float32)


def build_fused(n_exp=65):
    nc = bass.Bass("TRN2", target_bir_lowering=False)
    k = K(nc)
    EI, EO, IN = "ExternalInput", "ExternalOutput", "Internal"
    NH = 8
    xT_d = k.dram("xT", [D, S], F32, EI)
    c_d = k.dram("c_pk", [128, KC], F32, EI)
    wada_d = k.dram("wada", [D, 12288], F32, EI)
    bada_d = k.dram("bada1", [128, 32], F32, EI)
    ln1_d = k.dram("ln1_pk", [128, KC], F32, EI)
    win_d = k.dram("win", [NH, D, 1920], F32, EI)
    qg_d = k.dram("qg", [128, 1], F32, EI)
    kg_d = k.dram("kg", [128, 1], F32, EI)
    rb_d = k.dram("rb", [NH, 96], F32, EI)
    idx_d = k.dram("idxT", [128, 3 * 256], F32, EI)
    neg_d = k.dram("negT", [128, 256], F32, EI)
    cos_d = k.dram("cosT", [128, S], F32, EI)
    sin_d = k.dram("sinT", [128, S], F32, EI)
    rm_d = k.dram("rmat", [128, 128], F32, EI)
    idf_d = k.dram("identf", [128, 128], F32, EI)
    dmat_d = k.dram("dmatT", [NH, 128, 128], F32, EI)
    xi_d = k.dram("xibc", [NH, 128, 128], F32, EI)
    zeta_d = k.dram("zetacol", [128, NH], F32, EI)
    gch_d = k.dram("gchunk", [128, NH], F32, EI)
    gng_d = k.dram("gng", [NH, 256], F32, EI)
    kbias_d = k.dram("kbias", [128, 192], F32, EI)
    vmask_d = k.dram("vmask", [128, 64], F32, EI)
    qk_s = k.dram("qk_s", [8, 128, S], BF16, IN)
    vt_s = k.dram("vt_s", [S, 896], BF16, IN)
    ys = k.dram("ys", [3072, 1024], BF16, IN)
    xb_s = k.dram("xb_s", [S // 512, 128, KC * 512], BF16, IN)
    zpow_d = k.dram("zpow", [128, NH * 56], F32, EI)

    with ExitStack() as es0:
        rstd_all = k.sb(es0, "rstd_all", [128, S], F32)
        rcol_all = k.sb(es0, "rcol_all", [128, 64], F32)
        zpow = k.sb(es0, "zpow", [128, NH * 56], F32)
        k.dma("sp", zpow[:, :], zpow_d[:, :], writes=[zpow], semres=zpow)
        ones_b = k.sb(es0, "ones_b", [128, 128], BF16)
        idf = k.sb(es0, "idf", [128, 128], F32)
        idb = k.sb(es0, "idb", [128, 128], BF16)
        k.dve(lambda e: e.memset(ones_b[:, :], 1.0), writes=[ones_b])
        k.dma("sp", idf[:, :], idf_d[:, :], writes=[idf], semres=idf)
        k.dve(cp(idb[:, :], idf[:, :]), reads=[idf], writes=[idb])
        gg = k.sb(es0, "gg", [128, KC], F32)
        sh1b = k.sb(es0, "sh1b", [128, KC], BF16)
        sh1bc = k.sb(es0, "sh1bc", [128, KC, 128], BF16)
        qg = k.sb(es0, "qg", [128, 1], F32)
        kg = k.sb(es0, "kg", [128, 1], F32)
        rmb = k.sb(es0, "rmb", [128, 128], BF16)
        epsr = k.sb(es0, "epsr", [128, 1], F32)
        epsg = k.sb(es0, "epsg", [128, 1], F32)
        kbias = k.sb(es0, "kbias", [128, 192], F32)
        vmask = k.sb(es0, "vmask", [128, 64], F32)
        zeta = k.sb(es0, "zeta", [128, NH], F32)
        gch = k.sb(es0, "gch", [128, NH], F32)
        k.dma("sp", kbias[:, :], kbias_d[:, :], writes=[kbias], semres=kbias)
        k.dma("sp", vmask[:, :], vmask_d[:, :], writes=[vmask], semres=vmask)
        k.dma("sp", zeta[:, :], zeta_d[:, :], writes=[zeta], semres=zeta)
        k.dma("sp", gch[:, :], gch_d[:, :], writes=[gch], semres=gch)
        k.dve(lambda e: e.memset(epsr[:, :], RMS_EPS), writes=[epsr])
        k.dve(lambda e: e.memset(epsg[:, :], GN_EPS), writes=[epsg])
        with ExitStack() as es:
            psA0 = k.ps(es, "psM", [128, 512], F32)
            cb = emit_silu_c(k, es, c_d)
            mod1 = emit_mod_cols(k, es, cb, wada_d, bada_d, 32, "mod1", psA0)
            ln1 = k.sb(es, "ln1", [128, KC], F32)
            k.dma("sp", ln1[:, :], ln1_d[:, :], writes=[ln1], semres=ln1)
            k.dve(stt(gg[:, :], mod1[:, 16:32], 1.0, ln1[:, :], ALU.add, ALU.mult), reads=[mod1, ln1], writes=[gg])
            k.dve(cp(sh1b[:, :], mod1[:, 0:16]), reads=[mod1], writes=[sh1b])
            k.dve(cp(sh1bc[:, :, :], sh1b[:, :].unsqueeze(2).to_broadcast([128, KC, 128])), reads=[sh1b], writes=[sh1bc])
            k.dma("sp", qg[:, :], qg_d[:, :], writes=[qg], semres=qg)
            k.dma("sp", kg[:, :], kg_d[:, :], writes=[kg], semres=kg)
            k.dve(ts(qg[:, :], qg[:, :], 128.0 ** -0.5, ALU.mult), reads=[qg], writes=[qg])
            rmf = k.sb(es, "rmf", [128, 128], F32)
            k.dma("sp", rmf[:, :], rm_d[:, :], writes=[rmf], semres=rmf)
            k.dve(cp(rmb[:, :], rmf[:, :]), reads=[rmf], writes=[rmb])
            k.barrier()

        k.nm += 1
        wb = Tl(k, es0.enter_context(nc.sbuf_tensor(f"r{k.nm}_wb", [128, KC, 1920], BF16, side="right")), "wb")

        def load_w(hv_):
            wv = win_d.t[hv_].rearrange("(kc p) n -> p kc n", p=128)
            for kc4 in range(4):
                k.dma("pool", wb[:, kc4 * 4:(kc4 + 1) * 4, :], wv[:, kc4 * 4:(kc4 + 1) * 4, :], writes=[wb], semres=wb)

        load_w(0)
        for hv in range(NH):
            with ExitStack() as es:
                psA = [k.ps(es, f"psA{i}", [128, 512], F32) for i in range(8)]
                b1col = k.sb(es, "b1col", [128, 8], F32)
                for f in range(8):
                    for kc in range(KC):
                        k.pe(mm(psA[1][:, f:f + 1], wb[:, kc, f * 128:(f + 1) * 128], sh1b[:, kc:kc + 1], kc == 0, kc == KC - 1),
                             reads=[wb, sh1b], writes=[psA[1]])
                k.dve(cp(b1col[:, :], psA[1][:, 0:8]), reads=[psA[1]], writes=[b1col])
                b1bc = k.sb(es, "b1bc", [128, 896], F32)
                for j in range(2):
                    for kc in range(KC):
                        k.pe(mm(psA[2 + j][:, 0:448], sh1bc[:, kc, :], wb[:, kc, 1024 + j * 448:1024 + (j + 1) * 448], kc == 0, kc == KC - 1),
                             reads=[wb, sh1bc], writes=[psA[2 + j]])
                    k.dve(cp(b1bc[:, j * 448:(j + 1) * 448], psA[2 + j][:, 0:448]), reads=[psA[2 + j]], writes=[b1bc])
                for kc in range(KC):
                    k.dve(ts(wb[:, kc, :], wb[:, kc, :], gg[:, kc:kc + 1], ALU.mult), reads=[wb, gg], writes=[wb])
                b1k = k.sb(es, "b1k", [128, 1], F32)
                k.dve(ts(b1k[:, :], b1col[:, 7:8], 128.0 ** -0.5, ALU.mult), reads=[b1col], writes=[b1k])

                NT = S // 512
                xbs = [k.sb(es, f"xb{i}", [128, KC, 512], BF16) for i in range(2)]
                sq = k.sb(es, "sq", [128, KC, 512], BF16) if hv == 0 else None
                cs = [k.sb(es, f"cos{i}", [128, 512], F32) for i in range(2)]
                sn = [k.sb(es, f"sin{i}", [128, 512], F32) for i in range(2)]
                t1 = [k.sb(es, f"t1_{i}", [128, 512], F32) for i in range(2)]
                qf = [k.sb(es, f"qf_{i}", [128, 512], F32) for i in range(2)]
                qsq = [k.sb(es, f"qsq_{i}", [128, 512], BF16) for i in range(2)]
                rq = [k.sb(es, f"rq_{i}", [128, 512], F32) for i in range(2)]
                qo = [k.sb(es, f"qo_{i}", [128, 512], BF16) for i in range(4)]
                qfb = [k.sb(es, f"qfb_{i}", [128, 512], BF16) for i in range(2)]
                ra = [k.sb(es, f"ra_{i}", [128, 512], F32) for i in range(2)]
                vo = [k.sb(es, f"vo_{i}", [128, 896], BF16) for i in range(2)]
                xv = xT_d.t.rearrange("(kc p) s -> p kc s", p=128)
                nq = 0
                nv = 0

                def load_x(t):
                    xb = xbs[t % 2]
                    if hv == 0:
                        for h in range(4):
                            k.dma("pool", xb[:, h * 4:(h + 1) * 4, :], xv[:, h * 4:(h + 1) * 4, t * 512:(t + 1) * 512],
                                  writes=[xb], semres=xb)
                    else:
                        k.dma("sp", xb[:, :, :].rearrange("p a b -> p (a b)"), xb_s.t[t], writes=[xb], semres=xb)

                load_x(0)
                for t in range(NT):
                    xb = xbs[t % 2]
                    if t + 1 < NT:
                        load_x(t + 1)
                    c0 = t * 512
                    if t >= 14:
                        flist = list(range(8))
                        segs = [(0, 384), (384, 896)]
                    elif t == 13:
                        flist = [1, 3, 5, 7]
                        segs = [(0, 384), (384, 640)]
                    elif t >= 8:
                        flist = [5, 7]
                        segs = [(256, 384), (384, 640)]
                    else:
                        flist = [7]
                        segs = [(384, 640)]
                    k.dma("sp", cs[t % 2][:, :], cos_d[:, c0:c0 + 512], writes=[cs[t % 2]], semres=cs[t % 2])
                    k.dma("sp", sn[t % 2][:, :], sin_d[:, c0:c0 + 512], writes=[sn[t % 2]], semres=sn[t % 2])
                    rstd_t = rstd_all[:, c0:c0 + 512]
                    rres = rstd_all.r(t)
                    if hv == 0:
                        k.dma("sp", xb_s.t[t], xb[:, :, :].rearrange("p a b -> p (a b)"), reads=[xb], writes=[], semres=xb)
                        for h in range(2):
                            k.act(actf(sq[:, h * 8:(h + 1) * 8, :], xb[:, h * 8:(h + 1) * 8, :], AF.Square), reads=[xb], writes=[sq.r(h)])
                        for kc in range(KC):
                            k.pe(mm(psA[0][:, :], ones_b[:, :], sq[:, kc, :], kc == 0, kc == KC - 1),
                                 reads=[sq.r(kc // 8), ones_b], writes=[psA[0]])
                        k.act(actf(rstd_t, psA[0][:, :], AF.Sqrt, bias=epsr[:, :], scale=1.0 / D), reads=[psA[0], epsr], writes=[rres])
                        k.dve(recip(rstd_t, rstd_t), reads=[rres], writes=[rres])
                        for s4 in range(4):
                            k.pe(lambda e, s4=s4: e.transpose(psA[1][:, s4 * 128:(s4 + 1) * 128], rstd_all[:, c0 + s4 * 128:c0 + (s4 + 1) * 128], idf[:, :]),
                                 reads=[rres, idf], writes=[psA[1]])
                        k.dve(cp(rcol_all[:, t * 4:(t + 1) * 4], psA[1][:, :].rearrange("p (s n) -> p s n", n=128)[:, :, 0]), reads=[psA[1]], writes=[rres])
                    for fi, f in enumerate(flist):
                        pb = psA[2 + (fi % 2)]
                        for kc in range(KC):
                            k.pe(mm(pb[:, :], wb[:, kc, f * 128:(f + 1) * 128], xb[:, kc, :], kc == 0, kc == KC - 1),
                                 reads=[wb, xb], writes=[pb])
                        a = nq % 2
                        nq += 1
                        k.dve(tt(t1[a][:, :], pb[:, :], rstd_t, ALU.mult), reads=[pb, rres], writes=[t1[a]])
                        o = qo[nq % 4]
                        if f < 6:
                            gcol = qg if f % 2 == 0 else kg
                            k.act(actf(qsq[a][:, :], t1[a][:, :], AF.Square, bias=b1col[:, f:f + 1]), reads=[t1[a], b1col], writes=[qsq[a]])
                            k.act(actf(qf[a][:, :], t1[a][:, :], AF.Identity, bias=b1col[:, f:f + 1]), reads=[t1[a], b1col], writes=[qf[a]])
                            pn = psA[4 + a]
                            k.pe(mm(pn[:, :], ones_b[:, :], qsq[a][:, :], True, True), reads=[qsq[a], ones_b], writes=[pn])
                            k.act(actf(rq[a][:, :], pn[:, :], AF.Sqrt, bias=epsr[:, :], scale=1.0 / 128), reads=[pn, epsr], writes=[rq[a]])
                            k.dve(recip(rq[a][:, :], rq[a][:, :]), reads=[rq[a]], writes=[rq[a]])
                            k.dve(stt(o[:, :], qf[a][:, :], gcol[:, 0:1], rq[a][:, :], ALU.mult, ALU.mult), reads=[qf[a], gcol, rq[a]], writes=[o])
                        else:
                            sc = 1.0 if f == 6 else 128.0 ** -0.5
                            bcol = b1col[:, 6:7] if f == 6 else b1k[:, 0:1]
                            bres = b1col if f == 6 else b1k
                            k.act(actf(qf[a][:, :], t1[a][:, :], AF.Identity, bias=bcol, scale=sc), reads=[t1[a], bres], writes=[qf[a]])
                            k.act(actf(qfb[a][:, :], t1[a][:, :], AF.Identity, bias=bcol, scale=sc), reads=[t1[a], bres], writes=[qfb[a]])
                            pn = psA[4 + a]
                            k.pe(mm(pn[:, :], rmb[:, :], qfb[a][:, :], True, True), reads=[qfb[a], rmb], writes=[pn])
                            k.dve(tt(ra[a][:, :], qf[a][:, :], cs[t % 2][:, :], ALU.mult), reads=[qf[a], cs[t % 2]], writes=[ra[a]])
                            k.dve(tt(rq[a][:, :], pn[:, :], sn[t % 2][:, :], ALU.mult), reads=[pn, sn[t % 2]], writes=[rq[a]])
                            k.pool(tt(o[:, :], ra[a][:, :], rq[a][:, :], ALU.add), reads=[ra[a], rq[a]], writes=[o])
                        k.dma("sp", qk_s.t[f, :, c0:c0 + 512], o[:, :], reads=[o], writes=[], semres=o)
                    for s4 in range(4):
                        v = vo[nv % 2]
                        nv += 1
                        for si, (a0, a1) in enumerate(segs):
                            pb = psA[6 + si]
                            for kc in range(KC):
                                k.pe(mm(pb[:, 0:a1 - a0], xb[:, kc, s4 * 128:(s4 + 1) * 128], wb[:, kc, 1024 + a0:1024 + a1],
                                        kc == 0, kc == KC - 1), reads=[wb, xb], writes=[pb])
                            k.dve(stt(v[:, a0:a1], pb[:, 0:a1 - a0], rcol_all[:, t * 4 + s4:t * 4 + s4 + 1], b1bc[:, a0:a1], ALU.mult, ALU.add),
                                  reads=[pb, rres, b1bc], writes=[v])
                        tix = t * 4 + s4
                        k.dve(ts(v[:, 384:640], v[:, 384:640], vmask[:, tix:tix + 1], ALU.mult), reads=[v, vmask], writes=[v])
                        r0 = c0 + s4 * 128
                        lo_, hi_ = segs[0][0], segs[-1][1]
                        k.dma("sp", vt_s.t[r0:r0 + 128, lo_:hi_], v[:, lo_:hi_], reads=[v], writes=[], semres=v)
                k.barrier()
            if hv + 1 < NH:
                load_w(hv + 1)

            with ExitStack() as es:
                psS = [k.ps(es, f"psS{i}", [128, 512], F32) for i in range(2)]
                psO = [k.ps(es, f"psO{i}", [128, 512], F32) for i in range(2)]
                psL = [k.ps(es, f"psL{i}", [128, 512], F32) for i in range(2)]
                accO = k.sb(es, "accO", [128, 2048], F32)
                accL = k.sb(es, "accL", [128, 2048], F32)
                idx = k.sb(es, "idx", [128, 3 * 256], F32)
                negm = k.sb(es, "negm", [128, 256], F32)
                rb = k.sb(es, "rb", [128, 96], F32)
                k.dma("sp", idx[:, :], idx_d[:, :], writes=[idx], semres=idx)
                k.dma("sp", negm[:, :], neg_d[:, :], writes=[negm], semres=negm)
                k.dma("sp", rb[:, :], rb_d.t[hv:hv + 1, :].partition_broadcast(128), writes=[rb], semres=rb)
                mbs = [k.sb(es, f"mb{g}", [128, 256], F32) for g in range(3)]
                tmpm = k.sb(es, "tmpm", [128, 256], F32)
                for g in range(3):
                    k.dve(cp(mbs[g][:, :], negm[:, :]), reads=[negm], writes=[mbs[g]])
                    for b in range(32):
                        k.dve(ts(tmpm[:, :], idx[:, g * 256:(g + 1) * 256], float(b), ALU.is_equal, rb[:, g * 32 + b:g * 32 + b + 1], ALU.mult),
                              reads=[idx, rb], writes=[tmpm])
                        k.dve(tt(mbs[g][:, :], mbs[g][:, :], tmpm[:, :], ALU.add), reads=[tmpm, mbs[g]], writes=[mbs[g]])
                qm = [k.sb(es, f"qm{i}", [128, 2048], BF16) for i in range(2)]
                km = [k.sb(es, f"km{i}", [128, 2048], BF16) for i in range(2)]
                vm = [k.sb(es, f"vm{i}", [128, 16, 128], BF16) for i in range(2)]
                kpv = k.sb(es, "kpv", [128, 2048], BF16)
                vpv = k.sb(es, "vpv", [128, 16, 128], BF16)
                sadd = [k.sb(es, f"sadd{i}", [128, 256], F32) for i in range(2)]
                pT = [k.sb(es, f"pT{i}", [128, 256], BF16) for i in range(3)]
                nblk = 0
                nbatch = 0

                def load_v(dst, g, t0, d, nsb):
                    if d == 1:
                        k.dma("sp", dst[:, :, :], vt_s.t[t0:t0 + 2048, g * 128:(g + 1) * 128].rearrange("(n c) e -> c n e", c=128),
                              writes=[dst], semres=dst)
                    else:
                        for nl_ in range(nsb):
                            ta_ = t0 + nl_ * 128 * d
                            k.dma("sp", dst[:, nl_ * d:(nl_ + 1) * d, :],
                                  vt_s.t[ta_:ta_ + 128 * d, g * 128:(g + 1) * 128].rearrange("(c r) e -> c r e", r=d),
                                  writes=[dst], semres=dst)

                for g in range(3):
                    d = DILS[g]
                    nsb = 16 // d
                    q, kk, vv = qm[g % 2], km[g % 2], vm[g % 2]
                    t0 = 3 * 2048
                    k.dma("sp", q[:, :], qk_s.t[2 * g, :, t0:t0 + 2048], writes=[q], semres=q)
                    k.dma("sp", kk[:, :], qk_s.t[2 * g + 1, :, t0:t0 + 2048], writes=[kk], semres=kk)
                    load_v(vv, g, t0, d, nsb)
                    if g == 2:
                        k.dma("sp", kpv[:, :], qk_s.t[2 * g + 1, :, t0 - 2048:t0], writes=[kpv], semres=kpv)
                        load_v(vpv, g, t0 - 2048, d, nsb)
                    for jb in ((2, 3) if g < 2 else (0, 1, 2, 3)):
                        bo = psO[nbatch % 2]
                        bl = psL[nbatch % 2]
                        nbatch += 1
                        for ji in range(4):
                            j = jb * 4 + ji
                            nl, r = j // d, j % d
                            dsl = lambda st_: slice(st_, st_ + 127 * d + 1, d)
                            cols = dsl(nl * 128 * d + r)
                            N = 3 * nsb + nl
                            if nl > 0:
                                pk, pv = kk, vv
                                pcols = dsl((nl - 1) * 128 * d + r)
                                pj = (nl - 1) * d + r
                            else:
                                pk, pv = kpv, vpv
                                pcols = dsl((nsb - 1) * 128 * d + r)
                                pj = (nsb - 1) * d + r
                            ps = psS[nblk % 2]
                            sa = sadd[nblk % 2]
                            p = pT[nblk % 3]
                            nblk += 1
                            qa = q[:, cols]
                            k.pe(mm(ps[:, 0:128], pk[:, pcols], qa, True, True), reads=[pk, q], writes=[ps])
                            k.pe(mm(ps[:, 128:256], kk[:, cols], qa, True, True), reads=[kk, q], writes=[ps])
                            k.dve(tt(sa[:, 0:256], ps[:, 0:256], mbs[g][:, 0:256], ALU.add), reads=[ps, mbs[g]], writes=[sa])
                            k.act(actf(p[:, 0:128], sa[:, 0:128], AF.Exp, bias=kbias[:, g * 64 + N - 1:g * 64 + N]), reads=[sa, kbias], writes=[p.r(0)])
                            k.act(actf(p[:, 128:256], sa[:, 128:256], AF.Exp, bias=kbias[:, g * 64 + N:g * 64 + N + 1]), reads=[sa, kbias], writes=[p.r(1)])
                            osl = bo[:, ji * 128:(ji + 1) * 128]
                            lsl = bl[:, ji * 128:(ji + 1) * 128]
                            k.pe(mm(osl, pv[:, pj, :], p[:, 0:128], True, False), reads=[pv, p.r(0)], writes=[bo])
                            k.pe(mm(lsl, ones_b[:, :], p[:, 0:128], True, False), reads=[ones_b, p.r(0)], writes=[bl])
                            k.pe(mm(osl, vv[:, j, :], p[:, 128:256], False, True), reads=[vv, p.r(1)], writes=[bo])
                            k.pe(mm(lsl, ones_b[:, :], p[:, 128:256], False, True), reads=[ones_b, p.r(1)], writes=[bl])
                        j0 = jb * 4
                        nl0, r0 = j0 // d, j0 % d
                        if d == 1:
                            dst = slice(j0 * 128, j0 * 128 + 512)
                            dO, dL, sO, sL = accO[:, dst], accL[:, dst], bo[:, :], bl[:, :]
                        else:
                            base = nl0 * 128 * d
                            dO = accO[:, base:base + 128 * d].rearrange("p (a r) -> p r a", r=d)[:, r0:r0 + 4, :]
                            dL = accL[:, base:base + 128 * d].rearrange("p (a r) -> p r a", r=d)[:, r0:r0 + 4, :]
                            sO = bo[:, :].rearrange("p (r a) -> p r a", a=128)
                            sL = bl[:, :].rearrange("p (r a) -> p r a", a=128)
                        if g == 0:
                            k.dve(cp(dO, sO), reads=[bo], writes=[accO])
                            k.dve(cp(dL, sL), reads=[bl], writes=[accL])
                        else:
                            k.dve(tt(dO, sO, dO, ALU.add), reads=[bo, accO], writes=[accO])
                            k.dve(tt(dL, sL, dL, ALU.add), reads=[bl, accL], writes=[accL])
                yo = k.sb(es, "yo", [128, 1024], BF16)
                own = slice(1024, 2048)
                k.dve(recip(accL[:, own], accL[:, own]), reads=[accL], writes=[accL])
                k.dve(tt(yo[:, :], accO[:, own], accL[:, own], ALU.mult), reads=[accO, accL], writes=[yo])
                k.dma("sp", ys.t[hv * 128:(hv + 1) * 128, :], yo[:, :], reads=[yo], writes=[], semres=yo)
                k.barrier()

            with ExitStack() as es:
                psAT = [k.ps(es, f"psAT{i}", [128, 512], F32) for i in range(2)]
                psR = [k.ps(es, f"psR{i}", [128, 512], F32) for i in range(2)]
                psU = k.ps(es, "psU", [128, 512], F32)
                psK2 = [k.ps(es, f"psK{i}", [128, 1024], BF16) for i in range(2)]
                psY = [k.ps(es, f"psY{i}", [128, 1024], BF16) for i in range(1)] * 2
                dmat = k.sb(es, "dmat", [128, 128], F32)
                xib = k.sb(es, "xib", [128, 128], F32)
                gng = k.sb(es, "gng", [128, 256], F32)
                k.dma("sp", dmat[:, :], dmat_d.t[hv], writes=[dmat], semres=dmat)
                k.dma("sp", xib[:, :], xi_d.t[hv], writes=[xib], semres=xib)
                k.dma("sp", gng[:, :], gng_d.t[hv:hv + 1, :].partition_broadcast(128), writes=[gng], semres=gng)
                qm_ = k.sb(es, "rqm", [128, 2048], BF16)
                km = [k.sb(es, f"rkm{i}", [128, 2048], BF16) for i in range(2)]
                vm = [k.sb(es, f"rvm{i}", [128, 16, 256], BF16) for i in range(2)]
                gm_ = k.sb(es, "rgm", [128, 16, 256], BF16)
                St = k.sb(es, "St", [128, 256], F32)
                Sb = k.sb(es, "Sb", [128, 256], BF16)
                atd = [k.sb(es, f"atd{i}", [128, 128], BF16) for i in range(2)]
                qx = [k.sb(es, f"qx{i}", [128, 128], BF16) for i in range(2)]
                kz = [k.sb(es, f"kz{i}", [128, 128], BF16) for i in range(2)]
                s1 = [k.sb(es, f"s1_{i}", [128, 1], F32) for i in range(2)]
                ssq = [k.sb(es, f"ssq_{i}", [128, 1], F32) for i in range(2)]
                junk = [k.sb(es, f"junk{i}", [128, 256], F32) for i in range(2)]
                yn = [k.sb(es, f"yn{i}", [128, 256], F32) for i in range(2)]
                sg = [k.sb(es, f"sg{i}", [128, 256], F32) for i in range(2)]
                yb = [k.sb(es, f"yb{i}", [128, 256], BF16) for i in range(2)]
                ybT = k.sb(es, "ybT", [128, 2, 1024], BF16)
                k.dve(lambda e: e.memset(St[:, :], 0.0), writes=[St])
                k.dve(lambda e: e.memset(Sb[:, :], 0.0), writes=[Sb])
                for m in range(4):
                    t0 = m * 2048
                    kk, vv = km[m % 2], vm[m % 2]
                    k.dma("sp", kk[:, :], qk_s.t[7, :, t0:t0 + 2048], writes=[kk], semres=kk)
                    k.dma("sp", vv[:, :, :], vt_s.t[t0:t0 + 2048, 384:640].rearrange("(n c) e -> c n e", c=128), writes=[vv], semres=vv)
                    if m == 3:
                        k.dma("sp", qm_[:, :], qk_s.t[6, :, t0:t0 + 2048], writes=[qm_], semres=qm_)
                        k.dma("sp", gm_[:, :, :], vt_s.t[t0:t0 + 2048, 640:896].rearrange("(n c) e -> c n e", c=128), writes=[gm_], semres=gm_)
                    for n in range(16):
                        gn = m * 16 + n
                        a = gn % 2
                        cols = slice(n * 128, (n + 1) * 128)
                        own_c = gn >= 56
                        if own_c:
                            k.pe(mm(psAT[a][:, 0:128], kk[:, cols], qm_[:, cols], True, True), reads=[kk, qm_], writes=[psAT[a]])
                            k.dve(tt(atd[a][:, :], psAT[a][:, 0:128], dmat[:, :], ALU.mult), reads=[psAT[a], dmat], writes=[atd[a]])
                            k.pool(tt(qx[a][:, :], qm_[:, cols], xib[:, :], ALU.mult), reads=[qm_, xib], writes=[qx[a]])
                            k.pe(mm(psR[a][:, 0:256], atd[a][:, :], vv[:, n, :], True, False), reads=[atd[a], vv], writes=[psR[a]])
                            k.pe(mm(psR[a][:, 0:256], qx[a][:, :], Sb[:, :], False, True), reads=[qx[a], Sb], writes=[psR[a]])
                        if gn < 56:
                            pk_ = psK2[gn % 2]
                            k.pe(lambda e, cols=cols, kk=kk, pk_=pk_: e.transpose(pk_[:, 0:128], kk[:, cols], idb[:, :]), reads=[kk, idb], writes=[pk_])
                            k.act(actf(kz[a][:, :], pk_[:, 0:128], AF.Copy, scale=zpow[:, hv * 56 + gn:hv * 56 + gn + 1]), reads=[pk_, zpow], writes=[kz[a]])
                            k.pe(mm(psU[:, 0:256], kz[a][:, :], vv[:, n, :], gn == 0, gn == 55), reads=[kz[a], vv], writes=[psU])
                            if gn == 55:
                                k.dve(cp(St[:, :], psU[:, 0:256]), reads=[psU], writes=[St])
                                k.act(lambda e: e.copy(out=Sb[:, :], in_=St[:, :]), reads=[St], writes=[Sb])
                        elif gn < 63:
                            pk_ = psK2[gn % 2]
                            k.pe(lambda e, cols=cols, kk=kk, pk_=pk_: e.transpose(pk_[:, 0:128], kk[:, cols], idb[:, :]), reads=[kk, idb], writes=[pk_])
                            k.act(actf(kz[a][:, :], pk_[:, 0:128], AF.Copy, scale=zeta[:, hv:hv + 1]), reads=[pk_, zeta], writes=[kz[a]])
                            k.pe(mm(psU[:, 0:256], kz[a][:, :], vv[:, n, :], True, True), reads=[kz[a], vv], writes=[psU])
                            k.dve(stt(St[:, :], St[:, :], gch[:, hv:hv + 1], psU[:, 0:256], ALU.mult, ALU.add), reads=[St, gch, psU], writes=[St])
                            k.act(lambda e: e.copy(out=Sb[:, :], in_=St[:, :]), reads=[St], writes=[Sb])
                        if own_c:
                            k.dve(lambda e, a=a: e.tensor_reduce(out=s1[a][:, :], in_=psR[a][:, 0:256], op=ALU.add, axis=AX.X), reads=[psR[a]], writes=[s1[a]])
                            k.dve(ts(s1[a][:, :], s1[a][:, :], -1.0 / 256, ALU.mult), reads=[s1[a]], writes=[s1[a]])
                            k.act(actf(junk[a][:, :], psR[a][:, 0:256], AF.Square, bias=s1[a][:, 0:1], accum_out=ssq[a][:, :]), reads=[psR[a], s1[a]], writes=[junk[a], ssq[a]])
                            k.act(actf(ssq[a][:, :], ssq[a][:, :], AF.Sqrt, bias=epsg[:, :], scale=1.0 / 256), reads=[ssq[a], epsg], writes=[ssq[a]])
                            k.dve(recip(ssq[a][:, :], ssq[a][:, :]), reads=[ssq[a]], writes=[ssq[a]])
                            k.dve(ts(yn[a][:, :], psR[a][:, 0:256], s1[a][:, 0:1], ALU.add, ssq[a][:, 0:1], ALU.mult), reads=[psR[a], s1[a], ssq[a]], writes=[yn[a]])
                            k.act(actf(sg[a][:, :], gm_[:, n, :], AF.Silu), reads=[gm_], writes=[sg[a]])
                            k.pool(tt(yn[a][:, :], yn[a][:, :], gng[:, :], ALU.mult), reads=[yn[a], gng], writes=[yn[a]])
                            k.pool(tt(yb[a][:, :], yn[a][:, :], sg[a][:, :], ALU.mult), reads=[yn[a], sg[a]], writes=[yb[a]])
                            for h in range(2):
                                k.pe(lambda e, a=a, h=h: e.transpose(psY[a][:, h * 128:(h + 1) * 128], yb[a][:, h * 128:(h + 1) * 128], idb[:, :]),
                                     reads=[yb[a], idb], writes=[psY[a]])
                            lc = slice((n - 8) * 128, (n - 7) * 128)
                            k.dve(cp(ybT[:, :, lc], psY[a][:, 0:256].rearrange("p (h t) -> p h t", h=2)), reads=[psY[a]], writes=[ybT])
                for h in range(2):
                    r0 = 1024 + hv * 256 + h * 128
                    k.dma("sp", ys.t[r0:r0 + 128, :], ybT[:, h, :], reads=[ybT], writes=[], semres=ybT)
                k.barrier()
    shared = {"c_pk": c_d, "wada": wada_d, "bada1": bada_d, "ln1_pk": ln1_d, "identf": idf_d, "ys": ys}
    return build_l2(n_exp=n_exp, nc=nc, k=k, shared=shared)


def fused_inputs(inp, st):
    x = np.asarray(inp["x"], np.float32)[0]
    w_in = np.asarray(inp["w_in"], np.float32)[0]
    w_ada = np.asarray(inp["w_ada"], np.float32)[0]
    b_ada = np.asarray(inp["b_ada"], np.float32)[0]
    rel_bias = np.asarray(inp["rel_bias"], np.float32)
    TS = S // NCORES
    wins = []
    for c in range(NCORES):
        cols = []
        for g in range(3):
            h = g * 8 + c
            cols += [np.arange(h * 128, (h + 1) * 128), 3072 + np.arange(h * 128, (h + 1) * 128)]
        cols += [9216 + np.arange(c * 128, (c + 1) * 128), 10240 + np.arange(c * 128, (c + 1) * 128)]
        for g in range(3):
            h = g * 8 + c
            cols += [6144 + np.arange(h * 128, (h + 1) * 128)]
        cols += [11264 + np.arange(c * 256, (c + 1) * 256), 13312 + np.arange(c * 256, (c + 1) * 256)]
        wins.append(w_in[:, np.concatenate(cols)])
    win = np.ascontiguousarray(np.stack(wins))
    rb = np.ascontiguousarray(np.stack([np.concatenate([rel_bias[:, g * 8 + c] for g in range(3)]) for c in range(NCORES)]))
    shared = dict(
        c_pk=pk16(inp["c"][0]), wada=w_ada, bada1=pk16(b_ada[0:4096]), badar=np.ascontiguousarray(b_ada[4096:12288][None, :]),
        ln1_pk=pk16(inp["ln1_g"][0]), ln2_row=np.asarray(inp["ln2_g"], np.float32)[0][None, :].copy(),
        win=win, qg=np.asarray(inp["q_norm_g"], np.float32)[0][:, None].copy(), kg=np.asarray(inp["k_norm_g"], np.float32)[0][:, None].copy(),
        rb=rb, idxT=st["idxT"], negT=st["negT"], rmat=st["rmat"], identf=st["identf"],
        dmatT=np.ascontiguousarray(np.stack([r["dmatT"] for r in st["ret"]])),
        xibc=np.ascontiguousarray(np.stack([r["xibc"] for r in st["ret"]])),
        zetacol=np.ascontiguousarray(np.concatenate([r["zetacol"] for r in st["ret"]], axis=1)),
        gchunk=np.ascontiguousarray(np.concatenate([r["gchunk"] for r in st["ret"]], axis=1)),
        gng=np.ascontiguousarray(np.asarray(inp["ret_gn_g"], np.float32)[0].reshape(8, 256)),
        zpow=st["zpow"],
        wgates=np.ascontiguousarray(w_in[:, 15360:19456]), pa=np.asarray(inp["p_a"], np.float32)[0], pb=np.asarray(inp["p_b"], np.float32)[0],
        wo=np.asarray(inp["w_o"], np.float32)[0], rw=np.asarray(inp["router_w"], np.float32)[0],
        rbias=np.asarray(inp["router_bias"], np.float32)[0][None, :].copy(),
        wge=np.asarray(inp["w_gate_e"], np.float32)[0], wue=np.asarray(inp["w_up_e"], np.float32)[0],
        wde=np.asarray(inp["w_down_e"], np.float32)[0], wgs=np.asarray(inp["w_gate_s"], np.float32)[0],
        wus=np.asarray(inp["w_up_s"], np.float32)[0], wds=np.asarray(inp["w_down_s"], np.float32)[0],
    )
    maps = []
    cpart = np.arange(128)[:, None]
    for c in range(NCORES):
        shift = S - TS * (c + 1)
        xs_ = np.zeros((S, D), np.float32)
        xs_[shift:] = x[0:S - shift]
        cosT = np.zeros((128, S), np.float32)
        sinT = np.zeros((128, S), np.float32)
        cosT[:, shift:] = st["cosT"][:, 0:S - shift]
        sinT[:, shift:] = st["sinT"][:, 0:S - shift]
        kbias = np.zeros((128, 3, 64), np.float32)
        for g, d in enumerate(DILS):
            for N in range(64 // d):
                kbias[:, g, N] = np.where((N * 128 + cpart[:, 0]) * d >= shift, 0.0, NEG)
        vmask = np.tile(((np.arange(64) * 128) >= shift).astype(np.float32)[None, :], (128, 1))
        m = dict(shared)
        m.update(xT=np.ascontiguousarray(xs_.T), cosT=cosT, sinT=sinT, kbias=np.ascontiguousarray(kbias.reshape(128, 192)), vmask=vmask,
                 xTs=np.ascontiguousarray(x[c * TS:(c + 1) * TS].T), xs=np.ascontiguousarray(x[c * TS:(c + 1) * TS]))
        maps.append(m)
    return maps


def kernel(**inputs):
    st = static_tables()
    if "fused" not in _CACHE:
        _CACHE["fused"] = build_fused()
    res = run_bass_kernel_spmd(_CACHE["fused"], fused_inputs(inputs, st), core_ids=list(range(NCORES)))
    out = np.concatenate([np.asarray(res.results[c]["out"], np.float32) for c in range(NCORES)], axis=0)
    return out[None, :, :].astype(np.float32)
```
